# Optimizing a Trainium2 kernel written in Bass

```python
import jax, jax.numpy as jnp
from jax import lax
import numpy as np

D_MODEL = 1024
BATCH = 16
SEQ = 2048
DEPTH = 2

GDN_HEADS = 8
GDN_DK = 128
GDN_DV = 128
CONV_K = 5
CHUNK = 64
MLA_HEADS = 8
Q_LORA = 384
KV_LORA = 256
QK_NOPE = 128
QK_ROPE = 64
V_HEAD = 128
ROPE_BASE = 10000.0
Q_BLOCK = 128
N_GROUPS = 8
EXPERTS_PER_GROUP = 8
N_EXPERTS = N_GROUPS * EXPERTS_PER_GROUP
TOP_K_IN_GROUP = 2
D_EXPERT = 512
MOE_BLOCK = 128
DN_ALPHA = (2 * DEPTH) ** 0.25
DN_BETA = (8 * DEPTH) ** -0.25
LN_EPS = 1e-5
RMS_EPS = 1e-6

GDN_QK_W = GDN_HEADS * GDN_DK
GDN_V_W = GDN_HEADS * GDN_DV
MLA_Q_W = MLA_HEADS * (QK_NOPE + QK_ROPE)
MLA_KV_W = MLA_HEADS * (QK_NOPE + V_HEAD)
IN_WIDTHS = (GDN_QK_W, GDN_QK_W, GDN_V_W, GDN_V_W, 2 * GDN_HEADS, 2 * GDN_HEADS,
             Q_LORA, KV_LORA, QK_ROPE, 2 * D_MODEL)
IN_DIM = sum(IN_WIDTHS)

kernel_name = "hybrid_gdn_mla_hmoe_deepnorm"


def layer_norm(x, g, b):
    xf = x.astype(jnp.float32)
    xc = xf - jnp.mean(xf, -1, keepdims=True)
    var = jnp.mean(xc * xc, -1, keepdims=True)
    return (xc * lax.rsqrt(var + LN_EPS) * g.astype(jnp.float32) + b.astype(jnp.float32)).astype(x.dtype)


def rms_norm(x, w):
    xf = x.astype(jnp.float32)
    return (xf * lax.rsqrt(jnp.mean(xf * xf, -1, keepdims=True) + RMS_EPS) * w.astype(jnp.float32)).astype(x.dtype)


def l2_normalize(t):
    return t * lax.rsqrt(jnp.sum(t * t, -1, keepdims=True) + RMS_EPS)


def rope_rotate(t, cos, sin):
    half = t.shape[-1] // 2
    t1 = t[..., :half].astype(jnp.float32)
    t2 = t[..., half:].astype(jnp.float32)
    return jnp.concatenate([t1 * cos - t2 * sin, t1 * sin + t2 * cos], -1).astype(t.dtype)


def short_conv(x, w):
    c = x.shape[-1]
    y = lax.conv_general_dilated(x, w[:, None, :].astype(x.dtype), window_strides=(1,),
                                 padding=[(CONV_K // 2, CONV_K // 2)],
                                 dimension_numbers=('NWC', 'WIO', 'NWC'),
                                 feature_group_count=c)
    return jax.nn.silu(y)


def unit_lower_inverse(n_mat):
    c = n_mat.shape[-1]
    t_mat = jnp.eye(c, dtype=n_mat.dtype) + n_mat
    p_mat = n_mat
    for _ in range(int(np.log2(c)) - 1):
        p_mat = p_mat @ p_mat
        t_mat = t_mat + t_mat @ p_mat
    return t_mat


def chunk_gated_delta(q, k, v, g, beta):
    bsz, nh, s, dk = q.shape
    dv = v.shape[-1]
    n = s // CHUNK
    q = q.reshape(bsz, nh, n, CHUNK, dk)
    k = k.reshape(bsz, nh, n, CHUNK, dk)
    v = v.reshape(bsz, nh, n, CHUNK, dv)
    g = g.reshape(bsz, nh, n, CHUNK)
    beta = beta.reshape(bsz, nh, n, CHUNK)
    b = jnp.cumsum(g, axis=-1)
    idx = jnp.arange(CHUNK)
    incl = idx[:, None] >= idx[None, :]
    strict = idx[:, None] > idx[None, :]
    gamma = jnp.exp(jnp.where(incl, b[..., :, None] - b[..., None, :], -jnp.inf))
    kk = jnp.einsum('bhncd,bhnmd->bhncm', k, k)
    neg_a = jnp.where(strict, -beta[..., :, None] * kk * gamma, 0.0)
    t_inv = unit_lower_inverse(neg_a)
    w = t_inv @ (beta[..., None] * v)
    kc = t_inv @ ((beta * jnp.exp(b))[..., None] * k)
    p = jnp.einsum('bhncd,bhnmd->bhncm', q, k) * gamma
    qd = q * jnp.exp(b)[..., None]
    kd = k * jnp.exp(b[..., -1:] - b)[..., None]
    g_end = jnp.exp(b[..., -1])
    xs = (jnp.moveaxis(w, 2, 0), jnp.moveaxis(kc, 2, 0), jnp.moveaxis(p, 2, 0),
          jnp.moveaxis(qd, 2, 0), jnp.moveaxis(kd, 2, 0), jnp.moveaxis(g_end, 2, 0))

    def step(state, inp):
        w_c, kc_c, p_c, qd_c, kd_c, ge_c = inp
        u = w_c - kc_c @ state
        o = qd_c @ state + p_c @ u
        state = ge_c[..., None, None] * state + jnp.einsum('bhck,bhcv->bhkv', kd_c, u)
        return state, o

    s0 = jnp.zeros((bsz, nh, dk, dv), q.dtype)
    _, o = lax.scan(step, s0, xs)
    return jnp.moveaxis(o, 0, 2).reshape(bsz, nh, s, dv)


def gdn_branch(q, k, v, z, a, bt, conv_w, a_log, dt_bias, norm_w, w_o):
    dtype = q.dtype
    bsz, s, _ = q.shape
    f32 = jnp.float32
    qkv = short_conv(jnp.concatenate([q, k, v], -1), conv_w)
    q, k, v = jnp.split(qkv, [GDN_QK_W, 2 * GDN_QK_W], axis=-1)

    def heads(t, d):
        return jnp.swapaxes(t.reshape(bsz, s, GDN_HEADS, d), 1, 2).astype(f32)

    q = l2_normalize(heads(q, GDN_DK)) * (GDN_DK ** -0.5)
    k = l2_normalize(heads(k, GDN_DK))
    v = heads(v, GDN_DV)
    g = -jnp.exp(a_log.astype(f32).reshape(-1)) * jax.nn.softplus(a.astype(f32) + dt_bias.astype(f32).reshape(-1))
    beta = jax.nn.sigmoid(bt.astype(f32))
    g = jnp.swapaxes(g, 1, 2)
    beta = jnp.swapaxes(beta, 1, 2)

    def both_dirs(fwd, bwd):
        return jnp.concatenate([fwd, jnp.flip(bwd, axis=2)], axis=1)

    o2 = chunk_gated_delta(both_dirs(q, q), both_dirs(k, k), both_dirs(v, v),
                           both_dirs(g[:, :GDN_HEADS], g[:, GDN_HEADS:]),
                           both_dirs(beta[:, :GDN_HEADS], beta[:, GDN_HEADS:]))
    o = o2[:, :GDN_HEADS] + jnp.flip(o2[:, GDN_HEADS:], axis=2)
    o = jnp.swapaxes(o, 1, 2)
    o = rms_norm(o, norm_w) * jax.nn.silu(z.reshape(bsz, s, GDN_HEADS, GDN_DV).astype(f32))
    return o.reshape(bsz, s, GDN_V_W).astype(dtype) @ w_o


def mla_branch(c_q, c_kv, k_rope, cos, sin, q_norm_w, w_uq, kv_norm_w, w_ukv, w_o):
    bsz, s, _ = c_q.shape
    f32 = jnp.float32
    q = (rms_norm(c_q, q_norm_w) @ w_uq).reshape(bsz, s, MLA_HEADS, QK_NOPE + QK_ROPE)
    q_nope = q[..., :QK_NOPE]
    q_rope = rope_rotate(q[..., QK_NOPE:], cos[:, :, None, :], sin[:, :, None, :])
    kv = (rms_norm(c_kv, kv_norm_w) @ w_ukv).reshape(bsz, s, MLA_HEADS, QK_NOPE + V_HEAD)
    k_nope = kv[..., :QK_NOPE]
    v = kv[..., QK_NOPE:]
    k_rope = rope_rotate(k_rope, cos, sin)
    scale = (QK_NOPE + QK_ROPE) ** -0.5
    nb = s // Q_BLOCK

    def to_blocks(t):
        return jnp.swapaxes(t.reshape((bsz, nb, Q_BLOCK) + t.shape[2:]), 0, 1)

    def attend(blk):
        qn, qr = blk
        sc = (jnp.einsum('bqhd,bkhd->bhqk', qn, k_nope, preferred_element_type=f32)
              + jnp.einsum('bqhr,bkr->bhqk', qr, k_rope, preferred_element_type=f32))
        pr = jax.nn.softmax(sc * scale, axis=-1)
        return jnp.einsum('bhqk,bkhd->bqhd', pr.astype(v.dtype), v)

    o = lax.map(attend, (to_blocks(q_nope), to_blocks(q_rope)))
    o = jnp.swapaxes(o, 0, 1).reshape(bsz, s, MLA_HEADS * V_HEAD)
    return o @ w_o


def hybrid_mixer(h, cos, sin, w_in, conv_w, a_log, dt_bias, gdn_norm_w, w_o_gdn,
                 mla_q_norm_w, w_uq, mla_kv_norm_w, w_ukv, w_o_mla, w_out):
    proj = h @ w_in
    q, k, v, z, a, bt, c_q, c_kv, k_rope, gates = jnp.split(proj, np.cumsum(IN_WIDTHS)[:-1].tolist(), axis=-1)
    y_gdn = gdn_branch(q, k, v, z, a, bt, conv_w, a_log, dt_bias, gdn_norm_w, w_o_gdn)
    y_mla = mla_branch(c_q, c_kv, k_rope, cos, sin, mla_q_norm_w, w_uq, mla_kv_norm_w, w_ukv, w_o_mla)
    g_gdn, g_mla = jnp.split(jax.nn.sigmoid(gates.astype(jnp.float32)), 2, axis=-1)
    y = g_gdn * y_gdn + g_mla * y_mla
    return y.astype(h.dtype) @ w_out


def hier_moe(h, w_rg, b_rg, w_re, b_re, w_gate, w_up, w_down):
    bsz, s, d = h.shape
    f32 = jnp.float32
    t = bsz * s
    xt = h.reshape(t, d)
    tok_idx = jnp.arange(t)
    lg = (xt @ w_rg).astype(f32) + b_rg.astype(f32)
    grp = jnp.argmax(lg, axis=-1)
    p_grp = jax.nn.softmax(lg, axis=-1)[tok_idx, grp][:, None]
    le = ((xt @ w_re).astype(f32) + b_re.astype(f32)).reshape(t, N_GROUPS, EXPERTS_PER_GROUP)
    le = le[tok_idx, grp]
    top_l, top_i = lax.top_k(le, TOP_K_IN_GROUP)
    gate = p_grp * jax.nn.softmax(top_l, axis=-1)
    expert = grp[:, None] * EXPERTS_PER_GROUP + top_i
    n_assign = t * TOP_K_IN_GROUP
    e_flat = expert.reshape(n_assign)
    tok_flat = jnp.broadcast_to(tok_idx[:, None], (t, TOP_K_IN_GROUP)).reshape(n_assign)
    gate_flat = gate.reshape(n_assign)
    order = jnp.argsort(e_flat)
    e_sorted = e_flat[order]
    counts = jnp.bincount(e_flat, length=N_EXPERTS)
    padded = (counts + MOE_BLOCK - 1) // MOE_BLOCK * MOE_BLOCK
    pad_end = jnp.cumsum(padded)
    pad_start = pad_end - padded
    start = jnp.cumsum(counts) - counts
    dest = pad_start[e_sorted] + jnp.arange(n_assign) - start[e_sorted]
    n_blocks = -(-(n_assign + N_EXPERTS * (MOE_BLOCK - 1)) // MOE_BLOCK)
    n_rows = n_blocks * MOE_BLOCK
    row_tok = jnp.full((n_rows,), t, jnp.int32).at[dest].set(tok_flat[order].astype(jnp.int32))
    row_gate = jnp.zeros((n_rows,), f32).at[dest].set(gate_flat[order])
    blk_expert = jnp.minimum(jnp.searchsorted(pad_end, jnp.arange(n_blocks) * MOE_BLOCK, side='right'), N_EXPERTS - 1)
    xpad = jnp.concatenate([xt, jnp.zeros((1, d), xt.dtype)], axis=0)
    xb = xpad[row_tok].reshape(n_blocks, MOE_BLOCK, d)

    def expert_block(args):
        xblk, e = args
        hid = jax.nn.silu(xblk @ w_gate[e]) * (xblk @ w_up[e])
        return hid @ w_down[e]

    yb = lax.map(expert_block, (xb, blk_expert)).reshape(n_rows, d)
    y = jax.ops.segment_sum(yb.astype(f32) * row_gate[:, None], row_tok, num_segments=t + 1)[:t]
    return y.astype(h.dtype).reshape(bsz, s, d)


def setup_inputs(seed: int = 0) -> dict:
    key = jax.random.key(seed)
    k = jax.random.split(key, 32)
    f32 = jnp.float32
    L = DEPTH
    D = D_MODEL

    def normal(kk, shape, scale):
        return jax.random.normal(kk, shape, f32) * scale

    x = normal(k[0], (BATCH, SEQ, D), 1.0)
    positions = jnp.arange(SEQ, dtype=jnp.int32)[None, :] + jax.random.randint(k[1], (BATCH, 1), 0, 64, dtype=jnp.int32)
    ln_in_g = 1.0 + normal(k[2], (D,), 0.02)
    ln_in_b = normal(k[3], (D,), 0.02)
    col_scale = jnp.ones((IN_DIM,), f32).at[2 * GDN_QK_W:2 * GDN_QK_W + GDN_V_W].set(DN_BETA)
    w_in = normal(k[4], (L, D, IN_DIM), D ** -0.5) * col_scale
    conv_w = normal(k[5], (L, CONV_K, 2 * GDN_QK_W + GDN_V_W), CONV_K ** -0.5)
    a_log = jnp.log(jax.random.uniform(k[6], (L, 2, GDN_HEADS), f32, 1.0, 16.0))
    dt = jnp.exp(jax.random.uniform(k[7], (L, 2, GDN_HEADS), f32, np.log(1e-3), np.log(1e-1)))
    dt_bias = dt + jnp.log(-jnp.expm1(-dt))
    gdn_norm_w = 1.0 + normal(k[8], (L, GDN_DV), 0.02)
    w_o_gdn = normal(k[9], (L, GDN_V_W, D), GDN_V_W ** -0.5)
    mla_q_norm_w = 1.0 + normal(k[10], (L, Q_LORA), 0.02)
    w_uq = normal(k[11], (L, Q_LORA, MLA_Q_W), Q_LORA ** -0.5)
    mla_kv_norm_w = 1.0 + normal(k[12], (L, KV_LORA), 0.02)
    ukv_scale = jnp.tile(jnp.concatenate([jnp.ones((QK_NOPE,), f32), jnp.full((V_HEAD,), DN_BETA, f32)]), MLA_HEADS)
    w_ukv = normal(k[13], (L, KV_LORA, MLA_KV_W), KV_LORA ** -0.5) * ukv_scale
    w_o_mla = normal(k[14], (L, MLA_HEADS * V_HEAD, D), (MLA_HEADS * V_HEAD) ** -0.5)
    w_out = normal(k[15], (L, D, D), DN_BETA * D ** -0.5)
    ln1_g = 1.0 + normal(k[16], (L, D), 0.02)
    ln1_b = normal(k[17], (L, D), 0.02)
    w_router_group = normal(k[18], (L, D, N_GROUPS), D ** -0.5)
    b_router_group = normal(k[19], (L, N_GROUPS), 0.01)
    w_router_expert = normal(k[20], (L, D, N_EXPERTS), D ** -0.5)
    b_router_expert = normal(k[21], (L, N_EXPERTS), 0.01)
    w_gate = normal(k[22], (L, N_EXPERTS, D, D_EXPERT), D ** -0.5)
    w_up = normal(k[23], (L, N_EXPERTS, D, D_EXPERT), D ** -0.5)
    w_down = normal(k[24], (L, N_EXPERTS, D_EXPERT, D), DN_BETA * D_EXPERT ** -0.5)
    ln2_g = 1.0 + normal(k[25], (L, D), 0.02)
    ln2_b = normal(k[26], (L, D), 0.02)
    return {"x": x, "positions": positions, "ln_in_g": ln_in_g, "ln_in_b": ln_in_b,
            "w_in": w_in, "conv_w": conv_w, "a_log": a_log, "dt_bias": dt_bias,
            "gdn_norm_w": gdn_norm_w, "w_o_gdn": w_o_gdn, "mla_q_norm_w": mla_q_norm_w,
            "w_uq": w_uq, "mla_kv_norm_w": mla_kv_norm_w, "w_ukv": w_ukv, "w_o_mla": w_o_mla,
            "w_out": w_out, "ln1_g": ln1_g, "ln1_b": ln1_b,
            "w_router_group": w_router_group, "b_router_group": b_router_group,
            "w_router_expert": w_router_expert, "b_router_expert": b_router_expert,
            "w_gate": w_gate, "w_up": w_up, "w_down": w_down, "ln2_g": ln2_g, "ln2_b": ln2_b}


def reference(x, positions, ln_in_g, ln_in_b, w_in, conv_w, a_log, dt_bias, gdn_norm_w, w_o_gdn,
              mla_q_norm_w, w_uq, mla_kv_norm_w, w_ukv, w_o_mla, w_out, ln1_g, ln1_b,
              w_router_group, b_router_group, w_router_expert, b_router_expert,
              w_gate, w_up, w_down, ln2_g, ln2_b):
    f32 = jnp.float32
    half = QK_ROPE // 2
    inv_freq = jnp.power(ROPE_BASE, -jnp.arange(half, dtype=f32) / half)
    ang = positions.astype(f32)[..., None] * inv_freq
    cos, sin = jnp.cos(ang), jnp.sin(ang)
    h = layer_norm(x, ln_in_g, ln_in_b)
    for l in range(DEPTH):
        m = hybrid_mixer(h, cos, sin, w_in[l], conv_w[l], a_log[l], dt_bias[l], gdn_norm_w[l], w_o_gdn[l],
                         mla_q_norm_w[l], w_uq[l], mla_kv_norm_w[l], w_ukv[l], w_o_mla[l], w_out[l])
        h = layer_norm(DN_ALPHA * h + m, ln1_g[l], ln1_b[l])
        f = hier_moe(h, w_router_group[l], b_router_group[l], w_router_expert[l], b_router_expert[l],
                     w_gate[l], w_up[l], w_down[l])
        h = layer_norm(DN_ALPHA * h + f, ln2_g[l], ln2_b[l])
    return h
```

```python
import contextlib
import numpy as np
import concourse.bass as bass
import concourse.mybir as mybir
from concourse.bass_utils import run_bass_kernel_spmd

F32 = mybir.dt.float32
BF16 = mybir.dt.bfloat16
I32 = mybir.dt.int32
U32 = mybir.dt.uint32
AF = mybir.ActivationFunctionType
ALU = mybir.AluOpType
AX = mybir.AxisListType

D = 1024
SEQ = 2048
NSEQ = 2
T = NSEQ * SEQ
NT = T // 128
DEPTH = 2
H = 8
IN_DIM = 6880
OFF_Q, OFF_K, OFF_V, OFF_Z, OFF_A, OFF_BT, OFF_CQ, OFF_CKV, OFF_KR, OFF_G = (
    0, 1024, 2048, 3072, 4096, 4112, 4128, 4512, 4768, 4832)
QL, KVL, ROPE = 384, 256, 64
NE = 64
DEXP = 512
NBLK = 128
NROWS = NBLK * 128
DN_ALPHA = (2 * DEPTH) ** 0.25
LN_EPS = 1e-5
RMS_EPS = 1e-6
CH = 128
NCH = SEQ // CH


class Buf:
    def __init__(self, t, name):
        self.t = t
        self.name = name
        self.w = None
        self.r = {}

    def __getitem__(self, idx):
        return self.t[idx]


class KB:
    ENG = ("pe", "act", "dve", "pool", "sp")

    def __init__(self):
        self.nc = bass.Bass("TRN2", target_bir_lowering=False)
        nc = self.nc
        self.es = contextlib.ExitStack()
        self.eng = {"pe": nc.tensor, "act": nc.scalar, "dve": nc.vector, "pool": nc.gpsimd, "sp": nc.sync}
        self.sems = {}
        self.cnt = {}
        self.known = {e: {} for e in self.ENG}
        for e in self.ENG:
            self.sems[e] = self.es.enter_context(nc.semaphore("sem_" + e))
            self.cnt[e] = 0
        self.dq = {}
        for q, n in (("sp", 12), ("pool", 28), ("act", 2)):
            keys = []
            for i in range(n):
                k = "d_%s_%d" % (q, i)
                self.sems[k] = self.es.enter_context(nc.semaphore(k))
                self.cnt[k] = 0
                keys.append(k)
            self.dq[q] = [keys, 0]
        self.ninst = 0
        self.uid = 0
        self.scr = None
        self.serialize = False
        self.psum_rar = True
        self.chain = False
        self.chain_engines = ("pe", "act", "dve", "pool")
        self.last_op = None
        self.flush_nop = False

    def sbuf(self, stack, name, shape, dt):
        self.uid += 1
        name = "%s_u%d" % (name, self.uid)
        return Buf(stack.enter_context(self.nc.sbuf_tensor(name, list(shape), dt)), name)

    def psum(self, stack, name, shape, dt):
        self.uid += 1
        name = "%s_u%d" % (name, self.uid)
        b = Buf(stack.enter_context(self.nc.psum_tensor(name, list(shape), dt)), name)
        b.is_psum = True
        return b

    def dram(self, name, shape, dt, kind="Internal"):
        return Buf(self.nc.dram_tensor(name, list(shape), dt, kind=kind).ap(), name)

    def dram_tiles(self, name, shape, dt, n, kind="Internal"):
        ap = self.nc.dram_tensor(name, list(shape), dt, kind=kind).ap()
        return [Buf(ap, "%s_%d" % (name, i)) for i in range(n)]

    def _waits(self, en, r, w, is_dma=False):
        need = {}
        for b in r:
            if b.w is not None:
                need[b.w[0]] = max(need.get(b.w[0], 0), b.w[1])
            if self.psum_rar and getattr(b, "is_psum", False) and en != "pe":
                for sk, val in b.r.items():
                    if sk != en:
                        need[sk] = max(need.get(sk, 0), val)
        for b in w:
            if b.w is not None and (is_dma or b.w[0] != en):
                need[b.w[0]] = max(need.get(b.w[0], 0), b.w[1])
            for sk, val in b.r.items():
                if is_dma or sk != en:
                    need[sk] = max(need.get(sk, 0), val)
        E = self.eng[en]
        kn = self.known[en]
        for sk, val in need.items():
            if kn.get(sk, 0) < val:
                E.wait_ge(self.sems[sk], val)
                kn[sk] = val
                self.ninst += 1

    def _done(self, tok, r, w):
        for b in r:
            b.r[tok[0]] = max(b.r.get(tok[0], 0), tok[1])
        for b in w:
            b.w = tok
            b.r = {}

    def op(self, en, fn, r=(), w=(), inc=True):
        self._waits(en, r, w)
        if self.chain and en in self.chain_engines and self.last_op is not None and self.last_op[0] != en:
            lk, lv = self.last_op
            if self.known[en].get(lk, 0) < lv:
                self.eng[en].wait_ge(self.sems[lk], lv)
                self.known[en][lk] = lv
        ins = fn(self.eng[en])
        self.ninst += 1
        if inc:
            self.cnt[en] += 1
            ins.then_inc(self.sems[en], 1)
            self._done((en, self.cnt[en]), r, w)
        else:
            self._done((en, self.cnt[en] + 1), r, w)
        if en in self.chain_engines:
            self.last_op = (en, self.cnt[en] + (0 if inc else 1))
        if self.flush_nop and self.scr is not None and en in ("dve", "act") and any(getattr(b, "is_psum", False) for b in r):
            if en == "dve":
                self.eng[en].memset(self.scr[0:1, 0:1], 0.0)
            else:
                self.eng[en].memzero(self.scr[0:1, 2:3])
            self.ninst += 1
        if self.serialize and inc:
            self.barrier()
        return ins

    def dma(self, q, fn, r=(), w=()):
        keys, i = self.dq[q]
        k = keys[i % len(keys)]
        self.dq[q][1] = i + 1
        self._waits(q, r, w, True)
        kn = self.known[q]
        if kn.get(k, 0) < self.cnt[k]:
            self.eng[q].wait_ge(self.sems[k], self.cnt[k])
            kn[k] = self.cnt[k]
        ins = fn(self.eng[q])
        self.cnt[k] += 16
        ins.then_inc(self.sems[k], 16)
        self.ninst += 1
        self._done((k, self.cnt[k]), r, w)
        return ins

    def barrier(self):
        for en in self.ENG:
            kn = self.known[en]
            for sk, val in self.cnt.items():
                if sk != en and val > 0 and kn.get(sk, 0) < val:
                    self.eng[en].wait_ge(self.sems[sk], val)
                    kn[sk] = val

    def finish(self):
        kn = self.known["sp"]
        for sk, val in self.cnt.items():
            if sk != "sp" and val > 0 and kn.get(sk, 0) < val:
                self.nc.sync.wait_ge(self.sems[sk], val)
                kn[sk] = val
        self.es.close()


def mm(kb, ob, oap, lb, lap, rb, rap, start=True, stop=True, inc=None):
    if inc is None:
        inc = stop
    return kb.op("pe", lambda e: e.matmul(oap, lhsT=lap, rhs=rap, start=start, stop=stop), r=[lb, rb], w=[ob], inc=inc)


def tr(kb, ob, oap, ib, iap, idb, idap, inc=True):
    return kb.op("pe", lambda e: e.transpose(oap, iap, idap), r=[ib, idb], w=[ob], inc=inc)


def act(kb, ob, oap, ib, iap, func, bias=None, scale=None, extra_r=(), accum=None, en="act"):
    kw = {}
    if bias is not None:
        kw["bias"] = bias
    if scale is not None:
        kw["scale"] = scale
    w = [ob]
    if accum is not None:
        kw["accum_out"] = accum[1]
        w.append(accum[0])
    return kb.op("act", lambda e: e.activation(out=oap, in_=iap, func=func, **kw), r=[ib] + list(extra_r), w=w)


def tt(kb, en, ob, oap, ab, aap, bb, bap, op):
    return kb.op(en, lambda e: e.tensor_tensor(out=oap, in0=aap, in1=bap, op=op), r=[ab, bb], w=[ob])


def tsc(kb, en, ob, oap, ib, iap, s1, s2, op0, op1=None, extra_r=()):
    if op1 is None:
        return kb.op(en, lambda e: e.tensor_scalar(out=oap, in0=iap, scalar1=s1, scalar2=None, op0=op0),
                     r=[ib] + list(extra_r), w=[ob])
    return kb.op(en, lambda e: e.tensor_scalar(out=oap, in0=iap, scalar1=s1, scalar2=s2, op0=op0, op1=op1),
                 r=[ib] + list(extra_r), w=[ob])


def stt(kb, en, ob, oap, ab, aap, scalar, bb, bap, op0, op1, extra_r=()):
    return kb.op(en, lambda e: e.scalar_tensor_tensor(out=oap, in0=aap, scalar=scalar, in1=bap, op0=op0, op1=op1),
                 r=[ab, bb] + list(extra_r), w=[ob])


def cp(kb, en, ob, oap, ib, iap):
    if en == "act":
        return kb.op("act", lambda e: e.copy(oap, iap), r=[ib], w=[ob])
    return kb.op(en, lambda e: e.tensor_copy(oap, iap), r=[ib], w=[ob])


def memset(kb, en, ob, oap, val):
    return kb.op(en, lambda e: e.memset(oap, val), r=[], w=[ob])


def dma(kb, q, ob, oap, ib, iap):
    return kb.dma(q, lambda e: e.dma_start(out=oap, in_=iap), r=[ib], w=[ob])


class Consts:
    pass


def rsqrt(kb, C, ob, oap, ib, iap, eps):
    act(kb, ob, oap, ib, iap, AF.Sqrt, bias=C.eps_tile(eps)[0:oap.shape[0], 0:1], extra_r=[C.eps_buf])
    kb.op("dve", lambda e: e.reciprocal(oap, oap), r=[ob], w=[ob])


def setup_consts(kb, st):
    c = Consts()
    kb.scr = kb.sbuf(st, "kb_scr", [128, 8], F32).t
    c.ones_f = kb.sbuf(st, "ones_f", [128, 128], F32)
    c.ones_b = kb.sbuf(st, "ones_b", [128, 128], BF16)
    c.id_f = kb.sbuf(st, "id_f", [128, 128], F32)
    c.id_b = kb.sbuf(st, "id_b", [128, 128], BF16)
    c.id4_f = kb.sbuf(st, "id4_f", [128, 4, 128], F32)
    c.triF = kb.sbuf(st, "triF", [128, 128], F32)
    c.triB = kb.sbuf(st, "triB", [128, 128], F32)
    c.fill_zero = kb.nc.gpsimd.to_reg(0.0)
    c.fill_neg = kb.nc.gpsimd.to_reg(-30000.0)
    c.fill_pos = kb.nc.gpsimd.to_reg(30000.0)
    c.eps_buf = kb.sbuf(st, "eps_c", [128, 2], F32)
    memset(kb, "pool", c.eps_buf, c.eps_buf[:, 0:1], LN_EPS)
    memset(kb, "pool", c.eps_buf, c.eps_buf[:, 1:2], RMS_EPS)
    c.eps_tile = lambda eps: c.eps_buf[:, 0:1] if eps == LN_EPS else c.eps_buf[:, 1:2]
    memset(kb, "pool", c.ones_f, c.ones_f[:], 1.0)
    memset(kb, "pool", c.ones_b, c.ones_b[:], 1.0)
    kb.op("pool", lambda e: e.affine_select(out=c.id_f[:], in_=c.ones_f[:], pattern=[[-1, 128]],
                                            compare_op=ALU.is_equal, fill=c.fill_zero, base=0, channel_multiplier=1),
          r=[c.ones_f], w=[c.id_f])
    cp(kb, "pool", c.id_b, c.id_b[:], c.id_f, c.id_f[:])
    for i in range(4):
        cp(kb, "pool", c.id4_f, c.id4_f[:, i, :], c.id_f, c.id_f[:])
    kb.op("pool", lambda e: e.affine_select(out=c.triF[:], in_=c.ones_f[:], pattern=[[1, 128]],
                                            compare_op=ALU.is_ge, fill=c.fill_zero, base=0, channel_multiplier=-1),
          r=[c.ones_f], w=[c.triF])
    kb.op("pool", lambda e: e.affine_select(out=c.triB[:], in_=c.ones_f[:], pattern=[[-1, 128]],
                                            compare_op=ALU.is_ge, fill=c.fill_zero, base=0, channel_multiplier=1),
          r=[c.ones_f], w=[c.triB])
    zero4 = kb.sbuf(st, "zero4", [128, 4, 128], F32)
    memset(kb, "pool", zero4, zero4[:, :, :], 0.0)
    c.mT, c.mS = [], []
    for d in range(2):
        if d == 0:
            patT, cmT, pat, cm = [[0, 4], [1, 128]], -1, [[0, 4], [-1, 128]], 1
        else:
            patT, cmT, pat, cm = [[0, 4], [-1, 128]], 1, [[0, 4], [1, 128]], -1
        mT = kb.sbuf(st, "mT%d" % d, [128, 4, 128], F32)
        mS = kb.sbuf(st, "mS%d" % d, [128, 4, 128], F32)
        kb.op("pool", lambda e, mT=mT, patT=patT, cmT=cmT: e.affine_select(
            out=mT[:, :, :], in_=zero4[:, :, :], pattern=patT, compare_op=ALU.is_ge, fill=c.fill_neg, base=0,
            channel_multiplier=cmT), r=[zero4], w=[mT])
        kb.op("pool", lambda e, mS=mS, pat=pat, cm=cm: e.affine_select(
            out=mS[:, :, :], in_=zero4[:, :, :], pattern=pat, compare_op=ALU.is_ge, fill=c.fill_pos, base=-1,
            channel_multiplier=cm), r=[zero4], w=[mS])
        c.mT.append(mT)
        c.mS.append(mS)
    kb.barrier()
    return c


def sin_reduced(kb, st, nm, ob, oap, ib, iap, shift, shape):
    u = kb.sbuf(st, nm + "_u", shape, F32)
    ki = kb.sbuf(st, nm + "_ki", shape, I32)
    kf = kb.sbuf(st, nm + "_kf", shape, F32)
    mk = kb.sbuf(st, nm + "_mk", shape, F32)
    full = tuple(slice(None) for _ in shape)
    inv2pi = float(1.0 / (2.0 * np.pi))
    tsc(kb, "dve", u, u[full], ib, iap, inv2pi, float(shift) * inv2pi, ALU.mult, ALU.add)
    cp(kb, "dve", ki, ki[full], u, u[full])
    cp(kb, "dve", kf, kf[full], ki, ki[full])
    tt(kb, "dve", u, u[full], u, u[full], kf, kf[full], ALU.subtract)
    tsc(kb, "dve", mk, mk[full], u, u[full], 0.5, None, ALU.is_gt)
    tt(kb, "dve", u, u[full], u, u[full], mk, mk[full], ALU.subtract)
    tsc(kb, "dve", mk, mk[full], u, u[full], -0.5, None, ALU.is_lt)
    tt(kb, "dve", u, u[full], u, u[full], mk, mk[full], ALU.add)
    tsc(kb, "dve", u, u[full], u, u[full], -0.4999999, 0.4999999, ALU.max, ALU.min)
    act(kb, ob, oap, u, u[full], AF.Sin, scale=float(2.0 * np.pi))


def layernorm_tile(kb, wk, x_b, g_bc, b_bc, out_b, eps=LN_EPS):
    stats, mv, rstd = wk["stats"], wk["mv"], wk["rstd"]
    for ci in range(2):
        kb.op("dve", lambda e, ci=ci: e.bn_stats(stats[:, ci, :], x_b[:, ci * 512:(ci + 1) * 512]), r=[x_b], w=[stats])
    kb.op("dve", lambda e: e.bn_aggr(mv[:, :], stats[:, :, :]), r=[stats], w=[mv])
    rsqrt(kb, wk["C"], rstd, rstd[:, :], mv, mv[:, 1:2], eps)
    tsc(kb, "dve", out_b, out_b[:, :], x_b, x_b[:, :], mv[:, 0:1], rstd[:, 0:1], ALU.subtract, ALU.mult,
        extra_r=[mv, rstd])
    tt(kb, "dve", out_b, out_b[:, :], out_b, out_b[:, :], g_bc, g_bc[:, :], ALU.mult)
    tt(kb, "dve", out_b, out_b[:, :], out_b, out_b[:, :], b_bc, b_bc[:, :], ALU.add)


class IO:
    pass


def declare_io(kb, dbg=None, small=False):
    nc = kb.nc
    io = IO()

    def inp(name, shape, dt=F32):
        b = Buf(nc.dram_tensor(name, list(shape), dt, kind="ExternalInput").ap(), name)
        setattr(io, name, b)
        return b
    inp("x", [T, D])
    inp("positions", [1, T], I32)
    inp("ln_in_g", [1, D]); inp("ln_in_b", [1, D])
    inp("w_in", [DEPTH, D, IN_DIM])
    inp("conv_w", [DEPTH, 5, 3072])
    inp("a_log", [DEPTH, 1, 16]); inp("dt_bias", [DEPTH, 1, 16])
    inp("gdn_norm_w", [DEPTH, 128, 1])
    inp("w_o_gdn", [DEPTH, D, D])
    inp("mla_q_norm_w", [DEPTH, QL, 1])
    inp("w_uq", [DEPTH, QL, 1536])
    inp("mla_kv_norm_w", [DEPTH, KVL, 1])
    inp("w_ukv", [DEPTH, KVL, 2048])
    inp("w_o_mla", [DEPTH, D, D])
    inp("w_out", [DEPTH, D, D])
    inp("ln1_g", [DEPTH, D]); inp("ln1_b", [DEPTH, D])
    inp("w_router_group", [DEPTH, D, 8]); inp("b_router_group", [DEPTH, 8])
    inp("w_router_expert", [DEPTH, D, NE]); inp("b_router_expert", [DEPTH, NE])
    if not small:
        inp("w_gate", [DEPTH, NE, D, DEXP]); inp("w_up", [DEPTH, NE, D, DEXP]); inp("w_down", [DEPTH, NE, DEXP, D])
    inp("ln2_g", [DEPTH, D]); inp("ln2_b", [DEPTH, D])
    io.out = kb.dram_tiles("out", [T, D], F32, NT, kind="ExternalOutput")
    io.h_res = kb.dram_tiles("h_res", [T, D], F32, NT)
    io.h_mid = kb.dram_tiles("h_mid", [T, D], F32, NT)
    io.hT = kb.dram_tiles("hT_scr", [128, 8, T], BF16, NT)
    io.cs = kb.dram("cs_scr", [2, 64, T], F32)
    io.xb = kb.dram("xb_scr", [NROWS, D], F32)
    io.yb = kb.dram("yb_scr", [NROWS, D], F32)
    io.og_scr = kb.dram("og_scr", [NSEQ, 128, 8, SEQ], BF16)
    io.om_scr = kb.dram("om_scr", [NSEQ, 128, 8, SEQ], BF16)
    io.dbg = {}
    for name, shape in (dbg or {}).items():
        io.dbg[name] = Buf(nc.dram_tensor(name, list(shape), F32, kind="ExternalOutput").ap(), name)
    return io


def emit_ln_rows(kb, C, st, nm, src_fn, g_ap, b_ap, io, tiles, h_dst, pre_fn=None):
    g_bc = kb.sbuf(st, nm + "g", [128, D], F32)
    b_bc = kb.sbuf(st, nm + "b", [128, D], F32)
    dma(kb, "sp", g_bc, g_bc[:, :], g_ap[0], g_ap[1].partition_broadcast(128))
    dma(kb, "sp", b_bc, b_bc[:, :], b_ap[0], b_ap[1].partition_broadcast(128))
    NB = 2
    xt = [kb.sbuf(st, nm + "x%d" % j, [128, D], F32) for j in range(NB)]
    ht = [kb.sbuf(st, nm + "h%d" % j, [128, D], F32) for j in range(NB)]
    hb = [kb.sbuf(st, nm + "hb%d" % j, [128, D], BF16) for j in range(NB)]
    hTt = [kb.sbuf(st, nm + "hT%d" % j, [128, 8, 128], BF16) for j in range(NB)]
    pst = [kb.psum(st, nm + "ps%d" % j, [128, 8, 128], BF16) for j in range(NB)]
    wk = [dict(C=C, stats=kb.sbuf(st, nm + "st%d" % j, [128, 2, 6], F32), mv=kb.sbuf(st, nm + "mv%d" % j, [128, 2], F32),
               rstd=kb.sbuf(st, nm + "rs%d" % j, [128, 1], F32)) for j in range(NB)]
    for n, i in enumerate(tiles):
        j = n % NB
        src_fn(i, xt[j])
        layernorm_tile(kb, wk[j], xt[j], g_bc, b_bc, ht[j])
        dma(kb, "sp", h_dst[i], h_dst[i][i * 128:(i + 1) * 128, :], ht[j], ht[j][:, :])
        cp(kb, "act", hb[j], hb[j][:, :], ht[j], ht[j][:, :])
        for c in range(8):
            tr(kb, pst[j], pst[j][:, c, :], hb[j], hb[j][:, c * 128:(c + 1) * 128], C.id_b, C.id_b[:, :], inc=(c == 7))
        cp(kb, "dve", hTt[j], hTt[j][:, :, :], pst[j], pst[j][:, :, :])
        dma(kb, "sp", io.hT[i], io.hT[i][:, :, i * 128:(i + 1) * 128], hTt[j], hTt[j][:, :, :])


def stage_prologue(kb, C, io, tiles):
    with contextlib.ExitStack() as st:
        def src(i, buf):
            dma(kb, "sp", buf, buf[:, :], io.x, io.x[i * 128:(i + 1) * 128, :])
        emit_ln_rows(kb, C, st, "p_", src, (io.ln_in_g, io.ln_in_g[0:1, :]), (io.ln_in_b, io.ln_in_b[0:1, :]), io,
                     tiles, io.h_res)
        kb.barrier()


def make_in_map(inp, core, small=False):
    f = lambda k: np.ascontiguousarray(np.asarray(inp[k], dtype=np.float32))
    m = {}
    m["x"] = f("x")[NSEQ * core:NSEQ * (core + 1)].reshape(T, D)
    m["positions"] = np.ascontiguousarray(np.asarray(inp["positions"], dtype=np.int32)[NSEQ * core:NSEQ * (core + 1)].reshape(1, T))
    m["ln_in_g"] = f("ln_in_g").reshape(1, D)
    m["ln_in_b"] = f("ln_in_b").reshape(1, D)
    m["a_log"] = f("a_log").reshape(DEPTH, 1, 16)
    m["dt_bias"] = f("dt_bias").reshape(DEPTH, 1, 16)
    m["gdn_norm_w"] = f("gdn_norm_w").reshape(DEPTH, 128, 1)
    m["mla_q_norm_w"] = f("mla_q_norm_w").reshape(DEPTH, QL, 1)
    m["mla_kv_norm_w"] = f("mla_kv_norm_w").reshape(DEPTH, KVL, 1)
    for k in ("w_in", "conv_w", "w_o_gdn", "w_uq", "w_ukv", "w_o_mla", "w_out", "ln1_g", "ln1_b", "w_router_group",
              "b_router_group", "w_router_expert", "b_router_expert", "w_gate", "w_up", "w_down", "ln2_g", "ln2_b"):
        if small and k in ("w_gate", "w_up", "w_down"):
            continue
        m[k] = f(k)
    return m


class Rot:
    def __init__(self, bufs):
        self.bufs = bufs
        self.i = 0

    def next(self):
        b = self.bufs[self.i % len(self.bufs)]
        self.i += 1
        return b


def load_w_fm(kb, io_w, w_ap, wt):
    kb.dma("pool", lambda e: e.dma_start(out=wt[:, :, :], in_=w_ap.rearrange("(k p) m -> p k m", p=128)),
           r=[io_w], w=[wt])


def gdn_seq(kb, C, io, l, b, load_hT, og_scr, cwT, dbg):
    w_in = io.w_in
    with contextlib.ExitStack() as st:
        g_tm = kb.sbuf(st, "g_tm", [128, 2, NCH, 8], F32)
        be_tm = kb.sbuf(st, "be_tm", [128, 2, NCH, 8], F32)
        b_tm = kb.sbuf(st, "b_tm", [128, 2, NCH, 8], F32)
        nbe_tm = kb.sbuf(st, "nbe_tm", [128, 2, NCH, 8], F32)
        v4 = lambda x: x[:, :, :, :].rearrange("p d c h -> p c d h")
        with contextlib.ExitStack() as s2:
            hT_b = load_hT(s2)
            wab = kb.sbuf(s2, "wab", [128, 8, 32], BF16)
            load_w_fm(kb, w_in, w_in[l, :, OFF_A:OFF_A + 32], wab)
            alog = kb.sbuf(s2, "alog", [128, 16], F32)
            dtb = kb.sbuf(s2, "dtb", [128, 16], F32)
            dma(kb, "sp", alog, alog[:, :], io.a_log, io.a_log[l, 0:1, :].partition_broadcast(128))
            dma(kb, "sp", dtb, dtb[:, :], io.dt_bias, io.dt_bias[l, 0:1, :].partition_broadcast(128))
            nea = kb.sbuf(s2, "nea", [128, 16], F32)
            act(kb, nea, nea[:, :], alog, alog[:, :], AF.Exp)
            tsc(kb, "dve", nea, nea[:, :], nea, nea[:, :], -1.0, None, ALU.mult)
            ps = kb.psum(s2, "ps_ab", [128, NCH, 32], F32)
            for ci in range(NCH):
                for k in range(8):
                    mm(kb, ps, ps[:, ci, :], hT_b, hT_b[:, k, ci * 128:(ci + 1) * 128], wab, wab[:, k, :],
                       start=(k == 0), stop=(k == 7))
            tmp = kb.sbuf(s2, "ab_tmp", [128, NCH, 16], F32)
            tmp4 = tmp[:, :, :].rearrange("p c (d h) -> p c d h", d=2)
            tt(kb, "dve", tmp, tmp[:, :, :], ps, ps[:, :, 0:16], dtb, dtb[:, :].unsqueeze(1).to_broadcast([128, NCH, 16]), ALU.add)
            act(kb, tmp, tmp[:, :, :], tmp, tmp[:, :, :], AF.Exp)
            act(kb, tmp, tmp[:, :, :], tmp, tmp[:, :, :], AF.Ln, bias=1.0)
            tt(kb, "dve", g_tm, v4(g_tm), tmp, tmp4, nea,
               nea[:, :].rearrange("p (d h) -> p d h", d=2).unsqueeze(1).to_broadcast([128, NCH, 2, 8]), ALU.mult)
            act(kb, be_tm, v4(be_tm), ps, ps[:, :, 16:32].rearrange("p c (d h) -> p c d h", d=2), AF.Sigmoid)
            tsc(kb, "dve", nbe_tm, nbe_tm[:, :, :, :], be_tm, be_tm[:, :, :, :], -1.0, None, ALU.mult)
            ps2 = kb.psum(s2, "ps_b", [128, 2, NCH * 8], F32)
            mm(kb, ps2, ps2[:, 0, :], C.triF, C.triF[:, :], g_tm, g_tm[:, 0, :, :].rearrange("p c h -> p (c h)"))
            mm(kb, ps2, ps2[:, 1, :], C.triB, C.triB[:, :], g_tm, g_tm[:, 1, :, :].rearrange("p c h -> p (c h)"))
            cp(kb, "dve", b_tm, b_tm[:, :, :, :].rearrange("p d c h -> p d (c h)"), ps2, ps2[:, :, :])
            kb.barrier()
        for hg in range(2):
            gdn_group(kb, C, io, l, b, hg, load_hT, og_scr, cwT, g_tm, (be_tm, nbe_tm), b_tm, dbg)
        kb.barrier()


def gdn_group(kb, C, io, l, b, hg, load_hT, og_scr, cwT, g_tm, be_tm, b_tm, dbg):
    w_in = io.w_in
    h0 = hg * 4
    with contextlib.ExitStack() as st:
        qT = kb.sbuf(st, "qT", [128, 4, SEQ], BF16)
        kT = kb.sbuf(st, "kT", [128, 4, SEQ], BF16)
        ktok = kb.sbuf(st, "ktok", [128, NCH, 4, 128], BF16)
        vtok = kb.sbuf(st, "vtok", [128, NCH, 4, 128], BF16)
        with contextlib.ExitStack() as s2:
            hT_b = load_hT(s2)
            wts = Rot([kb.sbuf(s2, "wqkv%d" % i, [128, 8, 128], BF16) for i in range(3)])
            raw = Rot([kb.sbuf(s2, "raw%d" % i, [128, SEQ + 4], F32) for i in range(3)])
            acc = Rot([kb.sbuf(s2, "acc%d" % i, [128, SEQ], F32) for i in range(3)])
            vT = kb.sbuf(s2, "vT", [128, SEQ], BF16)
            sq = kb.sbuf(s2, "sq", [128, SEQ], F32)
            rs = kb.sbuf(s2, "rs", [128, 512], F32)
            pss = Rot([kb.psum(s2, "ps_p%d" % i, [128, 512], F32) for i in range(4)])
            psn = Rot([kb.psum(s2, "ps_n%d" % i, [128, 512], F32) for i in range(2)])
            pst = Rot([kb.psum(s2, "ps_t%d" % i, [128, 8, 128], BF16) for i in range(2)])
            def item(which, off, hh):
                h = h0 + hh
                ct = (off // 128) + h
                wt = wts.next()
                load_w_fm(kb, w_in, w_in[l, :, off + h * 128: off + (h + 1) * 128], wt)
                rw = raw.next()
                memset(kb, "pool", rw, rw[:, 0:2], 0.0)
                memset(kb, "pool", rw, rw[:, SEQ + 2:SEQ + 4], 0.0)
                for n in range(4):
                    ps = pss.next()
                    for k in range(8):
                        mm(kb, ps, ps[:, :], wt, wt[:, k, :], hT_b, hT_b[:, k, n * 512:(n + 1) * 512],
                           start=(k == 0), stop=(k == 7))
                    cp(kb, "act", rw, rw[:, 2 + n * 512: 2 + (n + 1) * 512], ps, ps[:, :])
                ac = acc.next()
                en = "dve"
                tsc(kb, en, ac, ac[:, :], rw, rw[:, 0:SEQ], cwT[:, ct, 0:1], None, ALU.mult, extra_r=[cwT])
                for j in range(1, 5):
                    stt(kb, en, ac, ac[:, :], rw, rw[:, j:j + SEQ], cwT[:, ct, j:j + 1], ac, ac[:, :],
                        ALU.mult, ALU.add, extra_r=[cwT])
                yield
                if which == "v":
                    act(kb, vT, vT[:, :], ac, ac[:, :], AF.Silu)
                    for half in range(2):
                        pt = pst.next()
                        for ci in range(8):
                            cc = half * 8 + ci
                            tr(kb, pt, pt[:, ci, :], vT, vT[:, cc * 128:(cc + 1) * 128], C.id_b, C.id_b[:, :], inc=(ci == 7))
                        cp(kb, "act", vtok, vtok[:, half * 8:(half + 1) * 8, hh, :], pt, pt[:, :, :])
                else:
                    act(kb, ac, ac[:, :], ac, ac[:, :], AF.Silu)
                    tt(kb, "pool", sq, sq[:, :], ac, ac[:, :], ac, ac[:, :], ALU.mult)
                    dst = qT if which == "q" else kT
                    for n in range(4):
                        pn = psn.next()
                        mm(kb, pn, pn[:, :], C.ones_f, C.ones_f[:, :], sq, sq[:, n * 512:(n + 1) * 512])
                        cp(kb, "act", rs, rs[:, :], pn, pn[:, :])
                        rsqrt(kb, C, rs, rs[:, :], rs, rs[:, :], RMS_EPS)
                        if which == "q":
                            stt(kb, "dve", dst, dst[:, hh, n * 512:(n + 1) * 512], ac, ac[:, n * 512:(n + 1) * 512],
                                float(128 ** -0.5), rs, rs[:, :], ALU.mult, ALU.mult)
                        else:
                            tt(kb, "dve", dst, dst[:, hh, n * 512:(n + 1) * 512], ac, ac[:, n * 512:(n + 1) * 512],
                               rs, rs[:, :], ALU.mult)
                    if which == "k":
                        for half in range(2):
                            pt = pst.next()
                            for ci in range(8):
                                cc = half * 8 + ci
                                tr(kb, pt, pt[:, ci, :], kT, kT[:, hh, cc * 128:(cc + 1) * 128], C.id_b, C.id_b[:, :], inc=(ci == 7))
                            cp(kb, "act", ktok, ktok[:, half * 8:(half + 1) * 8, hh, :], pt, pt[:, :, :])

            gens = [item(which, off, hh) for which, off in (("q", OFF_Q), ("k", OFF_K), ("v", OFF_V)) for hh in range(4)]
            next(gens[0])
            for gi in range(len(gens)):
                if gi + 1 < len(gens):
                    next(gens[gi + 1])
                for _ in gens[gi]:
                    pass
            kb.barrier()
        oT = kb.sbuf(st, "oT", [128, 4, SEQ], F32)
        if dbg.get("skip_scan"):
            memset(kb, "pool", oT, oT[:, :, :], 1.0)
            if "dump" in dbg and hg == 0:
                o = dbg["dump"]
                for j, src in enumerate((qT, kT)):
                    for hh in range(4):
                        cp(kb, "dve", oT, oT[:, hh, :], src, src[:, hh, :])
                    dma(kb, "sp", o, o[j, :, :, :], oT, oT[:, :, :])
                for j, src in enumerate((ktok, vtok)):
                    cp(kb, "dve", oT, oT[:, :, :].rearrange("p a (b c) -> p b a c", b=NCH), src, src[:, :, :, :])
                    dma(kb, "sp", o, o[2 + j, :, :, :], oT, oT[:, :, :])
                memset(kb, "pool", oT, oT[:, :, :], 1.0)
        else:
            if "scan_stop" in dbg:
                memset(kb, "pool", oT, oT[:, :, :], 1.0)
            gdn_scan(kb, C, b, h0, qT, kT, ktok, vtok, oT, g_tm, be_tm, b_tm, dbg)
        with contextlib.ExitStack() as s2:
            hT_b = load_hT(s2)
            ogb = Rot([kb.sbuf(s2, "ogb%d" % i, [128, SEQ], BF16) for i in range(2)])
            nw = kb.sbuf(s2, "gnw", [128, 1], F32)
            dma(kb, "sp", nw, nw[:, :], io.gdn_norm_w, io.gdn_norm_w[l, :, :])
            wts = Rot([kb.sbuf(s2, "wz%d" % i, [128, 8, 128], BF16) for i in range(2)])
            pss = Rot([kb.psum(s2, "ps_z%d" % i, [128, 512], F32) for i in range(2)])
            psn = Rot([kb.psum(s2, "ps_zn%d" % i, [128, 512], F32) for i in range(2)])
            sq = kb.sbuf(s2, "sqo", [128, 512], F32)
            rs = kb.sbuf(s2, "rso", [128, 512], F32)
            zs = kb.sbuf(s2, "zs", [128, 512], F32)
            for hh in range(4):
                h = h0 + hh
                wt = wts.next()
                og_ = ogb.next()
                load_w_fm(kb, w_in, w_in[l, :, OFF_Z + h * 128: OFF_Z + (h + 1) * 128], wt)
                for n in range(4):
                    sl = slice(n * 512, (n + 1) * 512)
                    ps = pss.next()
                    for k in range(8):
                        mm(kb, ps, ps[:, :], wt, wt[:, k, :], hT_b, hT_b[:, k, sl], start=(k == 0), stop=(k == 7))
                    act(kb, zs, zs[:, :], ps, ps[:, :], AF.Silu)
                    tt(kb, "pool", sq, sq[:, :], oT, oT[:, hh, sl], oT, oT[:, hh, sl], ALU.mult)
                    pn = psn.next()
                    mm(kb, pn, pn[:, :], C.ones_f, C.ones_f[:, :], sq, sq[:, :])
                    tsc(kb, "dve", rs, rs[:, :], pn, pn[:, :], 1.0 / 128.0, None, ALU.mult)
                    rsqrt(kb, C, rs, rs[:, :], rs, rs[:, :], RMS_EPS)
                    tt(kb, "dve", rs, rs[:, :], rs, rs[:, :], zs, zs[:, :], ALU.mult)
                    stt(kb, "dve", og_, og_[:, sl], oT, oT[:, hh, sl], nw[:, 0:1], rs, rs[:, :], ALU.mult, ALU.mult,
                        extra_r=[nw])
                dma(kb, "sp", og_scr, og_scr[b, :, h, :], og_, og_[:, :])
            kb.barrier()


def gdn_scan(kb, C, b, h0, qT, kT, ktok, vtok, oT, g_tm, be_pair, b_tm, dbg):
    be_tm, nbe_tm = be_pair
    kb.serialize = (dbg.get("serial") == 1)
    kb.chain = (dbg.get("serial") in (3, 4, 5))
    kb.chain_engines = {3: ("pe", "act", "dve", "pool"), 4: ("act", "dve", "pool"), 5: ("pe", "dve", "pool")}.get(dbg.get("serial"), ())
    kb.last_op = None
    kb.psum_rar = bool(dbg.get("psum_rar", 1))
    phase_bar = (lambda: kb.barrier()) if dbg.get("serial") == 2 else (lambda: None)
    with contextlib.ExitStack() as st:
        psA = Rot([kb.psum(st, "ps_s%d" % i, [128, 4, 128], F32) for i in range(7)])
        psT = kb.psum(st, "ps_sT", [128, 8, 128], BF16)
        S = kb.sbuf(st, "S", [128, 8, 128], F32)
        Sb = kb.sbuf(st, "Sb", [128, 8, 128], BF16)
        memset(kb, "dve", S, S[:, :, :], 0.0)
        memset(kb, "dve", Sb, Sb[:, :, :], 0.0)
        R2 = lambda nm, shape, dt, n=2: Rot([kb.sbuf(st, "%s%d" % (nm, i), shape, dt) for i in range(n)])
        diag = R2("diag", [128, 4, 128], F32)
        E = R2("E", [128, 4, 128], F32)
        diff = R2("diff", [128, 4, 128], F32)
        sel = R2("sel", [128, 4, 128], F32, 4)
        GT = R2("GT", [128, 4, 128], F32)
        G = R2("G", [128, 4, 128], F32)
        PT = R2("PT", [128, 4, 128], BF16)
        tmpf = R2("tmpf", [128, 4, 128], F32, 3)
        p_r = R2("p", [128, 8, 128], BF16)
        pT_r = R2("pT", [128, 8, 128], BF16)
        tTf = kb.sbuf(st, "tTf", [128, 8, 128], F32)
        tTb = R2("tTb", [128, 8, 128], BF16)
        kbT = R2("kbT", [128, 4, 128], BF16)
        qdT = R2("qdT", [128, 4, 128], BF16)
        Rb = R2("Rb", [128, 4, 128], BF16)
        Ub = R2("Ub", [128, 4, 128], BF16)
        Kd = R2("Kd", [128, 4, 128], BF16)
        wcol = R2("wcol", [128, 4], F32)
        for i in range(dbg.get("nsteps", NCH)):
            chunk = (i, NCH - 1 - i)
            Ed, dfd, PTd = [None, None], [None, None], [None, None]
            p = p_r.next()
            pT = pT_r.next()
            def prep(d):
                c = chunk[d]
                cs = slice(c * 128, (c + 1) * 128)
                bcol = b_tm[:, d, c, h0:h0 + 4]
                kk = psA.next()
                qk = psA.next()
                for hh in range(4):
                    mm(kb, kk, kk[:, hh, :], kT, kT[:, hh, cs], kT, kT[:, hh, cs], inc=(hh == 3))
                for hh in range(4):
                    mm(kb, qk, qk[:, hh, :], kT, kT[:, hh, cs], qT, qT[:, hh, cs], inc=(hh == 3))
                yield
                dg = diag.next()
                tt(kb, "dve", dg, dg[:, :, :], C.id4_f, C.id4_f[:, :, :], b_tm, bcol.unsqueeze(2).to_broadcast([128, 4, 128]), ALU.mult)
                yield
                Db = psA.next()
                mm(kb, Db, Db[:, :, :], C.ones_f, C.ones_f[:, :], dg, dg[:, :, :])
                yield
                Ed[d] = E.next()
                act(kb, Ed[d], Ed[d][:, :, :], Db, Db[:, :, :], AF.Exp)
                yield
                dfd[d] = diff.next()
                tt(kb, "dve", dfd[d], dfd[d][:, :, :], Db, Db[:, :, :], b_tm, bcol.unsqueeze(2).to_broadcast([128, 4, 128]), ALU.subtract)
                s1 = sel.next()
                tt(kb, "dve", s1, s1[:, :, :], dfd[d], dfd[d][:, :, :], C.mT[d], C.mT[d][:, :, :], ALU.add)
                s2 = sel.next()
                tt(kb, "dve", s2, s2[:, :, :], dfd[d], dfd[d][:, :, :], C.mS[d], C.mS[d][:, :, :], ALU.add)
                yield
                gt = GT.next()
                act(kb, gt, gt[:, :, :], s1, s1[:, :, :], AF.Exp)
                g = G.next()
                act(kb, g, g[:, :, :], s2, s2[:, :, :], AF.Exp, scale=-1.0)
                yield
                PTd[d] = PT.next()
                tt(kb, "dve", PTd[d], PTd[d][:, :, :], qk, qk[:, :, :], gt, gt[:, :, :], ALU.mult)
                tf = tmpf.next()
                tt(kb, "dve", tf, tf[:, :, :], kk, kk[:, :, :], g, g[:, :, :], ALU.mult)
                nbecol = nbe_tm[:, d, c, h0:h0 + 4]
                tt(kb, "dve", p, p[:, d * 4:(d + 1) * 4, :], tf, tf[:, :, :], nbe_tm,
                   nbecol.unsqueeze(2).to_broadcast([128, 4, 128]), ALU.mult)
                yield
            for _ in zip(prep(0), prep(1)):
                pass
            if dbg.get("scan_stop", 9) <= 1:
                continue
            phase_bar()
            for j in range(8):
                tr(kb, psT, psT[:, j, :], p, p[:, j, :], C.id_b, C.id_b[:, :], inc=(j == 7))
            cp(kb, "act", pT, pT[:, :, :], psT, psT[:, :, :])
            tt(kb, "dve", tTf, tTf[:, :, :], psT, psT[:, :, :], C.id_f, C.id_f[:, None, :].to_broadcast([128, 8, 128]), ALU.add)
            tb = tTb.next()
            cp(kb, "act", tb, tb[:, :, :], tTf, tTf[:, :, :])
            if dbg.get("scan_stop", 9) <= 2:
                continue
            phase_bar()
            for it in range(6):
                pn = p_r.next()
                pa = [psA.next(), psA.next()]
                for j in range(8):
                    mm(kb, pa[j // 4], pa[j // 4][:, j % 4, :], pT, pT[:, j, :], p, p[:, j, :], inc=(j % 4 == 3))
                if it < 5:
                    pTn = pT_r.next()
                    pb = [psA.next(), psA.next()]
                    for j in range(8):
                        mm(kb, pb[j // 4], pb[j // 4][:, j % 4, :], p, p[:, j, :], pT, pT[:, j, :], inc=(j % 4 == 3))
                for hf in range(2):
                    cp(kb, "act", pn, pn[:, hf * 4:(hf + 1) * 4, :], pa[hf], pa[hf][:, :, :])
                if it < 5:
                    for hf in range(2):
                        cp(kb, "dve", pTn, pTn[:, hf * 4:(hf + 1) * 4, :], pb[hf], pb[hf][:, :, :])
                phase_bar()
                pu = [psA.next(), psA.next()]
                for j in range(8):
                    mm(kb, pu[j // 4], pu[j // 4][:, j % 4, :], pn, pn[:, j, :], tb, tb[:, j, :], inc=(j % 4 == 3))
                for hf in range(2):
                    tt(kb, "dve", tTf, tTf[:, hf * 4:(hf + 1) * 4, :], tTf, tTf[:, hf * 4:(hf + 1) * 4, :],
                       pu[hf], pu[hf][:, :, :], ALU.add)
                tb = tTb.next()
                cp(kb, "act", tb, tb[:, :, :], tTf, tTf[:, :, :])
                phase_bar()
                p = pn
                if it < 5:
                    pT = pTn
            if dbg.get("scan_stop", 9) <= 3:
                continue
            def chain(d):
                c = chunk[d]
                cs = slice(c * 128, (c + 1) * 128)
                last = 127 if d == 0 else 0
                becol = be_tm[:, d, c, h0:h0 + 4]
                kb_ = kbT.next()
                tt(kb, "dve", kb_, kb_[:, :, :], kT, kT[:, :, cs], Ed[d], Ed[d][:, :, :], ALU.mult)
                qd_ = qdT.next()
                tt(kb, "dve", qd_, qd_[:, :, :], qT, qT[:, :, cs], Ed[d], Ed[d][:, :, :], ALU.mult)
                yield
                pr = psA.next()
                for hh in range(4):
                    mm(kb, pr, pr[:, hh, :], kb_, kb_[:, hh, :], Sb, Sb[:, d * 4 + hh, :], inc=(hh == 3))
                yield
                tf = tmpf.next()
                tt(kb, "dve", tf, tf[:, :, :], vtok, vtok[:, c, :, :], pr, pr[:, :, :], ALU.subtract)
                rb = Rb.next()
                tt(kb, "dve", rb, rb[:, :, :], tf, tf[:, :, :], be_tm, becol.unsqueeze(2).to_broadcast([128, 4, 128]), ALU.mult)
                yield
                pu = psA.next()
                for hh in range(4):
                    mm(kb, pu, pu[:, hh, :], tb, tb[:, d * 4 + hh, :], rb, rb[:, hh, :], inc=(hh == 3))
                yield
                ub = Ub.next()
                cp(kb, "act", ub, ub[:, :, :], pu, pu[:, :, :])
                yield
                po = psA.next()
                for hh in range(4):
                    mm(kb, po, po[:, hh, :], Sb, Sb[:, d * 4 + hh, :], qd_, qd_[:, hh, :], start=True, stop=False)
                    mm(kb, po, po[:, hh, :], ub, ub[:, hh, :], PTd[d], PTd[d][:, hh, :], start=False, stop=True, inc=(hh == 3))
                yield
                first = (d == 0) == (c < NCH // 2)
                if first:
                    cp(kb, "act", oT, oT[:, :, cs], po, po[:, :, :])
                else:
                    tt(kb, "dve", oT, oT[:, :, cs], oT, oT[:, :, cs], po, po[:, :, :], ALU.add)
                wc = wcol.next()
                act(kb, wc, wc[:, :], dfd[d], dfd[d][:, :, last], AF.Exp)
                kd = Kd.next()
                tt(kb, "dve", kd, kd[:, :, :], ktok, ktok[:, c, :, :], wc, wc[:, :].unsqueeze(2).to_broadcast([128, 4, 128]), ALU.mult)
                yield
                psn = psA.next()
                for hh in range(4):
                    mm(kb, psn, psn[:, hh, :], kd, kd[:, hh, :], ub, ub[:, hh, :], inc=(hh == 3))
                yield
                Sd = S[:, d * 4:(d + 1) * 4, :]
                tt(kb, "dve", S, Sd, S, Sd, Ed[d], Ed[d][:, :, last:last + 1].to_broadcast([128, 4, 128]), ALU.mult)
                tt(kb, "dve", S, Sd, S, Sd, psn, psn[:, :, :], ALU.add)
                cp(kb, "act", Sb, Sb[:, d * 4:(d + 1) * 4, :], S, Sd)
                yield
            for _ in zip(chain(0), chain(1)):
                pass
        kb.serialize = False
        kb.chain = False
        kb.barrier()


def stage_rope_tables(kb, C, io):
    for b in range(NSEQ):
        sl = slice(b * SEQ, (b + 1) * SEQ)
        with contextlib.ExitStack() as st:
            pi_ = kb.sbuf(st, "rp_pi", [64, SEQ], I32)
            dma(kb, "sp", pi_, pi_[:, :], io.positions, io.positions[0:1, sl].partition_broadcast(64))
            pf = kb.sbuf(st, "rp_pf", [64, SEQ], F32)
            cp(kb, "dve", pf, pf[:, :], pi_, pi_[:, :])
            idx_i = kb.sbuf(st, "rp_ii", [64, 1], I32)
            kb.op("pool", lambda e: e.iota(idx_i[:, :], pattern=[[0, 1]], base=0, channel_multiplier=1), r=[], w=[idx_i])
            idx = kb.sbuf(st, "rp_if", [64, 1], F32)
            cp(kb, "dve", idx, idx[:, :], idx_i, idx_i[:, :])
            tsc(kb, "dve", idx, idx[32:64, :], idx, idx[32:64, :], -32.0, None, ALU.add)
            invf = kb.sbuf(st, "rp_inv", [64, 1], F32)
            act(kb, invf, invf[:, :], idx, idx[:, :], AF.Exp, scale=float(-np.log(10000.0) / 32.0))
            ang = kb.sbuf(st, "rp_ang", [64, SEQ], F32)
            tsc(kb, "dve", ang, ang[:, :], pf, pf[:, :], invf[:, 0:1], None, ALU.mult, extra_r=[invf])
            res = kb.sbuf(st, "rp_res", [64, SEQ], F32)
            for which, shift in ((0, np.pi / 2.0), (1, 0.0)):
                with contextlib.ExitStack() as s2:
                    sin_reduced(kb, s2, "rp_c", res, res[:, :], ang, ang[:, :], shift, [64, SEQ])
                    if which == 1:
                        tsc(kb, "dve", res, res[0:32, :], res, res[0:32, :], -1.0, None, ALU.mult)
                    dma(kb, "sp", io.cs, io.cs[which, :, sl], res, res[:, :])
                    kb.barrier()
            kb.barrier()


def rope_apply(kb, wk, src_b, src_ap, cos_b, sin_b, dst_b, dst_ap, n):
    x, xs, t1 = wk["x"], wk["xs"], wk["t1"]
    cp(kb, "act", x, x[:, 0:n], src_b, src_ap)
    cp(kb, "dve", xs, xs[0:32, 0:n], x, x[32:64, 0:n])
    cp(kb, "dve", xs, xs[32:64, 0:n], x, x[0:32, 0:n])
    tt(kb, "dve", t1, t1[:, 0:n], x, x[:, 0:n], cos_b[0], cos_b[1], ALU.mult)
    tt(kb, "pool", xs, xs[:, 0:n], xs, xs[:, 0:n], sin_b[0], sin_b[1], ALU.mult)
    tt(kb, "dve", dst_b, dst_ap, t1, t1[:, 0:n], xs, xs[:, 0:n], ALU.add)


def mla_seq(kb, C, io, l, b, load_hT, omT_dst, dbg):
    w_in = io.w_in
    scale = float(192 ** -0.5)
    with contextlib.ExitStack() as st:
        cqn = kb.sbuf(st, "cqn", [128, 3, SEQ], BF16)
        ckvn = kb.sbuf(st, "ckvn", [128, 2, SEQ], BF16)
        krT = kb.sbuf(st, "krT", [64, SEQ], BF16)
        cosb = kb.sbuf(st, "cosb", [64, SEQ], F32)
        sinb = kb.sbuf(st, "sinb", [64, SEQ], F32)
        dma(kb, "sp", cosb, cosb[:, :], io.cs, io.cs[0, :, b * SEQ:(b + 1) * SEQ])
        dma(kb, "sp", sinb, sinb[:, :], io.cs, io.cs[1, :, b * SEQ:(b + 1) * SEQ])
        rwk = dict(x=kb.sbuf(st, "rp_x", [64, 512], F32), xs=kb.sbuf(st, "rp_xs", [64, 512], F32),
                   t1=kb.sbuf(st, "rp_t1", [64, 512], F32))
        with contextlib.ExitStack() as s2:
            hT_b = load_hT(s2)
            pss = Rot([kb.psum(s2, "ps_m%d" % i, [128, 512], F32) for i in range(3)])
            psn = Rot([kb.psum(s2, "ps_mn%d" % i, [128, 512], F32) for i in range(2)])
            for nm, off, nt_, dstn, wnorm in (("cq", OFF_CQ, 3, cqn, io.mla_q_norm_w), ("ckv", OFF_CKV, 2, ckvn, io.mla_kv_norm_w)):
                wt = kb.sbuf(s2, "w_" + nm, [128, 8, nt_ * 128], BF16)
                load_w_fm(kb, w_in, w_in[l, :, off:off + nt_ * 128], wt)
                nw = kb.sbuf(s2, "nw_" + nm, [128, nt_], F32)
                for t_ in range(nt_):
                    dma(kb, "sp", nw, nw[:, t_:t_ + 1], wnorm, wnorm[l, t_ * 128:(t_ + 1) * 128, :])
                raw = kb.sbuf(s2, "raw_" + nm, [128, nt_, 512], F32)
                sq = kb.sbuf(s2, "sq_" + nm, [128, nt_, 512], F32)
                rs = kb.sbuf(s2, "rs_" + nm, [128, 512], F32)
                for n in range(4):
                    sl = slice(n * 512, (n + 1) * 512)
                    for t_ in range(nt_):
                        ps = pss.next()
                        for k in range(8):
                            mm(kb, ps, ps[:, :], wt, wt[:, k, t_ * 128:(t_ + 1) * 128], hT_b, hT_b[:, k, sl],
                               start=(k == 0), stop=(k == 7))
                        cp(kb, "act", raw, raw[:, t_, :], ps, ps[:, :])
                    tt(kb, "pool", sq, sq[:, :, :], raw, raw[:, :, :], raw, raw[:, :, :], ALU.mult)
                    pn = psn.next()
                    for t_ in range(nt_):
                        mm(kb, pn, pn[:, :], C.ones_f, C.ones_f[:, :], sq, sq[:, t_, :], start=(t_ == 0), stop=(t_ == nt_ - 1))
                    tsc(kb, "dve", rs, rs[:, :], pn, pn[:, :], 1.0 / (nt_ * 128), None, ALU.mult)
                    rsqrt(kb, C, rs, rs[:, :], rs, rs[:, :], RMS_EPS)
                    for t_ in range(nt_):
                        stt(kb, "dve", dstn, dstn[:, t_, sl], raw, raw[:, t_, :], nw[:, t_:t_ + 1], rs, rs[:, :],
                            ALU.mult, ALU.mult, extra_r=[nw])
            wkr = kb.sbuf(s2, "w_kr", [128, 8, 64], BF16)
            load_w_fm(kb, w_in, w_in[l, :, OFF_KR:OFF_KR + 64], wkr)
            for n in range(4):
                sl = slice(n * 512, (n + 1) * 512)
                ps = pss.next()
                for k in range(8):
                    mm(kb, ps, ps[0:64, :], wkr, wkr[:, k, :], hT_b, hT_b[:, k, sl], start=(k == 0), stop=(k == 7))
                rope_apply(kb, rwk, ps, ps[0:64, :], (cosb, cosb[:, sl]), (sinb, sinb[:, sl]), krT, krT[:, sl], 512)
            kb.barrier()
        with contextlib.ExitStack() as s2:
            wq = Rot([kb.sbuf(s2, "w_uq%d" % i, [128, 3, 192], BF16) for i in range(2)])
            wkv = Rot([kb.sbuf(s2, "w_ukv%d" % i, [128, 2, 256], BF16) for i in range(2)])
            qnT = Rot([kb.sbuf(s2, "qnT%d" % i, [128, SEQ], BF16) for i in range(2)])
            qrT = Rot([kb.sbuf(s2, "qrT%d" % i, [64, SEQ], BF16) for i in range(2)])
            knT = Rot([kb.sbuf(s2, "knT%d" % i, [128, SEQ], BF16) for i in range(2)])
            vtk = Rot([kb.sbuf(s2, "vtk%d" % i, [128, NCH, 128], BF16) for i in range(2)])
            pex = Rot([kb.sbuf(s2, "pex%d" % i, [128, 512], BF16) for i in range(3)])
            oh = Rot([kb.sbuf(s2, "oh%d" % i, [128, SEQ], BF16) for i in range(2)])
            rden = kb.sbuf(s2, "rden", [128, 512], F32)
            pss = Rot([kb.psum(s2, "ps_a%d" % i, [128, 512], F32) for i in range(3)])
            pso = Rot([kb.psum(s2, "ps_o%d" % i, [128, 512], F32) for i in range(2)])
            psd = Rot([kb.psum(s2, "ps_d%d" % i, [128, 512], F32) for i in range(2)])
            psv = kb.psum(s2, "ps_v", [128, 4, 128], F32)
            for h in range(H):
                wq_ = wq.next()
                load_w_fm(kb, io.w_uq, io.w_uq[l, :, h * 192:(h + 1) * 192], wq_)
                wkv_ = wkv.next()
                load_w_fm(kb, io.w_ukv, io.w_ukv[l, :, h * 256:(h + 1) * 256], wkv_)
                qn, qr, kn, vt, o_ = qnT.next(), qrT.next(), knT.next(), vtk.next(), oh.next()
                for n in range(4):
                    sl = slice(n * 512, (n + 1) * 512)
                    ps = pss.next()
                    for k in range(3):
                        mm(kb, ps, ps[:, :], wq_, wq_[:, k, 0:128], cqn, cqn[:, k, sl], start=(k == 0), stop=(k == 2))
                    cp(kb, "act", qn, qn[:, sl], ps, ps[:, :])
                    ps = pss.next()
                    for k in range(3):
                        mm(kb, ps, ps[0:64, :], wq_, wq_[:, k, 128:192], cqn, cqn[:, k, sl], start=(k == 0), stop=(k == 2))
                    rope_apply(kb, rwk, ps, ps[0:64, :], (cosb, cosb[:, sl]), (sinb, sinb[:, sl]), qr, qr[:, sl], 512)
                    ps = pss.next()
                    for k in range(2):
                        mm(kb, ps, ps[:, :], wkv_, wkv_[:, k, 0:128], ckvn, ckvn[:, k, sl], start=(k == 0), stop=(k == 1))
                    cp(kb, "dve", kn, kn[:, sl], ps, ps[:, :])
                    for t4 in range(4):
                        tkn = n * 4 + t4
                        for k in range(2):
                            mm(kb, psv, psv[:, t4, :], ckvn, ckvn[:, k, tkn * 128:(tkn + 1) * 128], wkv_, wkv_[:, k, 128:256],
                               start=(k == 0), stop=(k == 1))
                    cp(kb, "act", vt, vt[:, n * 4:(n + 1) * 4, :], psv, psv[:, :, :])
                for qb in range(4):
                    qs = slice(qb * 512, (qb + 1) * 512)
                    po = pso.next()
                    pd = psd.next()
                    prev = None
                    for kt in range(NCH + 1):
                        cur = None
                        if kt < NCH:
                            ks = slice(kt * 128, (kt + 1) * 128)
                            ps = pss.next()
                            mm(kb, ps, ps[:, :], kn, kn[:, ks], qn, qn[:, qs], start=True, stop=False)
                            mm(kb, ps, ps[:, :], krT, krT[:, ks], qr, qr[:, qs], start=False, stop=True)
                            cur = pex.next()
                            act(kb, cur, cur[:, :], ps, ps[:, :], AF.Exp, scale=scale)
                        if prev is not None:
                            k0 = kt - 1
                            mm(kb, po, po[:, :], vt, vt[:, k0, :], prev, prev[:, :], start=(k0 == 0), stop=(k0 == NCH - 1))
                            mm(kb, pd, pd[:, :], C.ones_b, C.ones_b[:, :], prev, prev[:, :], start=(k0 == 0), stop=(k0 == NCH - 1))
                        prev = cur
                    kb.op("dve", lambda e, pd=pd: e.reciprocal(rden[:, :], pd[:, :]), r=[pd], w=[rden])
                    tt(kb, "dve", o_, o_[:, qs], po, po[:, :], rden, rden[:, :], ALU.mult)
                db, dap = omT_dst(h)
                dma(kb, "sp", db, dap, o_, o_[:, :])
            kb.barrier()


def mixer_out(kb, C, io, l, b, load_hT, og_scr, om_scr, dbg):
    with contextlib.ExitStack() as st:
        yT = kb.sbuf(st, "yT", [128, 8, SEQ], BF16)
        with contextlib.ExitStack() as s2:
            hT_b = load_hT(s2)
            ogT = kb.sbuf(s2, "ogT", [128, 8, SEQ], BF16)
            omT = kb.sbuf(s2, "omT", [128, 8, SEQ], BF16)
            dma(kb, "sp", ogT, ogT[:, :, :], og_scr, og_scr[b, :, :, :])
            dma(kb, "sp", omT, omT[:, :, :], om_scr, om_scr[b, :, :, :])
            wr = [Rot([kb.sbuf(s2, "wo%d_%d" % (j, i), [128, 8, 128], BF16) for i in range(2)]) for j in range(4)]
            ps = [Rot([kb.psum(s2, "ps_y%d_%d" % (j, i), [128, 512], F32) for i in range(2)]) for j in range(4)]
            sg = Rot([kb.sbuf(s2, "sg%d" % i, [128, 512], F32) for i in range(4)])
            tq = Rot([kb.sbuf(s2, "tq%d" % i, [128, 512], F32) for i in range(4)])
            for m in range(8):
                ms = slice(m * 128, (m + 1) * 128)
                w4 = [r_.next() for r_ in wr]
                load_w_fm(kb, io.w_o_gdn, io.w_o_gdn[l, :, ms], w4[0])
                load_w_fm(kb, io.w_o_mla, io.w_o_mla[l, :, ms], w4[1])
                load_w_fm(kb, io.w_in, io.w_in[l, :, OFF_G + m * 128:OFF_G + (m + 1) * 128], w4[2])
                load_w_fm(kb, io.w_in, io.w_in[l, :, OFF_G + 1024 + m * 128:OFF_G + 1024 + (m + 1) * 128], w4[3])
                for n in range(4):
                    sl = slice(n * 512, (n + 1) * 512)
                    p4 = [r_.next() for r_ in ps]
                    for j, src in enumerate((ogT, omT, hT_b, hT_b)):
                        for k in range(8):
                            mm(kb, p4[j], p4[j][:, :], w4[j], w4[j][:, k, :], src, src[:, k, sl], start=(k == 0), stop=(k == 7))
                    s1, s2_ = sg.next(), sg.next()
                    act(kb, s1, s1[:, :], p4[2], p4[2][:, :], AF.Sigmoid)
                    act(kb, s2_, s2_[:, :], p4[3], p4[3][:, :], AF.Sigmoid)
                    t1, t2 = tq.next(), tq.next()
                    tt(kb, "dve", t1, t1[:, :], p4[0], p4[0][:, :], s1, s1[:, :], ALU.mult)
                    tt(kb, "dve", t2, t2[:, :], p4[1], p4[1][:, :], s2_, s2_[:, :], ALU.mult)
                    tt(kb, "pool", yT, yT[:, m, sl], t1, t1[:, :], t2, t2[:, :], ALU.add)
            kb.barrier()
        with contextlib.ExitStack() as s2:
            wo = kb.sbuf(s2, "w_out", [128, 8, D], BF16)
            load_w_fm(kb, io.w_out, io.w_out[l, :, :], wo)
            mT = kb.sbuf(s2, "mT", [128, 8, 512], F32)
            psm = Rot([kb.psum(s2, "ps_mo%d" % i, [128, 512], F32) for i in range(2)])
            pst = Rot([kb.psum(s2, "ps_mt%d" % i, [128, 4, 128], F32) for i in range(4)])
            hold = Rot([kb.sbuf(s2, "hold%d" % i, [128, D], F32) for i in range(2)])
            state = {"n": -1}

            def src(i, buf):
                ti = i - b * NCH
                n, t4 = ti // 4, ti % 4
                if n != state["n"]:
                    state["n"] = n
                    sl = slice(n * 512, (n + 1) * 512)
                    for m in range(8):
                        pm = psm.next()
                        for k in range(8):
                            mm(kb, pm, pm[:, :], wo, wo[:, k, m * 128:(m + 1) * 128], yT, yT[:, k, sl], start=(k == 0), stop=(k == 7))
                        cp(kb, "act", mT, mT[:, m, :], pm, pm[:, :])
                ho = hold.next()
                dma(kb, "sp", ho, ho[:, :], io.h_res[i], io.h_res[i][i * 128:(i + 1) * 128, :])
                for half in range(2):
                    pt = pst.next()
                    for mm_ in range(4):
                        m = half * 4 + mm_
                        tr(kb, pt, pt[:, mm_, :], mT, mT[:, m, t4 * 128:(t4 + 1) * 128], C.id_f, C.id_f[:, :])
                    stt(kb, "dve", buf, buf[:, half * 512:(half + 1) * 512], ho, ho[:, half * 512:(half + 1) * 512], float(DN_ALPHA),
                        pt, pt[:, :, :].rearrange("p a b -> p (a b)"), ALU.mult, ALU.add)
            emit_ln_rows(kb, C, s2, "l1_", src, (io.ln1_g, io.ln1_g[l:l + 1, :]), (io.ln1_b, io.ln1_b[l:l + 1, :]), io,
                         list(range(b * NCH, (b + 1) * NCH)), io.h_mid)
            kb.barrier()


def stage_mixer(kb, C, io, l, seqs, dbg, parts=("gdn", "mla", "out")):
    og_scr, om_scr = io.og_scr, io.om_scr
    with contextlib.ExitStack() as st0:
        cwT = kb.sbuf(st0, "cwT", [128, 24, 5], F32)
        with contextlib.ExitStack() as s2:
            cwr = kb.sbuf(s2, "cwr", [5, 3072], F32)
            dma(kb, "sp", cwr, cwr[:, :], io.conv_w, io.conv_w[l, :, :])
            pc = kb.psum(s2, "ps_cw", [128, 24, 8], F32)
            for t_ in range(24):
                tr(kb, pc, pc[:, t_, 0:5], cwr, cwr[0:5, t_ * 128:(t_ + 1) * 128], C.id_f, C.id_f[0:5, 0:5])
            cp(kb, "dve", cwT, cwT[:, :, :], pc, pc[:, :, 0:5])
            kb.barrier()
        for b in seqs:
            def load_hT(stk, b=b):
                hT_b = kb.sbuf(stk, "hT_b", [128, 8, SEQ], BF16)
                kb.dma("sp", lambda e: e.dma_start(out=hT_b[:, :, :], in_=io.hT[0][:, :, b * SEQ:(b + 1) * SEQ]),
                       r=[io.hT[i] for i in range(b * NCH, (b + 1) * NCH)], w=[hT_b])
                return hT_b
            if "gdn" in parts:
                gdn_seq(kb, C, io, l, b, load_hT, og_scr, cwT, dbg)
            if "mla" in parts:
                mla_seq(kb, C, io, l, b, load_hT, lambda h, b=b: (om_scr, om_scr[b, :, h, :]), dbg)
            if "out" in parts:
                mixer_out(kb, C, io, l, b, load_hT, og_scr, om_scr, dbg)


def idma(kb, ob, oap, out_off, ib, iap, in_off, extra_r=(), nrows=None):
    if not hasattr(kb, "bc_regs"):
        kb.bc_regs = {}
    if nrows not in kb.bc_regs:
        kb.bc_regs[nrows] = kb.nc.gpsimd.to_reg(nrows - 1)
    bc = kb.bc_regs[nrows]

    def fn(e):
        return e.indirect_dma_start(
            out=oap, out_offset=(bass.IndirectOffsetOnAxis(ap=out_off, axis=0) if out_off is not None else None),
            in_=iap, in_offset=(bass.IndirectOffsetOnAxis(ap=in_off, axis=0) if in_off is not None else None),
            bounds_check=bc, oob_is_err=False)
    return kb.dma("pool", fn, r=[ib] + list(extra_r), w=[ob])


def stage_moe(kb, C, io, l, dst_tiles, dbg):
    NEG = -1.0e30
    with contextlib.ExitStack() as st:
        ohE = kb.sbuf(st, "ohE", [128, NT, 2, NE], F32)
        gate = kb.sbuf(st, "gate", [128, NT, 2], F32)
        destI = kb.sbuf(st, "destI", [128, NT, 2], I32)
        BEi = kb.sbuf(st, "BEi", [128, NBLK], I32)
        offs_gu = kb.sbuf(st, "offs_gu", [128, NBLK, 8], I32)
        offs_d = kb.sbuf(st, "offs_d", [128, NBLK, 4], I32)
        with contextlib.ExitStack() as s2:
            kb.serialize = bool(dbg.get("serial_moe", False))
            wr = kb.sbuf(s2, "wr", [128, 8, 72], F32)
            dma(kb, "sp", wr, wr[:, :, 0:8], io.w_router_group, io.w_router_group[l, :, :].rearrange("(k p) g -> p k g", p=128))
            dma(kb, "sp", wr, wr[:, :, 8:72], io.w_router_expert, io.w_router_expert[l, :, :].rearrange("(k p) g -> p k g", p=128))
            br = kb.sbuf(s2, "br", [128, 72], F32)
            dma(kb, "sp", br, br[:, 0:8], io.b_router_group, io.b_router_group[l:l + 1, :].partition_broadcast(128))
            dma(kb, "sp", br, br[:, 8:72], io.b_router_expert, io.b_router_expert[l:l + 1, :].partition_broadcast(128))
            xt = Rot([kb.sbuf(s2, "mx%d" % i, [128, D], F32) for i in range(2)])
            hTf = kb.sbuf(s2, "hTf", [128, 8, 128], F32)
            pst = Rot([kb.psum(s2, "ps_rt%d" % i, [128, 4, 128], F32) for i in range(2)])
            psl = kb.psum(s2, "ps_rl", [128, 72], F32)
            lgall = kb.sbuf(s2, "lgall", [128, NT, 72], F32)
            for i in range(NT):
                x = xt.next()
                dma(kb, "sp", x, x[:, :], io.h_mid[i], io.h_mid[i][i * 128:(i + 1) * 128, :])
                for half in range(2):
                    pt = pst.next()
                    for c4 in range(4):
                        c = half * 4 + c4
                        tr(kb, pt, pt[:, c4, :], x, x[:, c * 128:(c + 1) * 128], C.id_f, C.id_f[:, :], inc=(c4 == 3))
                    cp(kb, "act", hTf, hTf[:, half * 4:(half + 1) * 4, :], pt, pt[:, :, :])
                for k in range(8):
                    mm(kb, psl, psl[:, :], hTf, hTf[:, k, :], wr, wr[:, k, :], start=(k == 0), stop=(k == 7))
                tt(kb, "dve", lgall, lgall[:, i, :], psl, psl[:, :], br, br[:, :], ALU.add)
            A3 = lambda nm, n: kb.sbuf(s2, "rb_" + nm, [128, NT, n], F32)
            A2 = lambda nm: kb.sbuf(s2, "rb_" + nm, [128, NT], F32)
            LG = lgall[:, :, 0:8]
            LE4 = lgall[:, :, 8:72].rearrange("p t (g e) -> p t g e", g=8)
            bc8 = lambda buf: buf[:, :].unsqueeze(2).to_broadcast([128, NT, 8])
            gmax, se, pg, m1, m2, r_, dd = A2("gmax"), A2("se"), A2("pg"), A2("m1"), A2("m2"), A2("r"), A2("dd")
            ohg, eg, les, oh1, le2, oh2 = A3("ohg", 8), A3("eg", 8), A3("les", 8), A3("oh1", 8), A3("le2", 8), A3("oh2", 8)
            t64 = A3("t64", 64)
            t64v = t64[:, :, :].rearrange("p t (g e) -> p t g e", g=8)
            kb.op("dve", lambda e: e.reduce_max(out=gmax[:, :], in_=LG, axis=AX.X), r=[lgall], w=[gmax])
            tt(kb, "dve", ohg, ohg[:, :, :], lgall, LG, gmax, bc8(gmax), ALU.is_equal)
            tt(kb, "dve", eg, eg[:, :, :], lgall, LG, gmax, bc8(gmax), ALU.subtract)
            act(kb, eg, eg[:, :, :], eg, eg[:, :, :], AF.Exp)
            kb.op("dve", lambda e: e.reduce_sum(out=se[:, :], in_=eg[:, :, :], axis=AX.X), r=[eg], w=[se])
            kb.op("dve", lambda e: e.reciprocal(pg[:, :], se[:, :]), r=[se], w=[pg])
            tt(kb, "dve", t64, t64v, lgall, LE4, ohg, ohg[:, :, :].unsqueeze(3).to_broadcast([128, NT, 8, 8]), ALU.mult)
            kb.op("dve", lambda e: e.reduce_sum(out=les[:, :, :], in_=t64[:, :, :].rearrange("p t (g e) -> p t e g", g=8), axis=AX.X),
                  r=[t64], w=[les])
            kb.op("dve", lambda e: e.reduce_max(out=m1[:, :], in_=les[:, :, :], axis=AX.X), r=[les], w=[m1])
            tt(kb, "dve", oh1, oh1[:, :, :], les, les[:, :, :], m1, bc8(m1), ALU.is_equal)
            stt(kb, "dve", le2, le2[:, :, :], oh1, oh1[:, :, :], NEG, les, les[:, :, :], ALU.mult, ALU.add)
            kb.op("dve", lambda e: e.reduce_max(out=m2[:, :], in_=le2[:, :, :], axis=AX.X), r=[le2], w=[m2])
            tt(kb, "dve", oh2, oh2[:, :, :], le2, le2[:, :, :], m2, bc8(m2), ALU.is_equal)
            tt(kb, "dve", r_, r_[:, :], m2, m2[:, :], m1, m1[:, :], ALU.subtract)
            act(kb, r_, r_[:, :], r_, r_[:, :], AF.Exp)
            tsc(kb, "dve", dd, dd[:, :], r_, r_[:, :], 1.0, None, ALU.add)
            kb.op("dve", lambda e: e.reciprocal(dd[:, :], dd[:, :]), r=[dd], w=[dd])
            tt(kb, "dve", dd, dd[:, :], dd, dd[:, :], pg, pg[:, :], ALU.mult)
            cp(kb, "dve", gate, gate[:, :, 0], dd, dd[:, :])
            tt(kb, "dve", gate, gate[:, :, 1], dd, dd[:, :], r_, r_[:, :], ALU.mult)
            for k_, oh in ((0, oh1), (1, oh2)):
                tt(kb, "dve", ohE, ohE[:, :, k_, :].rearrange("p t (g e) -> p t g e", g=8), ohg,
                   ohg[:, :, :].unsqueeze(3).to_broadcast([128, NT, 8, 8]), oh, oh[:, :, :].unsqueeze(2).to_broadcast([128, NT, 8, 8]), ALU.mult)
            kb.barrier()
        with contextlib.ExitStack() as s2:
            ohs = kb.sbuf(s2, "ohs", [128, NT, NE], F32)
            cum = kb.sbuf(s2, "cum", [128, NT + 1, NE], F32)
            tt(kb, "dve", ohs, ohs[:, :, :], ohE, ohE[:, :, 0, :], ohE, ohE[:, :, 1, :], ALU.add)
            memset(kb, "dve", cum, cum[:, 0, :], 0.0)
            for i in range(NT):
                tt(kb, "dve", cum, cum[:, i + 1, :], cum, cum[:, i, :], ohs, ohs[:, i, :], ALU.add)
            striU = kb.sbuf(s2, "striU", [128, 128], F32)
            tt(kb, "dve", striU, striU[:, :], C.triF, C.triF[:, :], C.id_f, C.id_f[:, :], ALU.subtract)
            pc = kb.psum(s2, "ps_cnt", [64, 128], F32)
            for i in range(NT):
                mm(kb, pc, pc[:, :], ohs, ohs[:, i, :], C.ones_f, C.ones_f[:, :], start=(i == 0), stop=(i == NT - 1))
            cntT = kb.sbuf(s2, "cntT", [64, 128], F32)
            tsc(kb, "dve", cntT, cntT[:, :], pc, pc[:, :], 127.0, None, ALU.add)
            ci = kb.sbuf(s2, "cnt_i", [64, 128], I32)
            cp(kb, "dve", ci, ci[:, :], cntT, cntT[:, :])
            tsc(kb, "dve", ci, ci[:, :], ci, ci[:, :], 7, None, ALU.arith_shift_right)
            tsc(kb, "dve", ci, ci[:, :], ci, ci[:, :], 7, None, ALU.logical_shift_left)
            padT = kb.sbuf(s2, "padT", [64, 128], F32)
            cp(kb, "dve", padT, padT[:, :], ci, ci[:, :])
            pps = kb.psum(s2, "ps_pst", [128, 64], F32)
            mm(kb, pps, pps[:, :], padT, padT[:, :], striU, striU[0:64, 0:64])
            pstart = kb.sbuf(s2, "pstart", [128, NE], F32)
            cp(kb, "dve", pstart, pstart[:, :], pps, pps[:, :])
            ppe = kb.psum(s2, "ps_pend", [64, 128], F32)
            mm(kb, ppe, ppe[:, :], C.triF, C.triF[0:64, 0:64], padT, padT[:, :])
            jrow_i = kb.sbuf(s2, "jrow_i", [64, 128], I32)
            kb.op("pool", lambda e: e.iota(jrow_i[:, :], pattern=[[128, 128]], base=0, channel_multiplier=0), r=[], w=[jrow_i])
            jrow = kb.sbuf(s2, "jrow", [64, 128], F32)
            cp(kb, "dve", jrow, jrow[:, :], jrow_i, jrow_i[:, :])
            cmpm = kb.sbuf(s2, "cmpm", [64, 128], F32)
            tt(kb, "dve", cmpm, cmpm[:, :], ppe, ppe[:, :], jrow, jrow[:, :], ALU.is_le)
            pbe = kb.psum(s2, "ps_be", [128, 128], F32)
            mm(kb, pbe, pbe[:, :], C.ones_f, C.ones_f[0:64, :], cmpm, cmpm[:, :])
            bef = kb.sbuf(s2, "bef", [128, NBLK], F32)
            unused = kb.sbuf(s2, "unused", [128, NBLK], F32)
            tsc(kb, "dve", unused, unused[:, :], pbe, pbe[:, :], 63.5, 4194304.0, ALU.is_gt, ALU.mult)
            tsc(kb, "dve", bef, bef[:, :], pbe, pbe[:, :], 63.0, None, ALU.min)
            cp(kb, "dve", BEi, BEi[:, :], bef, bef[:, :])
            prow_i = kb.sbuf(s2, "prow_i", [128, 8], I32)
            kb.op("pool", lambda e: e.iota(prow_i[:, :], pattern=[[128, 8]], base=0, channel_multiplier=1), r=[], w=[prow_i])
            prow = kb.sbuf(s2, "prow", [128, 8], F32)
            cp(kb, "dve", prow, prow[:, :], prow_i, prow_i[:, :])
            of = kb.sbuf(s2, "off_f", [128, NBLK, 8], F32)
            stt(kb, "dve", of, of[:, :, :], bef, bef[:, :].unsqueeze(2).to_broadcast([128, NBLK, 8]), 128.0, prow,
                prow[:, 0:1].unsqueeze(1).to_broadcast([128, NBLK, 8]), ALU.mult, ALU.add)
            tsc(kb, "dve", of, of[:, :, :], of, of[:, :, :], float(l * NE * 128), None, ALU.add)
            tt(kb, "dve", of, of[:, :, :], of, of[:, :, :], unused, unused[:, :].unsqueeze(2).to_broadcast([128, NBLK, 8]), ALU.add)
            cp(kb, "dve", offs_gu, offs_gu[:, :, :], of, of[:, :, :])
            stt(kb, "dve", of, of[:, :, 0:4], bef, bef[:, :].unsqueeze(2).to_broadcast([128, NBLK, 4]), 512.0, prow,
                prow[:, 0:4].unsqueeze(1).to_broadcast([128, NBLK, 4]), ALU.mult, ALU.add)
            tsc(kb, "dve", of, of[:, :, 0:4], of, of[:, :, 0:4], float(l * NE * DEXP), None, ALU.add)
            tt(kb, "dve", of, of[:, :, 0:4], of, of[:, :, 0:4], unused, unused[:, :].unsqueeze(2).to_broadcast([128, NBLK, 4]), ALU.add)
            cp(kb, "dve", offs_d, offs_d[:, :, :], of, of[:, :, 0:4])
            ppf = Rot([kb.psum(s2, "ps_pf%d" % i, [128, 64], F32) for i in range(2)])
            tq = kb.sbuf(s2, "tq64", [128, NE], F32)
            tq2 = kb.sbuf(s2, "tq64b", [128, NE], F32)
            dsf = kb.sbuf(s2, "dsf", [128, NT, 2], F32)
            for i in range(NT):
                pp = ppf.next()
                mm(kb, pp, pp[:, :], striU, striU[:, :], ohs, ohs[:, i, :], start=True, stop=False)
                mm(kb, pp, pp[:, :], C.ones_f, C.ones_f[:, :], cum, cum[:, i, :], start=False, stop=True)
                tt(kb, "dve", tq, tq[:, :], pp, pp[:, :], pstart, pstart[:, :], ALU.add)
                for k_ in range(2):
                    tt(kb, "dve", tq2, tq2[:, :], tq, tq[:, :], ohE, ohE[:, i, k_, :], ALU.mult)
                    kb.op("dve", lambda e, i=i, k_=k_: e.reduce_sum(out=dsf[:, i, k_:k_ + 1], in_=tq2[:, :], axis=AX.X), r=[tq2], w=[dsf])
            cp(kb, "dve", destI, destI[:, :, :], dsf, dsf[:, :, :])
            kb.barrier()
        kb.serialize = False
        xb = io.xb
        with contextlib.ExitStack() as s2:
            zt = kb.sbuf(s2, "zt", [128, 8, D], F32)
            memset(kb, "dve", zt, zt[:, :, :], 0.0)
            for j in range(NROWS // 1024):
                kb.dma("sp", lambda e, j=j: e.dma_start(out=xb[j * 1024:(j + 1) * 1024, :].rearrange("(a p) d -> p a d", p=128),
                                                        in_=zt[:, :, :]), r=[zt], w=[xb])
            xt = Rot([kb.sbuf(s2, "sx%d" % i, [128, D], F32) for i in range(3)])
            for i in range(NT):
                x = xt.next()
                dma(kb, "sp", x, x[:, :], io.h_mid[i], io.h_mid[i][i * 128:(i + 1) * 128, :])
                for k_ in range(2):
                    idma(kb, xb, xb[:, :], destI[:, i, k_:k_ + 1], x, x[:, :], None, extra_r=[destI], nrows=NROWS)
            kb.barrier()
        yb = io.yb
        wg_v = io.w_gate[:, :, :, :].rearrange("l e (p c) n -> (l e p) (c n)", c=8)
        wu_v = io.w_up[:, :, :, :].rearrange("l e (p c) n -> (l e p) (c n)", c=8)
        wd_v = io.w_down[:, :, :, :].rearrange("l e (p c) n -> (l e p) (c n)", c=4)
        with contextlib.ExitStack() as s2:
            WDT = BF16
            wg = Rot([kb.sbuf(s2, "wg%d" % i, [128, 8, DEXP], WDT) for i in range(3)])
            wu = Rot([kb.sbuf(s2, "wu%d" % i, [128, 8, DEXP], WDT) for i in range(3)])
            wd = Rot([kb.sbuf(s2, "wd%d" % i, [128, 4, D], WDT) for i in range(3)])
            xr = Rot([kb.sbuf(s2, "xr%d" % i, [128, D], F32) for i in range(2)])
            xrb = Rot([kb.sbuf(s2, "xrb%d" % i, [128, D], BF16) for i in range(2)])
            xT = Rot([kb.sbuf(s2, "xT%d" % i, [128, 8, 128], BF16) for i in range(2)])
            hid = Rot([kb.sbuf(s2, "hid%d" % i, [128, 4, 128], BF16) for i in range(2)])
            sg_ = Rot([kb.sbuf(s2, "sgx%d" % i, [128, 4, 128], F32) for i in range(2)])
            yo = Rot([kb.sbuf(s2, "yo%d" % i, [128, D], F32) for i in range(2)])
            pst = Rot([kb.psum(s2, "ps_xt%d" % i, [128, 8, 128], BF16) for i in range(2)])
            psg = Rot([kb.psum(s2, "ps_g%d" % i, [128, 4, 128], F32) for i in range(2)])
            psu = Rot([kb.psum(s2, "ps_u%d" % i, [128, 4, 128], F32) for i in range(2)])
            psy = Rot([kb.psum(s2, "ps_yy%d" % i, [128, 512], F32) for i in range(2)])
            for j in range(dbg.get("nblk", NBLK)):
                wg_, wu_, wd_ = wg.next(), wu.next(), wd.next()
                NR = DEPTH * NE * 128
                idma(kb, wg_, wg_[:, :, :].rearrange("p c n -> p (c n)"), None, io.w_gate, wg_v, offs_gu[:, j, 0:1], extra_r=[offs_gu], nrows=NR)
                idma(kb, wu_, wu_[:, :, :].rearrange("p c n -> p (c n)"), None, io.w_up, wu_v, offs_gu[:, j, 0:1], extra_r=[offs_gu], nrows=NR)
                idma(kb, wd_, wd_[:, :, :].rearrange("p c n -> p (c n)"), None, io.w_down, wd_v, offs_gu[:, j, 0:1], extra_r=[offs_gu], nrows=NR)
                x = xr.next()
                dma(kb, "sp", x, x[:, :], xb, xb[j * 128:(j + 1) * 128, :])
                xb_ = xrb.next()
                cp(kb, "dve", xb_, xb_[:, :], x, x[:, :])
                xT_ = xT.next()
                pt = pst.next()
                xperm = xb_[:, :].rearrange("r (p c) -> r c p", c=8)
                for c in range(8):
                    tr(kb, pt, pt[:, c, :], xb_, xperm[:, c, :], C.id_b, C.id_b[:, :], inc=(c == 7))
                cp(kb, "act", xT_, xT_[:, :, :], pt, pt[:, :, :])
                pg, pu = psg.next(), psu.next()
                for m in range(4):
                    for c in range(8):
                        mm(kb, pg, pg[:, m, :], wg_, wg_[:, c, :].rearrange("k (p m) -> k m p", m=4)[:, m, :], xT_, xT_[:, c, :],
                           start=(c == 0), stop=(c == 7), inc=(c == 7 and m == 3))
                for m in range(4):
                    for c in range(8):
                        mm(kb, pu, pu[:, m, :], wu_, wu_[:, c, :].rearrange("k (p m) -> k m p", m=4)[:, m, :], xT_, xT_[:, c, :],
                           start=(c == 0), stop=(c == 7), inc=(c == 7 and m == 3))
                s_ = sg_.next()
                act(kb, s_, s_[:, :, :], pg, pg[:, :, :], AF.Silu)
                h_ = hid.next()
                tt(kb, "dve", h_, h_[:, :, :], s_, s_[:, :, :], pu, pu[:, :, :], ALU.mult)
                y_ = yo.next()
                for nh in range(2):
                    py = psy.next()
                    for c in range(4):
                        mm(kb, py, py[:, :], h_, h_[:, c, :], wd_, wd_[:, c, nh * 512:(nh + 1) * 512], start=(c == 0), stop=(c == 3))
                    cp(kb, "act", y_, y_[:, nh * 512:(nh + 1) * 512], py, py[:, :])
                dma(kb, "sp", yb, yb[j * 128:(j + 1) * 128, :], y_, y_[:, :])
            kb.barrier()
        with contextlib.ExitStack() as s2:
            y0 = Rot([kb.sbuf(s2, "gy0_%d" % i, [128, D], F32) for i in range(2)])
            y1 = Rot([kb.sbuf(s2, "gy1_%d" % i, [128, D], F32) for i in range(2)])
            hm = Rot([kb.sbuf(s2, "ghm%d" % i, [128, D], F32) for i in range(2)])

            def src(i, buf):
                a, b_, h_ = y0.next(), y1.next(), hm.next()
                idma(kb, a, a[:, :], None, yb, yb[:, :], destI[:, i, 0:1], extra_r=[destI], nrows=NROWS)
                idma(kb, b_, b_[:, :], None, yb, yb[:, :], destI[:, i, 1:2], extra_r=[destI], nrows=NROWS)
                dma(kb, "sp", h_, h_[:, :], io.h_mid[i], io.h_mid[i][i * 128:(i + 1) * 128, :])
                tsc(kb, "dve", a, a[:, :], a, a[:, :], gate[:, i, 0:1], None, ALU.mult, extra_r=[gate])
                stt(kb, "dve", a, a[:, :], b_, b_[:, :], gate[:, i, 1:2], a, a[:, :], ALU.mult, ALU.add, extra_r=[gate])
                stt(kb, "dve", buf, buf[:, :], h_, h_[:, :], float(DN_ALPHA), a, a[:, :], ALU.mult, ALU.add)
            emit_ln_rows(kb, C, s2, "l2_", src, (io.ln2_g, io.ln2_g[l:l + 1, :]), (io.ln2_b, io.ln2_b[l:l + 1, :]), io,
                         list(range(NT)), dst_tiles)
            kb.barrier()


def build_program(dbg=None):
    dbg = dict(dbg or {})
    dbg.setdefault("serial", 0)
    kb = KB()
    io = declare_io(kb)
    with contextlib.ExitStack() as st:
        C = setup_consts(kb, st)
        stage_prologue(kb, C, io, list(range(NT)))
        stage_rope_tables(kb, C, io)
        depth = dbg.get("depth", DEPTH)
        for l in range(depth):
            stage_mixer(kb, C, io, l, list(range(NSEQ)), dbg)
            stage_moe(kb, C, io, l, io.h_res if l < depth - 1 else io.out, dbg)
        kb.finish()
    return kb


def kernel(**inputs):
    n = 8
    kb = build_program()
    in_maps = [make_in_map(inputs, c) for c in range(n)]
    res = run_bass_kernel_spmd(kb.nc, in_maps, core_ids=list(range(n)))
    out = np.concatenate([np.asarray(r["out"], dtype=np.float32).reshape(NSEQ, SEQ, D) for r in res.results], axis=0)
    return out
```

```python
import contextlib
import numpy as np
import concourse.bass as bass
import concourse.mybir as mybir
from concourse.bass_utils import run_bass_kernel_spmd

F32 = mybir.dt.float32
BF16 = mybir.dt.bfloat16
I32 = mybir.dt.int32
U32 = mybir.dt.uint32
AF = mybir.ActivationFunctionType
ALU = mybir.AluOpType
AX = mybir.AxisListType

D = 1024
SEQ = 2048
NSEQ = 2
T = NSEQ * SEQ
NT = T // 128
DEPTH = 2
H = 8
IN_DIM = 6880
OFF_Q, OFF_K, OFF_V, OFF_Z, OFF_A, OFF_BT, OFF_CQ, OFF_CKV, OFF_KR, OFF_G = (
    0, 1024, 2048, 3072, 4096, 4112, 4128, 4512, 4768, 4832)
QL, KVL, ROPE = 384, 256, 64
NE = 64
DEXP = 512
NBLK = 128
NROWS = NBLK * 128
DN_ALPHA = (2 * DEPTH) ** 0.25
LN_EPS = 1e-5
RMS_EPS = 1e-6
CH = 128
NCH = SEQ // CH


class Buf:
    def __init__(self, t, name):
        self.t = t
        self.name = name
        self.w = None
        self.r = {}

    def __getitem__(self, idx):
        return self.t[idx]


class KB:
    ENG = ("pe", "act", "dve", "pool", "sp")

    def __init__(self):
        self.nc = bass.Bass("TRN2", target_bir_lowering=False)
        nc = self.nc
        self.es = contextlib.ExitStack()
        self.eng = {"pe": nc.tensor, "act": nc.scalar, "dve": nc.vector, "pool": nc.gpsimd, "sp": nc.sync}
        self.sems = {}
        self.cnt = {}
        self.known = {e: {} for e in self.ENG}
        for e in self.ENG:
            self.sems[e] = self.es.enter_context(nc.semaphore("sem_" + e))
            self.cnt[e] = 0
        self.dq = {}
        for q, n in (("sp", 12), ("pool", 28), ("act", 2)):
            keys = []
            for i in range(n):
                k = "d_%s_%d" % (q, i)
                self.sems[k] = self.es.enter_context(nc.semaphore(k))
                self.cnt[k] = 0
                keys.append(k)
            self.dq[q] = [keys, 0]
        self.ninst = 0
        self.uid = 0
        self.scr = None
        self.serialize = False
        self.psum_rar = True
        self.chain = False
        self.chain_engines = ("pe", "act", "dve", "pool")
        self.last_op = None
        self.flush_nop = False

    def sbuf(self, stack, name, shape, dt):
        self.uid += 1
        name = "%s_u%d" % (name, self.uid)
        return Buf(stack.enter_context(self.nc.sbuf_tensor(name, list(shape), dt)), name)

    def psum(self, stack, name, shape, dt):
        self.uid += 1
        name = "%s_u%d" % (name, self.uid)
        b = Buf(stack.enter_context(self.nc.psum_tensor(name, list(shape), dt)), name)
        b.is_psum = True
        return b

    def dram(self, name, shape, dt, kind="Internal"):
        return Buf(self.nc.dram_tensor(name, list(shape), dt, kind=kind).ap(), name)

    def dram_tiles(self, name, shape, dt, n, kind="Internal"):
        ap = self.nc.dram_tensor(name, list(shape), dt, kind=kind).ap()
        return [Buf(ap, "%s_%d" % (name, i)) for i in range(n)]

    def _waits(self, en, r, w, is_dma=False):
        need = {}
        for b in r:
            if b.w is not None:
                need[b.w[0]] = max(need.get(b.w[0], 0), b.w[1])
            if self.psum_rar and getattr(b, "is_psum", False) and en != "pe":
                for sk, val in b.r.items():
                    if sk != en:
                        need[sk] = max(need.get(sk, 0), val)
        for b in w:
            if b.w is not None and (is_dma or b.w[0] != en):
                need[b.w[0]] = max(need.get(b.w[0], 0), b.w[1])
            for sk, val in b.r.items():
                if is_dma or sk != en:
                    need[sk] = max(need.get(sk, 0), val)
        E = self.eng[en]
        kn = self.known[en]
        for sk, val in need.items():
            if kn.get(sk, 0) < val:
                E.wait_ge(self.sems[sk], val)
                kn[sk] = val
                self.ninst += 1

    def _done(self, tok, r, w):
        for b in r:
            b.r[tok[0]] = max(b.r.get(tok[0], 0), tok[1])
        for b in w:
            b.w = tok
            b.r = {}

    def op(self, en, fn, r=(), w=(), inc=True):
        self._waits(en, r, w)
        if self.chain and en in self.chain_engines and self.last_op is not None and self.last_op[0] != en:
            lk, lv = self.last_op
            if self.known[en].get(lk, 0) < lv:
                self.eng[en].wait_ge(self.sems[lk], lv)
                self.known[en][lk] = lv
        ins = fn(self.eng[en])
        self.ninst += 1
        if inc:
            self.cnt[en] += 1
            ins.then_inc(self.sems[en], 1)
            self._done((en, self.cnt[en]), r, w)
        else:
            self._done((en, self.cnt[en] + 1), r, w)
        if en in self.chain_engines:
            self.last_op = (en, self.cnt[en] + (0 if inc else 1))
        if self.flush_nop and self.scr is not None and en in ("dve", "act") and any(getattr(b, "is_psum", False) for b in r):
            if en == "dve":
                self.eng[en].memset(self.scr[0:1, 0:1], 0.0)
            else:
                self.eng[en].memzero(self.scr[0:1, 2:3])
            self.ninst += 1
        if self.serialize and inc:
            self.barrier()
        return ins

    def dma(self, q, fn, r=(), w=()):
        keys, i = self.dq[q]
        k = keys[i % len(keys)]
        self.dq[q][1] = i + 1
        self._waits(q, r, w, True)
        kn = self.known[q]
        if kn.get(k, 0) < self.cnt[k]:
            self.eng[q].wait_ge(self.sems[k], self.cnt[k])
            kn[k] = self.cnt[k]
        ins = fn(self.eng[q])
        self.cnt[k] += 16
        ins.then_inc(self.sems[k], 16)
        self.ninst += 1
        self._done((k, self.cnt[k]), r, w)
        return ins

    def barrier(self):
        for en in self.ENG:
            kn = self.known[en]
            for sk, val in self.cnt.items():
                if sk != en and val > 0 and kn.get(sk, 0) < val:
                    self.eng[en].wait_ge(self.sems[sk], val)
                    kn[sk] = val

    def finish(self):
        kn = self.known["sp"]
        for sk, val in self.cnt.items():
            if sk != "sp" and val > 0 and kn.get(sk, 0) < val:
                self.nc.sync.wait_ge(self.sems[sk], val)
                kn[sk] = val
        self.es.close()


def mm(kb, ob, oap, lb, lap, rb, rap, start=True, stop=True, inc=None):
    if inc is None:
        inc = stop
    return kb.op("pe", lambda e: e.matmul(oap, lhsT=lap, rhs=rap, start=start, stop=stop), r=[lb, rb], w=[ob], inc=inc)


def tr(kb, ob, oap, ib, iap, idb, idap, inc=True):
    return kb.op("pe", lambda e: e.transpose(oap, iap, idap), r=[ib, idb], w=[ob], inc=inc)


def act(kb, ob, oap, ib, iap, func, bias=None, scale=None, extra_r=(), accum=None, en="act"):
    kw = {}
    if bias is not None:
        kw["bias"] = bias
    if scale is not None:
        kw["scale"] = scale
    w = [ob]
    if accum is not None:
        kw["accum_out"] = accum[1]
        w.append(accum[0])
    return kb.op("act", lambda e: e.activation(out=oap, in_=iap, func=func, **kw), r=[ib] + list(extra_r), w=w)


def tt(kb, en, ob, oap, ab, aap, bb, bap, op):
    return kb.op(en, lambda e: e.tensor_tensor(out=oap, in0=aap, in1=bap, op=op), r=[ab, bb], w=[ob])


def tsc(kb, en, ob, oap, ib, iap, s1, s2, op0, op1=None, extra_r=()):
    if op1 is None:
        return kb.op(en, lambda e: e.tensor_scalar(out=oap, in0=iap, scalar1=s1, scalar2=None, op0=op0),
                     r=[ib] + list(extra_r), w=[ob])
    return kb.op(en, lambda e: e.tensor_scalar(out=oap, in0=iap, scalar1=s1, scalar2=s2, op0=op0, op1=op1),
                 r=[ib] + list(extra_r), w=[ob])


def stt(kb, en, ob, oap, ab, aap, scalar, bb, bap, op0, op1, extra_r=()):
    return kb.op(en, lambda e: e.scalar_tensor_tensor(out=oap, in0=aap, scalar=scalar, in1=bap, op0=op0, op1=op1),
                 r=[ab, bb] + list(extra_r), w=[ob])


def cp(kb, en, ob, oap, ib, iap):
    if en == "act":
        return kb.op("act", lambda e: e.copy(oap, iap), r=[ib], w=[ob])
    return kb.op(en, lambda e: e.tensor_copy(oap, iap), r=[ib], w=[ob])


def memset(kb, en, ob, oap, val):
    return kb.op(en, lambda e: e.memset(oap, val), r=[], w=[ob])


def dma(kb, q, ob, oap, ib, iap):
    return kb.dma(q, lambda e: e.dma_start(out=oap, in_=iap), r=[ib], w=[ob])


class Consts:
    pass


def rsqrt(kb, C, ob, oap, ib, iap, eps):
    act(kb, ob, oap, ib, iap, AF.Sqrt, bias=C.eps_tile(eps)[0:oap.shape[0], 0:1], extra_r=[C.eps_buf])
    kb.op("dve", lambda e: e.reciprocal(oap, oap), r=[ob], w=[ob])


def setup_consts(kb, st):
    c = Consts()
    kb.scr = kb.sbuf(st, "kb_scr", [128, 8], F32).t
    c.ones_f = kb.sbuf(st, "ones_f", [128, 128], F32)
    c.ones_b = kb.sbuf(st, "ones_b", [128, 128], BF16)
    c.id_f = kb.sbuf(st, "id_f", [128, 128], F32)
    c.id_b = kb.sbuf(st, "id_b", [128, 128], BF16)
    c.id4_f = kb.sbuf(st, "id4_f", [128, 4, 128], F32)
    c.triF = kb.sbuf(st, "triF", [128, 128], F32)
    c.triB = kb.sbuf(st, "triB", [128, 128], F32)
    c.fill_zero = kb.nc.gpsimd.to_reg(0.0)
    c.fill_neg = kb.nc.gpsimd.to_reg(-30000.0)
    c.fill_pos = kb.nc.gpsimd.to_reg(30000.0)
    c.eps_buf = kb.sbuf(st, "eps_c", [128, 2], F32)
    memset(kb, "pool", c.eps_buf, c.eps_buf[:, 0:1], LN_EPS)
    memset(kb, "pool", c.eps_buf, c.eps_buf[:, 1:2], RMS_EPS)
    c.eps_tile = lambda eps: c.eps_buf[:, 0:1] if eps == LN_EPS else c.eps_buf[:, 1:2]
    memset(kb, "pool", c.ones_f, c.ones_f[:], 1.0)
    memset(kb, "pool", c.ones_b, c.ones_b[:], 1.0)
    kb.op("pool", lambda e: e.affine_select(out=c.id_f[:], in_=c.ones_f[:], pattern=[[-1, 128]],
                                            compare_op=ALU.is_equal, fill=c.fill_zero, base=0, channel_multiplier=1),
          r=[c.ones_f], w=[c.id_f])
    cp(kb, "pool", c.id_b, c.id_b[:], c.id_f, c.id_f[:])
    for i in range(4):
        cp(kb, "pool", c.id4_f, c.id4_f[:, i, :], c.id_f, c.id_f[:])
    kb.op("pool", lambda e: e.affine_select(out=c.triF[:], in_=c.ones_f[:], pattern=[[1, 128]],
                                            compare_op=ALU.is_ge, fill=c.fill_zero, base=0, channel_multiplier=-1),
          r=[c.ones_f], w=[c.triF])
    kb.op("pool", lambda e: e.affine_select(out=c.triB[:], in_=c.ones_f[:], pattern=[[-1, 128]],
                                            compare_op=ALU.is_ge, fill=c.fill_zero, base=0, channel_multiplier=1),
          r=[c.ones_f], w=[c.triB])
    zero4 = kb.sbuf(st, "zero4", [128, 4, 128], F32)
    memset(kb, "pool", zero4, zero4[:, :, :], 0.0)
    c.mT, c.mS = [], []
    for d in range(2):
        if d == 0:
            patT, cmT, pat, cm = [[0, 4], [1, 128]], -1, [[0, 4], [-1, 128]], 1
        else:
            patT, cmT, pat, cm = [[0, 4], [-1, 128]], 1, [[0, 4], [1, 128]], -1
        mT = kb.sbuf(st, "mT%d" % d, [128, 4, 128], F32)
        mS = kb.sbuf(st, "mS%d" % d, [128, 4, 128], F32)
        kb.op("pool", lambda e, mT=mT, patT=patT, cmT=cmT: e.affine_select(
            out=mT[:, :, :], in_=zero4[:, :, :], pattern=patT, compare_op=ALU.is_ge, fill=c.fill_neg, base=0,
            channel_multiplier=cmT), r=[zero4], w=[mT])
        kb.op("pool", lambda e, mS=mS, pat=pat, cm=cm: e.affine_select(
            out=mS[:, :, :], in_=zero4[:, :, :], pattern=pat, compare_op=ALU.is_ge, fill=c.fill_pos, base=-1,
            channel_multiplier=cm), r=[zero4], w=[mS])
        c.mT.append(mT)
        c.mS.append(mS)
    kb.barrier()
    return c


def sin_reduced(kb, st, nm, ob, oap, ib, iap, shift, shape):
    u = kb.sbuf(st, nm + "_u", shape, F32)
    ki = kb.sbuf(st, nm + "_ki", shape, I32)
    kf = kb.sbuf(st, nm + "_kf", shape, F32)
    mk = kb.sbuf(st, nm + "_mk", shape, F32)
    full = tuple(slice(None) for _ in shape)
    inv2pi = float(1.0 / (2.0 * np.pi))
    tsc(kb, "dve", u, u[full], ib, iap, inv2pi, float(shift) * inv2pi, ALU.mult, ALU.add)
    cp(kb, "dve", ki, ki[full], u, u[full])
    cp(kb, "dve", kf, kf[full], ki, ki[full])
    tt(kb, "dve", u, u[full], u, u[full], kf, kf[full], ALU.subtract)
    tsc(kb, "dve", mk, mk[full], u, u[full], 0.5, None, ALU.is_gt)
    tt(kb, "dve", u, u[full], u, u[full], mk, mk[full], ALU.subtract)
    tsc(kb, "dve", mk, mk[full], u, u[full], -0.5, None, ALU.is_lt)
    tt(kb, "dve", u, u[full], u, u[full], mk, mk[full], ALU.add)
    tsc(kb, "dve", u, u[full], u, u[full], -0.4999999, 0.4999999, ALU.max, ALU.min)
    act(kb, ob, oap, u, u[full], AF.Sin, scale=float(2.0 * np.pi))


def layernorm_tile(kb, wk, x_b, g_bc, b_bc, out_b, eps=LN_EPS):
    stats, mv, rstd = wk["stats"], wk["mv"], wk["rstd"]
    for ci in range(2):
        kb.op("dve", lambda e, ci=ci: e.bn_stats(stats[:, ci, :], x_b[:, ci * 512:(ci + 1) * 512]), r=[x_b], w=[stats])
    kb.op("dve", lambda e: e.bn_aggr(mv[:, :], stats[:, :, :]), r=[stats], w=[mv])
    rsqrt(kb, wk["C"], rstd, rstd[:, :], mv, mv[:, 1:2], eps)
    tsc(kb, "dve", out_b, out_b[:, :], x_b, x_b[:, :], mv[:, 0:1], rstd[:, 0:1], ALU.subtract, ALU.mult,
        extra_r=[mv, rstd])
    tt(kb, "dve", out_b, out_b[:, :], out_b, out_b[:, :], g_bc, g_bc[:, :], ALU.mult)
    tt(kb, "dve", out_b, out_b[:, :], out_b, out_b[:, :], b_bc, b_bc[:, :], ALU.add)


class IO:
    pass


def declare_io(kb, dbg=None, small=False):
    nc = kb.nc
    io = IO()

    def inp(name, shape, dt=F32):
        b = Buf(nc.dram_tensor(name, list(shape), dt, kind="ExternalInput").ap(), name)
        setattr(io, name, b)
        return b
    inp("x", [T, D])
    inp("positions", [1, T], I32)
    inp("ln_in_g", [1, D]); inp("ln_in_b", [1, D])
    inp("w_in", [DEPTH, D, IN_DIM])
    inp("conv_w", [DEPTH, 5, 3072])
    inp("a_log", [DEPTH, 1, 16]); inp("dt_bias", [DEPTH, 1, 16])
    inp("gdn_norm_w", [DEPTH, 128, 1])
    inp("w_o_gdn", [DEPTH, D, D])
    inp("mla_q_norm_w", [DEPTH, QL, 1])
    inp("w_uq", [DEPTH, QL, 1536])
    inp("mla_kv_norm_w", [DEPTH, KVL, 1])
    inp("w_ukv", [DEPTH, KVL, 2048])
    inp("w_o_mla", [DEPTH, D, D])
    inp("w_out", [DEPTH, D, D])
    inp("ln1_g", [DEPTH, D]); inp("ln1_b", [DEPTH, D])
    inp("w_router_group", [DEPTH, D, 8]); inp("b_router_group", [DEPTH, 8])
    inp("w_router_expert", [DEPTH, D, NE]); inp("b_router_expert", [DEPTH, NE])
    if not small:
        inp("w_gate", [DEPTH, NE, D, DEXP]); inp("w_up", [DEPTH, NE, D, DEXP]); inp("w_down", [DEPTH, NE, DEXP, D])
    inp("ln2_g", [DEPTH, D]); inp("ln2_b", [DEPTH, D])
    io.out = kb.dram_tiles("out", [T, D], F32, NT, kind="ExternalOutput")
    io.h_res = kb.dram_tiles("h_res", [T, D], F32, NT)
    io.h_mid = kb.dram_tiles("h_mid", [T, D], F32, NT)
    io.hT = kb.dram_tiles("hT_scr", [128, 8, T], BF16, NT)
    io.cs = kb.dram("cs_scr", [2, 64, T], F32)
    io.xb = kb.dram("xb_scr", [NROWS, D], F32)
    io.yb = kb.dram("yb_scr", [NROWS, D], F32)
    io.og_scr = kb.dram("og_scr", [NSEQ, 128, 8, SEQ], BF16)
    io.om_scr = kb.dram("om_scr", [NSEQ, 128, 8, SEQ], BF16)
    io.dbg = {}
    for name, shape in (dbg or {}).items():
        io.dbg[name] = Buf(nc.dram_tensor(name, list(shape), F32, kind="ExternalOutput").ap(), name)
    return io


def emit_ln_rows(kb, C, st, nm, src_fn, g_ap, b_ap, io, tiles, h_dst, pre_fn=None, nb=2):
    g_bc = kb.sbuf(st, nm + "g", [128, D], F32)
    b_bc = kb.sbuf(st, nm + "b", [128, D], F32)
    dma(kb, "sp", g_bc, g_bc[:, :], g_ap[0], g_ap[1].partition_broadcast(128))
    dma(kb, "sp", b_bc, b_bc[:, :], b_ap[0], b_ap[1].partition_broadcast(128))
    NB = nb
    xt = [kb.sbuf(st, nm + "x%d" % j, [128, D], F32) for j in range(NB)]
    ht = [kb.sbuf(st, nm + "h%d" % j, [128, D], F32) for j in range(NB)]
    hb = [kb.sbuf(st, nm + "hb%d" % j, [128, D], BF16) for j in range(NB)]
    hTt = [kb.sbuf(st, nm + "hT%d" % j, [128, 8, 128], BF16) for j in range(NB)]
    pst = [kb.psum(st, nm + "ps%d" % j, [128, 8, 128], BF16) for j in range(NB)]
    wk = [dict(C=C, stats=kb.sbuf(st, nm + "st%d" % j, [128, 2, 6], F32), mv=kb.sbuf(st, nm + "mv%d" % j, [128, 2], F32),
               rstd=kb.sbuf(st, nm + "rs%d" % j, [128, 1], F32)) for j in range(NB)]
    for n, i in enumerate(tiles):
        j = n % NB
        src_fn(i, xt[j])
        layernorm_tile(kb, wk[j], xt[j], g_bc, b_bc, ht[j])
        dma(kb, "sp", h_dst[i], h_dst[i][i * 128:(i + 1) * 128, :], ht[j], ht[j][:, :])
        cp(kb, "act", hb[j], hb[j][:, :], ht[j], ht[j][:, :])
        for c in range(8):
            tr(kb, pst[j], pst[j][:, c, :], hb[j], hb[j][:, c * 128:(c + 1) * 128], C.id_b, C.id_b[:, :], inc=(c == 7))
        cp(kb, "dve", hTt[j], hTt[j][:, :, :], pst[j], pst[j][:, :, :])
        dma(kb, "sp", io.hT[i], io.hT[i][:, :, i * 128:(i + 1) * 128], hTt[j], hTt[j][:, :, :])


def stage_prologue(kb, C, io, tiles):
    with contextlib.ExitStack() as st:
        def src(i, buf):
            dma(kb, "pool", buf, buf[:, :], io.x, io.x[i * 128:(i + 1) * 128, :])
        emit_ln_rows(kb, C, st, "p_", src, (io.ln_in_g, io.ln_in_g[0:1, :]), (io.ln_in_b, io.ln_in_b[0:1, :]), io,
                     tiles, io.h_res, nb=4)
        kb.barrier()


def make_in_map(inp, core, small=False):
    f = lambda k: np.ascontiguousarray(np.asarray(inp[k], dtype=np.float32))
    m = {}
    m["x"] = f("x")[NSEQ * core:NSEQ * (core + 1)].reshape(T, D)
    m["positions"] = np.ascontiguousarray(np.asarray(inp["positions"], dtype=np.int32)[NSEQ * core:NSEQ * (core + 1)].reshape(1, T))
    m["ln_in_g"] = f("ln_in_g").reshape(1, D)
    m["ln_in_b"] = f("ln_in_b").reshape(1, D)
    m["a_log"] = f("a_log").reshape(DEPTH, 1, 16)
    m["dt_bias"] = f("dt_bias").reshape(DEPTH, 1, 16)
    m["gdn_norm_w"] = f("gdn_norm_w").reshape(DEPTH, 128, 1)
    m["mla_q_norm_w"] = f("mla_q_norm_w").reshape(DEPTH, QL, 1)
    m["mla_kv_norm_w"] = f("mla_kv_norm_w").reshape(DEPTH, KVL, 1)
    for k in ("w_in", "conv_w", "w_o_gdn", "w_uq", "w_ukv", "w_o_mla", "w_out", "ln1_g", "ln1_b", "w_router_group",
              "b_router_group", "w_router_expert", "b_router_expert", "w_gate", "w_up", "w_down", "ln2_g", "ln2_b"):
        if small and k in ("w_gate", "w_up", "w_down"):
            continue
        m[k] = f(k)
    return m


class Rot:
    def __init__(self, bufs):
        self.bufs = bufs
        self.i = 0

    def next(self):
        b = self.bufs[self.i % len(self.bufs)]
        self.i += 1
        return b


def load_w_fm(kb, io_w, w_ap, wt):
    kb.dma("pool", lambda e: e.dma_start(out=wt[:, :, :], in_=w_ap.rearrange("(k p) m -> p k m", p=128)),
           r=[io_w], w=[wt])


def gdn_seq(kb, C, io, l, b, load_hT, og_scr, cwT, dbg):
    w_in = io.w_in
    with contextlib.ExitStack() as st:
        g_tm = kb.sbuf(st, "g_tm", [128, 2, NCH, 8], F32)
        be_tm = kb.sbuf(st, "be_tm", [128, 2, NCH, 8], F32)
        b_tm = kb.sbuf(st, "b_tm", [128, 2, NCH, 8], F32)
        nbe_tm = kb.sbuf(st, "nbe_tm", [128, 2, NCH, 8], F32)
        v4 = lambda x: x[:, :, :, :].rearrange("p d c h -> p c d h")
        with contextlib.ExitStack() as s2:
            hT_b = load_hT(s2)
            wab = kb.sbuf(s2, "wab", [128, 8, 32], BF16)
            load_w_fm(kb, w_in, w_in[l, :, OFF_A:OFF_A + 32], wab)
            alog = kb.sbuf(s2, "alog", [128, 16], F32)
            dtb = kb.sbuf(s2, "dtb", [128, 16], F32)
            dma(kb, "sp", alog, alog[:, :], io.a_log, io.a_log[l, 0:1, :].partition_broadcast(128))
            dma(kb, "sp", dtb, dtb[:, :], io.dt_bias, io.dt_bias[l, 0:1, :].partition_broadcast(128))
            nea = kb.sbuf(s2, "nea", [128, 16], F32)
            act(kb, nea, nea[:, :], alog, alog[:, :], AF.Exp)
            tsc(kb, "dve", nea, nea[:, :], nea, nea[:, :], -1.0, None, ALU.mult)
            ps = kb.psum(s2, "ps_ab", [128, NCH, 32], F32)
            for ci in range(NCH):
                for k in range(8):
                    mm(kb, ps, ps[:, ci, :], hT_b, hT_b[:, k, ci * 128:(ci + 1) * 128], wab, wab[:, k, :],
                       start=(k == 0), stop=(k == 7))
            tmp = kb.sbuf(s2, "ab_tmp", [128, NCH, 16], F32)
            tmp4 = tmp[:, :, :].rearrange("p c (d h) -> p c d h", d=2)
            tt(kb, "dve", tmp, tmp[:, :, :], ps, ps[:, :, 0:16], dtb, dtb[:, :].unsqueeze(1).to_broadcast([128, NCH, 16]), ALU.add)
            act(kb, tmp, tmp[:, :, :], tmp, tmp[:, :, :], AF.Exp)
            act(kb, tmp, tmp[:, :, :], tmp, tmp[:, :, :], AF.Ln, bias=1.0)
            tt(kb, "dve", g_tm, v4(g_tm), tmp, tmp4, nea,
               nea[:, :].rearrange("p (d h) -> p d h", d=2).unsqueeze(1).to_broadcast([128, NCH, 2, 8]), ALU.mult)
            act(kb, be_tm, v4(be_tm), ps, ps[:, :, 16:32].rearrange("p c (d h) -> p c d h", d=2), AF.Sigmoid)
            tsc(kb, "dve", nbe_tm, nbe_tm[:, :, :, :], be_tm, be_tm[:, :, :, :], -1.0, None, ALU.mult)
            ps2 = kb.psum(s2, "ps_b", [128, 2, NCH * 8], F32)
            mm(kb, ps2, ps2[:, 0, :], C.triF, C.triF[:, :], g_tm, g_tm[:, 0, :, :].rearrange("p c h -> p (c h)"))
            mm(kb, ps2, ps2[:, 1, :], C.triB, C.triB[:, :], g_tm, g_tm[:, 1, :, :].rearrange("p c h -> p (c h)"))
            cp(kb, "dve", b_tm, b_tm[:, :, :, :].rearrange("p d c h -> p d (c h)"), ps2, ps2[:, :, :])
            kb.barrier()
        for hg in range(2):
            gdn_group(kb, C, io, l, b, hg, load_hT, og_scr, cwT, g_tm, (be_tm, nbe_tm), b_tm, dbg)
        kb.barrier()


def gdn_group(kb, C, io, l, b, hg, load_hT, og_scr, cwT, g_tm, be_tm, b_tm, dbg):
    w_in = io.w_in
    h0 = hg * 4
    with contextlib.ExitStack() as st:
        qT = kb.sbuf(st, "qT", [128, 4, SEQ], BF16)
        kT = kb.sbuf(st, "kT", [128, 4, SEQ], BF16)
        ktok = kb.sbuf(st, "ktok", [128, NCH, 4, 128], BF16)
        vtok = kb.sbuf(st, "vtok", [128, NCH, 4, 128], BF16)
        with contextlib.ExitStack() as s2:
            hT_b = load_hT(s2)
            wts = Rot([kb.sbuf(s2, "wqkv%d" % i, [128, 8, 128], BF16) for i in range(3)])
            raw = Rot([kb.sbuf(s2, "raw%d" % i, [128, SEQ + 4], F32) for i in range(3)])
            acc = Rot([kb.sbuf(s2, "acc%d" % i, [128, SEQ], F32) for i in range(3)])
            vT = kb.sbuf(s2, "vT", [128, SEQ], BF16)
            sq = kb.sbuf(s2, "sq", [128, SEQ], F32)
            rs = kb.sbuf(s2, "rs", [128, 512], F32)
            pss = Rot([kb.psum(s2, "ps_p%d" % i, [128, 512], F32) for i in range(4)])
            psn = Rot([kb.psum(s2, "ps_n%d" % i, [128, 512], F32) for i in range(2)])
            pst = Rot([kb.psum(s2, "ps_t%d" % i, [128, 8, 128], BF16) for i in range(2)])
            def item(which, off, hh):
                h = h0 + hh
                ct = (off // 128) + h
                wt = wts.next()
                load_w_fm(kb, w_in, w_in[l, :, off + h * 128: off + (h + 1) * 128], wt)
                rw = raw.next()
                memset(kb, "pool", rw, rw[:, 0:2], 0.0)
                memset(kb, "pool", rw, rw[:, SEQ + 2:SEQ + 4], 0.0)
                for n in range(4):
                    ps = pss.next()
                    for k in range(8):
                        mm(kb, ps, ps[:, :], wt, wt[:, k, :], hT_b, hT_b[:, k, n * 512:(n + 1) * 512],
                           start=(k == 0), stop=(k == 7))
                    cp(kb, "act", rw, rw[:, 2 + n * 512: 2 + (n + 1) * 512], ps, ps[:, :])
                ac = acc.next()
                en = "dve"
                tsc(kb, en, ac, ac[:, :], rw, rw[:, 0:SEQ], cwT[:, ct, 0:1], None, ALU.mult, extra_r=[cwT])
                for j in range(1, 5):
                    stt(kb, en, ac, ac[:, :], rw, rw[:, j:j + SEQ], cwT[:, ct, j:j + 1], ac, ac[:, :],
                        ALU.mult, ALU.add, extra_r=[cwT])
                yield
                if which == "v":
                    act(kb, vT, vT[:, :], ac, ac[:, :], AF.Silu)
                    for half in range(2):
                        pt = pst.next()
                        for ci in range(8):
                            cc = half * 8 + ci
                            tr(kb, pt, pt[:, ci, :], vT, vT[:, cc * 128:(cc + 1) * 128], C.id_b, C.id_b[:, :], inc=(ci == 7))
                        cp(kb, "act", vtok, vtok[:, half * 8:(half + 1) * 8, hh, :], pt, pt[:, :, :])
                else:
                    act(kb, ac, ac[:, :], ac, ac[:, :], AF.Silu)
                    tt(kb, "pool", sq, sq[:, :], ac, ac[:, :], ac, ac[:, :], ALU.mult)
                    dst = qT if which == "q" else kT
                    for n in range(4):
                        pn = psn.next()
                        mm(kb, pn, pn[:, :], C.ones_f, C.ones_f[:, :], sq, sq[:, n * 512:(n + 1) * 512])
                        cp(kb, "act", rs, rs[:, :], pn, pn[:, :])
                        rsqrt(kb, C, rs, rs[:, :], rs, rs[:, :], RMS_EPS)
                        if which == "q":
                            stt(kb, "dve", dst, dst[:, hh, n * 512:(n + 1) * 512], ac, ac[:, n * 512:(n + 1) * 512],
                                float(128 ** -0.5), rs, rs[:, :], ALU.mult, ALU.mult)
                        else:
                            tt(kb, "dve", dst, dst[:, hh, n * 512:(n + 1) * 512], ac, ac[:, n * 512:(n + 1) * 512],
                               rs, rs[:, :], ALU.mult)
                    if which == "k":
                        for half in range(2):
                            pt = pst.next()
                            for ci in range(8):
                                cc = half * 8 + ci
                                tr(kb, pt, pt[:, ci, :], kT, kT[:, hh, cc * 128:(cc + 1) * 128], C.id_b, C.id_b[:, :], inc=(ci == 7))
                            cp(kb, "act", ktok, ktok[:, half * 8:(half + 1) * 8, hh, :], pt, pt[:, :, :])

            gens = [item(which, off, hh) for which, off in (("q", OFF_Q), ("k", OFF_K), ("v", OFF_V)) for hh in range(4)]
            next(gens[0])
            for gi in range(len(gens)):
                if gi + 1 < len(gens):
                    next(gens[gi + 1])
                for _ in gens[gi]:
                    pass
            kb.barrier()
        oT = kb.sbuf(st, "oT", [128, 4, SEQ], F32)
        if dbg.get("skip_scan"):
            memset(kb, "pool", oT, oT[:, :, :], 1.0)
            if "dump" in dbg and hg == 0:
                o = dbg["dump"]
                for j, src in enumerate((qT, kT)):
                    for hh in range(4):
                        cp(kb, "dve", oT, oT[:, hh, :], src, src[:, hh, :])
                    dma(kb, "sp", o, o[j, :, :, :], oT, oT[:, :, :])
                for j, src in enumerate((ktok, vtok)):
                    cp(kb, "dve", oT, oT[:, :, :].rearrange("p a (b c) -> p b a c", b=NCH), src, src[:, :, :, :])
                    dma(kb, "sp", o, o[2 + j, :, :, :], oT, oT[:, :, :])
                memset(kb, "pool", oT, oT[:, :, :], 1.0)
        else:
            if "scan_stop" in dbg:
                memset(kb, "pool", oT, oT[:, :, :], 1.0)
            gdn_scan(kb, C, b, h0, qT, kT, ktok, vtok, oT, g_tm, be_tm, b_tm, dbg)
        with contextlib.ExitStack() as s2:
            hT_b = load_hT(s2)
            ogb = Rot([kb.sbuf(s2, "ogb%d" % i, [128, SEQ], BF16) for i in range(2)])
            nw = kb.sbuf(s2, "gnw", [128, 1], F32)
            dma(kb, "sp", nw, nw[:, :], io.gdn_norm_w, io.gdn_norm_w[l, :, :])
            wts = Rot([kb.sbuf(s2, "wz%d" % i, [128, 8, 128], BF16) for i in range(2)])
            pss = Rot([kb.psum(s2, "ps_z%d" % i, [128, 512], F32) for i in range(2)])
            psn = Rot([kb.psum(s2, "ps_zn%d" % i, [128, 512], F32) for i in range(2)])
            sq = kb.sbuf(s2, "sqo", [128, 512], F32)
            rs = kb.sbuf(s2, "rso", [128, 512], F32)
            zs = kb.sbuf(s2, "zs", [128, 512], F32)
            for hh in range(4):
                h = h0 + hh
                wt = wts.next()
                og_ = ogb.next()
                load_w_fm(kb, w_in, w_in[l, :, OFF_Z + h * 128: OFF_Z + (h + 1) * 128], wt)
                for n in range(4):
                    sl = slice(n * 512, (n + 1) * 512)
                    ps = pss.next()
                    for k in range(8):
                        mm(kb, ps, ps[:, :], wt, wt[:, k, :], hT_b, hT_b[:, k, sl], start=(k == 0), stop=(k == 7))
                    act(kb, zs, zs[:, :], ps, ps[:, :], AF.Silu)
                    tt(kb, "pool", sq, sq[:, :], oT, oT[:, hh, sl], oT, oT[:, hh, sl], ALU.mult)
                    pn = psn.next()
                    mm(kb, pn, pn[:, :], C.ones_f, C.ones_f[:, :], sq, sq[:, :])
                    tsc(kb, "dve", rs, rs[:, :], pn, pn[:, :], 1.0 / 128.0, None, ALU.mult)
                    rsqrt(kb, C, rs, rs[:, :], rs, rs[:, :], RMS_EPS)
                    tt(kb, "dve", rs, rs[:, :], rs, rs[:, :], zs, zs[:, :], ALU.mult)
                    stt(kb, "dve", og_, og_[:, sl], oT, oT[:, hh, sl], nw[:, 0:1], rs, rs[:, :], ALU.mult, ALU.mult,
                        extra_r=[nw])
                dma(kb, "sp", og_scr, og_scr[b, :, h, :], og_, og_[:, :])
            kb.barrier()


def gdn_scan(kb, C, b, h0, qT, kT, ktok, vtok, oT, g_tm, be_pair, b_tm, dbg):
    be_tm, nbe_tm = be_pair
    kb.serialize = (dbg.get("serial") == 1)
    kb.chain = (dbg.get("serial") in (3, 4, 5))
    kb.chain_engines = {3: ("pe", "act", "dve", "pool"), 4: ("act", "dve", "pool"), 5: ("pe", "dve", "pool")}.get(dbg.get("serial"), ())
    kb.last_op = None
    kb.psum_rar = bool(dbg.get("psum_rar", 1))
    phase_bar = (lambda: kb.barrier()) if dbg.get("serial") == 2 else (lambda: None)
    with contextlib.ExitStack() as st:
        psA = Rot([kb.psum(st, "ps_s%d" % i, [128, 4, 128], F32) for i in range(7)])
        psT = kb.psum(st, "ps_sT", [128, 8, 128], BF16)
        S = kb.sbuf(st, "S", [128, 8, 128], F32)
        Sb = kb.sbuf(st, "Sb", [128, 8, 128], BF16)
        memset(kb, "dve", S, S[:, :, :], 0.0)
        memset(kb, "dve", Sb, Sb[:, :, :], 0.0)
        R2 = lambda nm, shape, dt, n=2: Rot([kb.sbuf(st, "%s%d" % (nm, i), shape, dt) for i in range(n)])
        diag = R2("diag", [128, 4, 128], F32)
        E = R2("E", [128, 4, 128], F32)
        diff = R2("diff", [128, 4, 128], F32)
        sel = R2("sel", [128, 4, 128], F32, 4)
        GT = R2("GT", [128, 4, 128], F32)
        G = R2("G", [128, 4, 128], F32)
        PT = R2("PT", [128, 4, 128], BF16)
        tmpf = R2("tmpf", [128, 4, 128], F32, 3)
        p_r = R2("p", [128, 8, 128], BF16)
        pT_r = R2("pT", [128, 8, 128], BF16)
        tTf = kb.sbuf(st, "tTf", [128, 8, 128], F32)
        tTb = R2("tTb", [128, 8, 128], BF16)
        kbT = R2("kbT", [128, 4, 128], BF16)
        qdT = R2("qdT", [128, 4, 128], BF16)
        Rb = R2("Rb", [128, 4, 128], BF16)
        Ub = R2("Ub", [128, 4, 128], BF16)
        Kd = R2("Kd", [128, 4, 128], BF16)
        wcol = R2("wcol", [128, 4], F32)
        for i in range(dbg.get("nsteps", NCH)):
            chunk = (i, NCH - 1 - i)
            Ed, dfd, PTd = [None, None], [None, None], [None, None]
            p = p_r.next()
            pT = pT_r.next()
            def prep(d):
                c = chunk[d]
                cs = slice(c * 128, (c + 1) * 128)
                bcol = b_tm[:, d, c, h0:h0 + 4]
                kk = psA.next()
                qk = psA.next()
                for hh in range(4):
                    mm(kb, kk, kk[:, hh, :], kT, kT[:, hh, cs], kT, kT[:, hh, cs], inc=(hh == 3))
                for hh in range(4):
                    mm(kb, qk, qk[:, hh, :], kT, kT[:, hh, cs], qT, qT[:, hh, cs], inc=(hh == 3))
                yield
                dg = diag.next()
                tt(kb, "dve", dg, dg[:, :, :], C.id4_f, C.id4_f[:, :, :], b_tm, bcol.unsqueeze(2).to_broadcast([128, 4, 128]), ALU.mult)
                yield
                Db = psA.next()
                mm(kb, Db, Db[:, :, :], C.ones_f, C.ones_f[:, :], dg, dg[:, :, :])
                yield
                Ed[d] = E.next()
                act(kb, Ed[d], Ed[d][:, :, :], Db, Db[:, :, :], AF.Exp)
                yield
                dfd[d] = diff.next()
                tt(kb, "dve", dfd[d], dfd[d][:, :, :], Db, Db[:, :, :], b_tm, bcol.unsqueeze(2).to_broadcast([128, 4, 128]), ALU.subtract)
                s1 = sel.next()
                tt(kb, "dve", s1, s1[:, :, :], dfd[d], dfd[d][:, :, :], C.mT[d], C.mT[d][:, :, :], ALU.add)
                s2 = sel.next()
                tt(kb, "dve", s2, s2[:, :, :], dfd[d], dfd[d][:, :, :], C.mS[d], C.mS[d][:, :, :], ALU.add)
                yield
                gt = GT.next()
                act(kb, gt, gt[:, :, :], s1, s1[:, :, :], AF.Exp)
                g = G.next()
                act(kb, g, g[:, :, :], s2, s2[:, :, :], AF.Exp, scale=-1.0)
                yield
                PTd[d] = PT.next()
                tt(kb, "dve", PTd[d], PTd[d][:, :, :], qk, qk[:, :, :], gt, gt[:, :, :], ALU.mult)
                tf = tmpf.next()
                tt(kb, "dve", tf, tf[:, :, :], kk, kk[:, :, :], g, g[:, :, :], ALU.mult)
                nbecol = nbe_tm[:, d, c, h0:h0 + 4]
                tt(kb, "dve", p, p[:, d * 4:(d + 1) * 4, :], tf, tf[:, :, :], nbe_tm,
                   nbecol.unsqueeze(2).to_broadcast([128, 4, 128]), ALU.mult)
                yield
            for _ in zip(prep(0), prep(1)):
                pass
            if dbg.get("scan_stop", 9) <= 1:
                continue
            phase_bar()
            for j in range(8):
                tr(kb, psT, psT[:, j, :], p, p[:, j, :], C.id_b, C.id_b[:, :], inc=(j == 7))
            cp(kb, "act", pT, pT[:, :, :], psT, psT[:, :, :])
            tt(kb, "dve", tTf, tTf[:, :, :], psT, psT[:, :, :], C.id_f, C.id_f[:, None, :].to_broadcast([128, 8, 128]), ALU.add)
            tb = tTb.next()
            cp(kb, "act", tb, tb[:, :, :], tTf, tTf[:, :, :])
            if dbg.get("scan_stop", 9) <= 2:
                continue
            phase_bar()
            for it in range(6):
                pn = p_r.next()
                pa = [psA.next(), psA.next()]
                for j in range(8):
                    mm(kb, pa[j // 4], pa[j // 4][:, j % 4, :], pT, pT[:, j, :], p, p[:, j, :], inc=(j % 4 == 3))
                if it < 5:
                    pTn = pT_r.next()
                    pb = [psA.next(), psA.next()]
                    for j in range(8):
                        mm(kb, pb[j // 4], pb[j // 4][:, j % 4, :], p, p[:, j, :], pT, pT[:, j, :], inc=(j % 4 == 3))
                for hf in range(2):
                    cp(kb, "act", pn, pn[:, hf * 4:(hf + 1) * 4, :], pa[hf], pa[hf][:, :, :])
                if it < 5:
                    for hf in range(2):
                        cp(kb, "dve", pTn, pTn[:, hf * 4:(hf + 1) * 4, :], pb[hf], pb[hf][:, :, :])
                phase_bar()
                pu = [psA.next(), psA.next()]
                for j in range(8):
                    mm(kb, pu[j // 4], pu[j // 4][:, j % 4, :], pn, pn[:, j, :], tb, tb[:, j, :], inc=(j % 4 == 3))
                for hf in range(2):
                    tt(kb, "dve", tTf, tTf[:, hf * 4:(hf + 1) * 4, :], tTf, tTf[:, hf * 4:(hf + 1) * 4, :],
                       pu[hf], pu[hf][:, :, :], ALU.add)
                tb = tTb.next()
                cp(kb, "act", tb, tb[:, :, :], tTf, tTf[:, :, :])
                phase_bar()
                p = pn
                if it < 5:
                    pT = pTn
            if dbg.get("scan_stop", 9) <= 3:
                continue
            def chain(d):
                c = chunk[d]
                cs = slice(c * 128, (c + 1) * 128)
                last = 127 if d == 0 else 0
                becol = be_tm[:, d, c, h0:h0 + 4]
                kb_ = kbT.next()
                tt(kb, "dve", kb_, kb_[:, :, :], kT, kT[:, :, cs], Ed[d], Ed[d][:, :, :], ALU.mult)
                qd_ = qdT.next()
                tt(kb, "dve", qd_, qd_[:, :, :], qT, qT[:, :, cs], Ed[d], Ed[d][:, :, :], ALU.mult)
                yield
                pr = psA.next()
                for hh in range(4):
                    mm(kb, pr, pr[:, hh, :], kb_, kb_[:, hh, :], Sb, Sb[:, d * 4 + hh, :], inc=(hh == 3))
                yield
                tf = tmpf.next()
                tt(kb, "dve", tf, tf[:, :, :], vtok, vtok[:, c, :, :], pr, pr[:, :, :], ALU.subtract)
                rb = Rb.next()
                tt(kb, "dve", rb, rb[:, :, :], tf, tf[:, :, :], be_tm, becol.unsqueeze(2).to_broadcast([128, 4, 128]), ALU.mult)
                yield
                pu = psA.next()
                for hh in range(4):
                    mm(kb, pu, pu[:, hh, :], tb, tb[:, d * 4 + hh, :], rb, rb[:, hh, :], inc=(hh == 3))
                yield
                ub = Ub.next()
                cp(kb, "act", ub, ub[:, :, :], pu, pu[:, :, :])
                yield
                po = psA.next()
                for hh in range(4):
                    mm(kb, po, po[:, hh, :], Sb, Sb[:, d * 4 + hh, :], qd_, qd_[:, hh, :], start=True, stop=False)
                    mm(kb, po, po[:, hh, :], ub, ub[:, hh, :], PTd[d], PTd[d][:, hh, :], start=False, stop=True, inc=(hh == 3))
                yield
                first = (d == 0) == (c < NCH // 2)
                if first:
                    cp(kb, "act", oT, oT[:, :, cs], po, po[:, :, :])
                else:
                    tt(kb, "dve", oT, oT[:, :, cs], oT, oT[:, :, cs], po, po[:, :, :], ALU.add)
                wc = wcol.next()
                act(kb, wc, wc[:, :], dfd[d], dfd[d][:, :, last], AF.Exp)
                kd = Kd.next()
                tt(kb, "dve", kd, kd[:, :, :], ktok, ktok[:, c, :, :], wc, wc[:, :].unsqueeze(2).to_broadcast([128, 4, 128]), ALU.mult)
                yield
                psn = psA.next()
                for hh in range(4):
                    mm(kb, psn, psn[:, hh, :], kd, kd[:, hh, :], ub, ub[:, hh, :], inc=(hh == 3))
                yield
                Sd = S[:, d * 4:(d + 1) * 4, :]
                tt(kb, "dve", S, Sd, S, Sd, Ed[d], Ed[d][:, :, last:last + 1].to_broadcast([128, 4, 128]), ALU.mult)
                tt(kb, "dve", S, Sd, S, Sd, psn, psn[:, :, :], ALU.add)
                cp(kb, "act", Sb, Sb[:, d * 4:(d + 1) * 4, :], S, Sd)
                yield
            for _ in zip(chain(0), chain(1)):
                pass
        kb.serialize = False
        kb.chain = False
        kb.barrier()


def stage_rope_tables(kb, C, io):
    for b in range(NSEQ):
        sl = slice(b * SEQ, (b + 1) * SEQ)
        with contextlib.ExitStack() as st:
            pi_ = kb.sbuf(st, "rp_pi", [64, SEQ], I32)
            dma(kb, "sp", pi_, pi_[:, :], io.positions, io.positions[0:1, sl].partition_broadcast(64))
            pf = kb.sbuf(st, "rp_pf", [64, SEQ], F32)
            cp(kb, "dve", pf, pf[:, :], pi_, pi_[:, :])
            idx_i = kb.sbuf(st, "rp_ii", [64, 1], I32)
            kb.op("pool", lambda e: e.iota(idx_i[:, :], pattern=[[0, 1]], base=0, channel_multiplier=1), r=[], w=[idx_i])
            idx = kb.sbuf(st, "rp_if", [64, 1], F32)
            cp(kb, "dve", idx, idx[:, :], idx_i, idx_i[:, :])
            tsc(kb, "dve", idx, idx[32:64, :], idx, idx[32:64, :], -32.0, None, ALU.add)
            invf = kb.sbuf(st, "rp_inv", [64, 1], F32)
            act(kb, invf, invf[:, :], idx, idx[:, :], AF.Exp, scale=float(-np.log(10000.0) / 32.0))
            ang = kb.sbuf(st, "rp_ang", [64, SEQ], F32)
            tsc(kb, "dve", ang, ang[:, :], pf, pf[:, :], invf[:, 0:1], None, ALU.mult, extra_r=[invf])
            res = kb.sbuf(st, "rp_res", [64, SEQ], F32)
            for which, shift in ((0, np.pi / 2.0), (1, 0.0)):
                with contextlib.ExitStack() as s2:
                    sin_reduced(kb, s2, "rp_c", res, res[:, :], ang, ang[:, :], shift, [64, SEQ])
                    if which == 1:
                        tsc(kb, "dve", res, res[0:32, :], res, res[0:32, :], -1.0, None, ALU.mult)
                    dma(kb, "sp", io.cs, io.cs[which, :, sl], res, res[:, :])
                    kb.barrier()
            kb.barrier()


def rope_apply(kb, wk, src_b, src_ap, cos_b, sin_b, dst_b, dst_ap, n):
    x, xs, t1 = wk["x"], wk["xs"], wk["t1"]
    cp(kb, "act", x, x[:, 0:n], src_b, src_ap)
    cp(kb, "dve", xs, xs[0:32, 0:n], x, x[32:64, 0:n])
    cp(kb, "dve", xs, xs[32:64, 0:n], x, x[0:32, 0:n])
    tt(kb, "dve", t1, t1[:, 0:n], x, x[:, 0:n], cos_b[0], cos_b[1], ALU.mult)
    tt(kb, "pool", xs, xs[:, 0:n], xs, xs[:, 0:n], sin_b[0], sin_b[1], ALU.mult)
    tt(kb, "dve", dst_b, dst_ap, t1, t1[:, 0:n], xs, xs[:, 0:n], ALU.add)


def mla_seq(kb, C, io, l, b, load_hT, omT_dst, dbg):
    w_in = io.w_in
    scale = float(192 ** -0.5)
    with contextlib.ExitStack() as st:
        cqn = kb.sbuf(st, "cqn", [128, 3, SEQ], BF16)
        ckvn = kb.sbuf(st, "ckvn", [128, 2, SEQ], BF16)
        krT = kb.sbuf(st, "krT", [64, SEQ], BF16)
        cosb = kb.sbuf(st, "cosb", [64, SEQ], F32)
        sinb = kb.sbuf(st, "sinb", [64, SEQ], F32)
        dma(kb, "sp", cosb, cosb[:, :], io.cs, io.cs[0, :, b * SEQ:(b + 1) * SEQ])
        dma(kb, "sp", sinb, sinb[:, :], io.cs, io.cs[1, :, b * SEQ:(b + 1) * SEQ])
        rwk = dict(x=kb.sbuf(st, "rp_x", [64, 512], F32), xs=kb.sbuf(st, "rp_xs", [64, 512], F32),
                   t1=kb.sbuf(st, "rp_t1", [64, 512], F32))
        with contextlib.ExitStack() as s2:
            hT_b = load_hT(s2)
            pss = Rot([kb.psum(s2, "ps_m%d" % i, [128, 512], F32) for i in range(3)])
            psn = Rot([kb.psum(s2, "ps_mn%d" % i, [128, 512], F32) for i in range(2)])
            for nm, off, nt_, dstn, wnorm in (("cq", OFF_CQ, 3, cqn, io.mla_q_norm_w), ("ckv", OFF_CKV, 2, ckvn, io.mla_kv_norm_w)):
                wt = kb.sbuf(s2, "w_" + nm, [128, 8, nt_ * 128], BF16)
                load_w_fm(kb, w_in, w_in[l, :, off:off + nt_ * 128], wt)
                nw = kb.sbuf(s2, "nw_" + nm, [128, nt_], F32)
                for t_ in range(nt_):
                    dma(kb, "sp", nw, nw[:, t_:t_ + 1], wnorm, wnorm[l, t_ * 128:(t_ + 1) * 128, :])
                raw = kb.sbuf(s2, "raw_" + nm, [128, nt_, 512], F32)
                sq = kb.sbuf(s2, "sq_" + nm, [128, nt_, 512], F32)
                rs = kb.sbuf(s2, "rs_" + nm, [128, 512], F32)
                for n in range(4):
                    sl = slice(n * 512, (n + 1) * 512)
                    for t_ in range(nt_):
                        ps = pss.next()
                        for k in range(8):
                            mm(kb, ps, ps[:, :], wt, wt[:, k, t_ * 128:(t_ + 1) * 128], hT_b, hT_b[:, k, sl],
                               start=(k == 0), stop=(k == 7))
                        cp(kb, "act", raw, raw[:, t_, :], ps, ps[:, :])
                    tt(kb, "pool", sq, sq[:, :, :], raw, raw[:, :, :], raw, raw[:, :, :], ALU.mult)
                    pn = psn.next()
                    for t_ in range(nt_):
                        mm(kb, pn, pn[:, :], C.ones_f, C.ones_f[:, :], sq, sq[:, t_, :], start=(t_ == 0), stop=(t_ == nt_ - 1))
                    tsc(kb, "dve", rs, rs[:, :], pn, pn[:, :], 1.0 / (nt_ * 128), None, ALU.mult)
                    rsqrt(kb, C, rs, rs[:, :], rs, rs[:, :], RMS_EPS)
                    for t_ in range(nt_):
                        stt(kb, "dve", dstn, dstn[:, t_, sl], raw, raw[:, t_, :], nw[:, t_:t_ + 1], rs, rs[:, :],
                            ALU.mult, ALU.mult, extra_r=[nw])
            wkr = kb.sbuf(s2, "w_kr", [128, 8, 64], BF16)
            load_w_fm(kb, w_in, w_in[l, :, OFF_KR:OFF_KR + 64], wkr)
            for n in range(4):
                sl = slice(n * 512, (n + 1) * 512)
                ps = pss.next()
                for k in range(8):
                    mm(kb, ps, ps[0:64, :], wkr, wkr[:, k, :], hT_b, hT_b[:, k, sl], start=(k == 0), stop=(k == 7))
                rope_apply(kb, rwk, ps, ps[0:64, :], (cosb, cosb[:, sl]), (sinb, sinb[:, sl]), krT, krT[:, sl], 512)
            kb.barrier()
        with contextlib.ExitStack() as s2:
            wq = Rot([kb.sbuf(s2, "w_uq%d" % i, [128, 3, 192], BF16) for i in range(2)])
            wkv = Rot([kb.sbuf(s2, "w_ukv%d" % i, [128, 2, 256], BF16) for i in range(2)])
            qnT = Rot([kb.sbuf(s2, "qnT%d" % i, [128, SEQ], BF16) for i in range(2)])
            qrT = Rot([kb.sbuf(s2, "qrT%d" % i, [64, SEQ], BF16) for i in range(2)])
            knT = Rot([kb.sbuf(s2, "knT%d" % i, [128, SEQ], BF16) for i in range(2)])
            vtk = Rot([kb.sbuf(s2, "vtk%d" % i, [128, NCH, 128], BF16) for i in range(2)])
            pex = Rot([kb.sbuf(s2, "pex%d" % i, [128, 512], BF16) for i in range(3)])
            oh = Rot([kb.sbuf(s2, "oh%d" % i, [128, SEQ], BF16) for i in range(2)])
            rden = kb.sbuf(s2, "rden", [128, 512], F32)
            pss = Rot([kb.psum(s2, "ps_a%d" % i, [128, 512], F32) for i in range(3)])
            pso = Rot([kb.psum(s2, "ps_o%d" % i, [128, 512], F32) for i in range(2)])
            psd = Rot([kb.psum(s2, "ps_d%d" % i, [128, 512], F32) for i in range(2)])
            psv = kb.psum(s2, "ps_v", [128, 4, 128], F32)
            for h in range(H):
                wq_ = wq.next()
                load_w_fm(kb, io.w_uq, io.w_uq[l, :, h * 192:(h + 1) * 192], wq_)
                wkv_ = wkv.next()
                load_w_fm(kb, io.w_ukv, io.w_ukv[l, :, h * 256:(h + 1) * 256], wkv_)
                qn, qr, kn, vt, o_ = qnT.next(), qrT.next(), knT.next(), vtk.next(), oh.next()
                for n in range(4):
                    sl = slice(n * 512, (n + 1) * 512)
                    ps = pss.next()
                    for k in range(3):
                        mm(kb, ps, ps[:, :], wq_, wq_[:, k, 0:128], cqn, cqn[:, k, sl], start=(k == 0), stop=(k == 2))
                    cp(kb, "act", qn, qn[:, sl], ps, ps[:, :])
                    ps = pss.next()
                    for k in range(3):
                        mm(kb, ps, ps[0:64, :], wq_, wq_[:, k, 128:192], cqn, cqn[:, k, sl], start=(k == 0), stop=(k == 2))
                    rope_apply(kb, rwk, ps, ps[0:64, :], (cosb, cosb[:, sl]), (sinb, sinb[:, sl]), qr, qr[:, sl], 512)
                    ps = pss.next()
                    for k in range(2):
                        mm(kb, ps, ps[:, :], wkv_, wkv_[:, k, 0:128], ckvn, ckvn[:, k, sl], start=(k == 0), stop=(k == 1))
                    cp(kb, "dve", kn, kn[:, sl], ps, ps[:, :])
                    for t4 in range(4):
                        tkn = n * 4 + t4
                        for k in range(2):
                            mm(kb, psv, psv[:, t4, :], ckvn, ckvn[:, k, tkn * 128:(tkn + 1) * 128], wkv_, wkv_[:, k, 128:256],
                               start=(k == 0), stop=(k == 1))
                    cp(kb, "act", vt, vt[:, n * 4:(n + 1) * 4, :], psv, psv[:, :, :])
                for qb in range(4):
                    qs = slice(qb * 512, (qb + 1) * 512)
                    po = pso.next()
                    pd = psd.next()
                    prev = None
                    for kt in range(NCH + 1):
                        cur = None
                        if kt < NCH:
                            ks = slice(kt * 128, (kt + 1) * 128)
                            ps = pss.next()
                            mm(kb, ps, ps[:, :], kn, kn[:, ks], qn, qn[:, qs], start=True, stop=False)
                            mm(kb, ps, ps[:, :], krT, krT[:, ks], qr, qr[:, qs], start=False, stop=True)
                            cur = pex.next()
                            act(kb, cur, cur[:, :], ps, ps[:, :], AF.Exp, scale=scale)
                        if prev is not None:
                            k0 = kt - 1
                            mm(kb, po, po[:, :], vt, vt[:, k0, :], prev, prev[:, :], start=(k0 == 0), stop=(k0 == NCH - 1))
                            mm(kb, pd, pd[:, :], C.ones_b, C.ones_b[:, :], prev, prev[:, :], start=(k0 == 0), stop=(k0 == NCH - 1))
                        prev = cur
                    kb.op("dve", lambda e, pd=pd: e.reciprocal(rden[:, :], pd[:, :]), r=[pd], w=[rden])
                    tt(kb, "dve", o_, o_[:, qs], po, po[:, :], rden, rden[:, :], ALU.mult)
                db, dap = omT_dst(h)
                dma(kb, "sp", db, dap, o_, o_[:, :])
            kb.barrier()


def mixer_out(kb, C, io, l, b, load_hT, og_scr, om_scr, dbg):
    with contextlib.ExitStack() as st:
        yT = kb.sbuf(st, "yT", [128, 8, SEQ], BF16)
        with contextlib.ExitStack() as s2:
            hT_b = load_hT(s2)
            ogT = kb.sbuf(s2, "ogT", [128, 8, SEQ], BF16)
            omT = kb.sbuf(s2, "omT", [128, 8, SEQ], BF16)
            dma(kb, "sp", ogT, ogT[:, :, :], og_scr, og_scr[b, :, :, :])
            dma(kb, "sp", omT, omT[:, :, :], om_scr, om_scr[b, :, :, :])
            wr = [Rot([kb.sbuf(s2, "wo%d_%d" % (j, i), [128, 8, 128], BF16) for i in range(2)]) for j in range(4)]
            ps = [Rot([kb.psum(s2, "ps_y%d_%d" % (j, i), [128, 512], F32) for i in range(2)]) for j in range(4)]
            sg = Rot([kb.sbuf(s2, "sg%d" % i, [128, 512], F32) for i in range(4)])
            tq = Rot([kb.sbuf(s2, "tq%d" % i, [128, 512], F32) for i in range(4)])
            for m in range(8):
                ms = slice(m * 128, (m + 1) * 128)
                w4 = [r_.next() for r_ in wr]
                load_w_fm(kb, io.w_o_gdn, io.w_o_gdn[l, :, ms], w4[0])
                load_w_fm(kb, io.w_o_mla, io.w_o_mla[l, :, ms], w4[1])
                load_w_fm(kb, io.w_in, io.w_in[l, :, OFF_G + m * 128:OFF_G + (m + 1) * 128], w4[2])
                load_w_fm(kb, io.w_in, io.w_in[l, :, OFF_G + 1024 + m * 128:OFF_G + 1024 + (m + 1) * 128], w4[3])
                for n in range(4):
                    sl = slice(n * 512, (n + 1) * 512)
                    p4 = [r_.next() for r_ in ps]
                    for j, src in enumerate((ogT, omT, hT_b, hT_b)):
                        for k in range(8):
                            mm(kb, p4[j], p4[j][:, :], w4[j], w4[j][:, k, :], src, src[:, k, sl], start=(k == 0), stop=(k == 7))
                    s1, s2_ = sg.next(), sg.next()
                    act(kb, s1, s1[:, :], p4[2], p4[2][:, :], AF.Sigmoid)
                    act(kb, s2_, s2_[:, :], p4[3], p4[3][:, :], AF.Sigmoid)
                    t1, t2 = tq.next(), tq.next()
                    tt(kb, "dve", t1, t1[:, :], p4[0], p4[0][:, :], s1, s1[:, :], ALU.mult)
                    tt(kb, "dve", t2, t2[:, :], p4[1], p4[1][:, :], s2_, s2_[:, :], ALU.mult)
                    tt(kb, "pool", yT, yT[:, m, sl], t1, t1[:, :], t2, t2[:, :], ALU.add)
            kb.barrier()
        with contextlib.ExitStack() as s2:
            wo = kb.sbuf(s2, "w_out", [128, 8, D], BF16)
            load_w_fm(kb, io.w_out, io.w_out[l, :, :], wo)
            mT = kb.sbuf(s2, "mT", [128, 8, 512], F32)
            psm = Rot([kb.psum(s2, "ps_mo%d" % i, [128, 512], F32) for i in range(2)])
            pst = Rot([kb.psum(s2, "ps_mt%d" % i, [128, 4, 128], F32) for i in range(4)])
            hold = Rot([kb.sbuf(s2, "hold%d" % i, [128, D], F32) for i in range(2)])
            state = {"n": -1}

            def src(i, buf):
                ti = i - b * NCH
                n, t4 = ti // 4, ti % 4
                if n != state["n"]:
                    state["n"] = n
                    sl = slice(n * 512, (n + 1) * 512)
                    for m in range(8):
                        pm = psm.next()
                        for k in range(8):
                            mm(kb, pm, pm[:, :], wo, wo[:, k, m * 128:(m + 1) * 128], yT, yT[:, k, sl], start=(k == 0), stop=(k == 7))
                        cp(kb, "act", mT, mT[:, m, :], pm, pm[:, :])
                ho = hold.next()
                dma(kb, "pool", ho, ho[:, :], io.h_res[i], io.h_res[i][i * 128:(i + 1) * 128, :])
                for half in range(2):
                    pt = pst.next()
                    for mm_ in range(4):
                        m = half * 4 + mm_
                        tr(kb, pt, pt[:, mm_, :], mT, mT[:, m, t4 * 128:(t4 + 1) * 128], C.id_f, C.id_f[:, :])
                    stt(kb, "dve", buf, buf[:, half * 512:(half + 1) * 512], ho, ho[:, half * 512:(half + 1) * 512], float(DN_ALPHA),
                        pt, pt[:, :, :].rearrange("p a b -> p (a b)"), ALU.mult, ALU.add)
            emit_ln_rows(kb, C, s2, "l1_", src, (io.ln1_g, io.ln1_g[l:l + 1, :]), (io.ln1_b, io.ln1_b[l:l + 1, :]), io,
                         list(range(b * NCH, (b + 1) * NCH)), io.h_mid)
            kb.barrier()


def stage_mixer(kb, C, io, l, seqs, dbg, parts=("gdn", "mla", "out")):
    og_scr, om_scr = io.og_scr, io.om_scr
    with contextlib.ExitStack() as st0:
        cwT = kb.sbuf(st0, "cwT", [128, 24, 5], F32)
        with contextlib.ExitStack() as s2:
            cwr = kb.sbuf(s2, "cwr", [5, 3072], F32)
            dma(kb, "sp", cwr, cwr[:, :], io.conv_w, io.conv_w[l, :, :])
            pc = kb.psum(s2, "ps_cw", [128, 24, 8], F32)
            for t_ in range(24):
                tr(kb, pc, pc[:, t_, 0:5], cwr, cwr[0:5, t_ * 128:(t_ + 1) * 128], C.id_f, C.id_f[0:5, 0:5])
            cp(kb, "dve", cwT, cwT[:, :, :], pc, pc[:, :, 0:5])
            kb.barrier()
        for b in seqs:
            def load_hT(stk, b=b):
                hT_b = kb.sbuf(stk, "hT_b", [128, 8, SEQ], BF16)
                kb.dma("sp", lambda e: e.dma_start(out=hT_b[:, :, :], in_=io.hT[0][:, :, b * SEQ:(b + 1) * SEQ]),
                       r=[io.hT[i] for i in range(b * NCH, (b + 1) * NCH)], w=[hT_b])
                return hT_b
            if "gdn" in parts:
                gdn_seq(kb, C, io, l, b, load_hT, og_scr, cwT, dbg)
            if "mla" in parts:
                mla_seq(kb, C, io, l, b, load_hT, lambda h, b=b: (om_scr, om_scr[b, :, h, :]), dbg)
            if "out" in parts:
                mixer_out(kb, C, io, l, b, load_hT, og_scr, om_scr, dbg)


def idma(kb, ob, oap, out_off, ib, iap, in_off, extra_r=(), nrows=None):
    if not hasattr(kb, "bc_regs"):
        kb.bc_regs = {}
    if nrows not in kb.bc_regs:
        kb.bc_regs[nrows] = kb.nc.gpsimd.to_reg(nrows - 1)
    bc = kb.bc_regs[nrows]

    def fn(e):
        return e.indirect_dma_start(
            out=oap, out_offset=(bass.IndirectOffsetOnAxis(ap=out_off, axis=0) if out_off is not None else None),
            in_=iap, in_offset=(bass.IndirectOffsetOnAxis(ap=in_off, axis=0) if in_off is not None else None),
            bounds_check=bc, oob_is_err=False)
    return kb.dma("pool", fn, r=[ib] + list(extra_r), w=[ob])


def stage_moe(kb, C, io, l, dst_tiles, dbg):
    NEG = -1.0e30
    with contextlib.ExitStack() as st:
        ohE = kb.sbuf(st, "ohE", [128, NT, 2, NE], F32)
        gate = kb.sbuf(st, "gate", [128, NT, 2], F32)
        destI = kb.sbuf(st, "destI", [128, NT, 2], I32)
        BEi = kb.sbuf(st, "BEi", [128, NBLK], I32)
        offs_gu = kb.sbuf(st, "offs_gu", [128, NBLK, 8], I32)
        offs_d = kb.sbuf(st, "offs_d", [128, NBLK, 4], I32)
        with contextlib.ExitStack() as s2:
            kb.serialize = bool(dbg.get("serial_moe", False))
            wr = kb.sbuf(s2, "wr", [128, 8, 72], F32)
            dma(kb, "sp", wr, wr[:, :, 0:8], io.w_router_group, io.w_router_group[l, :, :].rearrange("(k p) g -> p k g", p=128))
            dma(kb, "sp", wr, wr[:, :, 8:72], io.w_router_expert, io.w_router_expert[l, :, :].rearrange("(k p) g -> p k g", p=128))
            br = kb.sbuf(s2, "br", [128, 72], F32)
            dma(kb, "sp", br, br[:, 0:8], io.b_router_group, io.b_router_group[l:l + 1, :].partition_broadcast(128))
            dma(kb, "sp", br, br[:, 8:72], io.b_router_expert, io.b_router_expert[l:l + 1, :].partition_broadcast(128))
            xt = Rot([kb.sbuf(s2, "mx%d" % i, [128, D], F32) for i in range(2)])
            hTf = kb.sbuf(s2, "hTf", [128, 8, 128], F32)
            pst = Rot([kb.psum(s2, "ps_rt%d" % i, [128, 4, 128], F32) for i in range(2)])
            psl = kb.psum(s2, "ps_rl", [128, 72], F32)
            lgall = kb.sbuf(s2, "lgall", [128, NT, 72], F32)
            for i in range(NT):
                x = xt.next()
                dma(kb, "sp", x, x[:, :], io.h_mid[i], io.h_mid[i][i * 128:(i + 1) * 128, :])
                for half in range(2):
                    pt = pst.next()
                    for c4 in range(4):
                        c = half * 4 + c4
                        tr(kb, pt, pt[:, c4, :], x, x[:, c * 128:(c + 1) * 128], C.id_f, C.id_f[:, :], inc=(c4 == 3))
                    cp(kb, "act", hTf, hTf[:, half * 4:(half + 1) * 4, :], pt, pt[:, :, :])
                for k in range(8):
                    mm(kb, psl, psl[:, :], hTf, hTf[:, k, :], wr, wr[:, k, :], start=(k == 0), stop=(k == 7))
                tt(kb, "dve", lgall, lgall[:, i, :], psl, psl[:, :], br, br[:, :], ALU.add)
            A3 = lambda nm, n: kb.sbuf(s2, "rb_" + nm, [128, NT, n], F32)
            A2 = lambda nm: kb.sbuf(s2, "rb_" + nm, [128, NT], F32)
            LG = lgall[:, :, 0:8]
            LE4 = lgall[:, :, 8:72].rearrange("p t (g e) -> p t g e", g=8)
            bc8 = lambda buf: buf[:, :].unsqueeze(2).to_broadcast([128, NT, 8])
            gmax, se, pg, m1, m2, r_, dd = A2("gmax"), A2("se"), A2("pg"), A2("m1"), A2("m2"), A2("r"), A2("dd")
            ohg, eg, les, oh1, le2, oh2 = A3("ohg", 8), A3("eg", 8), A3("les", 8), A3("oh1", 8), A3("le2", 8), A3("oh2", 8)
            t64 = A3("t64", 64)
            t64v = t64[:, :, :].rearrange("p t (g e) -> p t g e", g=8)
            kb.op("dve", lambda e: e.reduce_max(out=gmax[:, :], in_=LG, axis=AX.X), r=[lgall], w=[gmax])
            tt(kb, "dve", ohg, ohg[:, :, :], lgall, LG, gmax, bc8(gmax), ALU.is_equal)
            tt(kb, "dve", eg, eg[:, :, :], lgall, LG, gmax, bc8(gmax), ALU.subtract)
            act(kb, eg, eg[:, :, :], eg, eg[:, :, :], AF.Exp)
            kb.op("dve", lambda e: e.reduce_sum(out=se[:, :], in_=eg[:, :, :], axis=AX.X), r=[eg], w=[se])
            kb.op("dve", lambda e: e.reciprocal(pg[:, :], se[:, :]), r=[se], w=[pg])
            tt(kb, "dve", t64, t64v, lgall, LE4, ohg, ohg[:, :, :].unsqueeze(3).to_broadcast([128, NT, 8, 8]), ALU.mult)
            kb.op("dve", lambda e: e.reduce_sum(out=les[:, :, :], in_=t64[:, :, :].rearrange("p t (g e) -> p t e g", g=8), axis=AX.X),
                  r=[t64], w=[les])
            kb.op("dve", lambda e: e.reduce_max(out=m1[:, :], in_=les[:, :, :], axis=AX.X), r=[les], w=[m1])
            tt(kb, "dve", oh1, oh1[:, :, :], les, les[:, :, :], m1, bc8(m1), ALU.is_equal)
            stt(kb, "dve", le2, le2[:, :, :], oh1, oh1[:, :, :], NEG, les, les[:, :, :], ALU.mult, ALU.add)
            kb.op("dve", lambda e: e.reduce_max(out=m2[:, :], in_=le2[:, :, :], axis=AX.X), r=[le2], w=[m2])
            tt(kb, "dve", oh2, oh2[:, :, :], le2, le2[:, :, :], m2, bc8(m2), ALU.is_equal)
            tt(kb, "dve", r_, r_[:, :], m2, m2[:, :], m1, m1[:, :], ALU.subtract)
            act(kb, r_, r_[:, :], r_, r_[:, :], AF.Exp)
            tsc(kb, "dve", dd, dd[:, :], r_, r_[:, :], 1.0, None, ALU.add)
            kb.op("dve", lambda e: e.reciprocal(dd[:, :], dd[:, :]), r=[dd], w=[dd])
            tt(kb, "dve", dd, dd[:, :], dd, dd[:, :], pg, pg[:, :], ALU.mult)
            cp(kb, "dve", gate, gate[:, :, 0], dd, dd[:, :])
            tt(kb, "dve", gate, gate[:, :, 1], dd, dd[:, :], r_, r_[:, :], ALU.mult)
            for k_, oh in ((0, oh1), (1, oh2)):
                tt(kb, "dve", ohE, ohE[:, :, k_, :].rearrange("p t (g e) -> p t g e", g=8), ohg,
                   ohg[:, :, :].unsqueeze(3).to_broadcast([128, NT, 8, 8]), oh, oh[:, :, :].unsqueeze(2).to_broadcast([128, NT, 8, 8]), ALU.mult)
            kb.barrier()
        with contextlib.ExitStack() as s2:
            ohs = kb.sbuf(s2, "ohs", [128, NT, NE], F32)
            cum = kb.sbuf(s2, "cum", [128, NT + 1, NE], F32)
            tt(kb, "dve", ohs, ohs[:, :, :], ohE, ohE[:, :, 0, :], ohE, ohE[:, :, 1, :], ALU.add)
            memset(kb, "dve", cum, cum[:, 0, :], 0.0)
            for i in range(NT):
                tt(kb, "dve", cum, cum[:, i + 1, :], cum, cum[:, i, :], ohs, ohs[:, i, :], ALU.add)
            striU = kb.sbuf(s2, "striU", [128, 128], F32)
            tt(kb, "dve", striU, striU[:, :], C.triF, C.triF[:, :], C.id_f, C.id_f[:, :], ALU.subtract)
            pc = kb.psum(s2, "ps_cnt", [64, 128], F32)
            for i in range(NT):
                mm(kb, pc, pc[:, :], ohs, ohs[:, i, :], C.ones_f, C.ones_f[:, :], start=(i == 0), stop=(i == NT - 1))
            cntT = kb.sbuf(s2, "cntT", [64, 128], F32)
            tsc(kb, "dve", cntT, cntT[:, :], pc, pc[:, :], 127.0, None, ALU.add)
            ci = kb.sbuf(s2, "cnt_i", [64, 128], I32)
            cp(kb, "dve", ci, ci[:, :], cntT, cntT[:, :])
            tsc(kb, "dve", ci, ci[:, :], ci, ci[:, :], 7, None, ALU.arith_shift_right)
            tsc(kb, "dve", ci, ci[:, :], ci, ci[:, :], 7, None, ALU.logical_shift_left)
            padT = kb.sbuf(s2, "padT", [64, 128], F32)
            cp(kb, "dve", padT, padT[:, :], ci, ci[:, :])
            pps = kb.psum(s2, "ps_pst", [128, 64], F32)
            mm(kb, pps, pps[:, :], padT, padT[:, :], striU, striU[0:64, 0:64])
            pstart = kb.sbuf(s2, "pstart", [128, NE], F32)
            cp(kb, "dve", pstart, pstart[:, :], pps, pps[:, :])
            ppe = kb.psum(s2, "ps_pend", [64, 128], F32)
            mm(kb, ppe, ppe[:, :], C.triF, C.triF[0:64, 0:64], padT, padT[:, :])
            jrow_i = kb.sbuf(s2, "jrow_i", [64, 128], I32)
            kb.op("pool", lambda e: e.iota(jrow_i[:, :], pattern=[[128, 128]], base=0, channel_multiplier=0), r=[], w=[jrow_i])
            jrow = kb.sbuf(s2, "jrow", [64, 128], F32)
            cp(kb, "dve", jrow, jrow[:, :], jrow_i, jrow_i[:, :])
            cmpm = kb.sbuf(s2, "cmpm", [64, 128], F32)
            tt(kb, "dve", cmpm, cmpm[:, :], ppe, ppe[:, :], jrow, jrow[:, :], ALU.is_le)
            pbe = kb.psum(s2, "ps_be", [128, 128], F32)
            mm(kb, pbe, pbe[:, :], C.ones_f, C.ones_f[0:64, :], cmpm, cmpm[:, :])
            bef = kb.sbuf(s2, "bef", [128, NBLK], F32)
            unused = kb.sbuf(s2, "unused", [128, NBLK], F32)
            tsc(kb, "dve", unused, unused[:, :], pbe, pbe[:, :], 63.5, 4194304.0, ALU.is_gt, ALU.mult)
            tsc(kb, "dve", bef, bef[:, :], pbe, pbe[:, :], 63.0, None, ALU.min)
            cp(kb, "dve", BEi, BEi[:, :], bef, bef[:, :])
            prow_i = kb.sbuf(s2, "prow_i", [128, 8], I32)
            kb.op("pool", lambda e: e.iota(prow_i[:, :], pattern=[[128, 8]], base=0, channel_multiplier=1), r=[], w=[prow_i])
            prow = kb.sbuf(s2, "prow", [128, 8], F32)
            cp(kb, "dve", prow, prow[:, :], prow_i, prow_i[:, :])
            of = kb.sbuf(s2, "off_f", [128, NBLK, 8], F32)
            stt(kb, "dve", of, of[:, :, :], bef, bef[:, :].unsqueeze(2).to_broadcast([128, NBLK, 8]), 128.0, prow,
                prow[:, 0:1].unsqueeze(1).to_broadcast([128, NBLK, 8]), ALU.mult, ALU.add)
            tsc(kb, "dve", of, of[:, :, :], of, of[:, :, :], float(l * NE * 128), None, ALU.add)
            tt(kb, "dve", of, of[:, :, :], of, of[:, :, :], unused, unused[:, :].unsqueeze(2).to_broadcast([128, NBLK, 8]), ALU.add)
            cp(kb, "dve", offs_gu, offs_gu[:, :, :], of, of[:, :, :])
            stt(kb, "dve", of, of[:, :, 0:4], bef, bef[:, :].unsqueeze(2).to_broadcast([128, NBLK, 4]), 512.0, prow,
                prow[:, 0:4].unsqueeze(1).to_broadcast([128, NBLK, 4]), ALU.mult, ALU.add)
            tsc(kb, "dve", of, of[:, :, 0:4], of, of[:, :, 0:4], float(l * NE * DEXP), None, ALU.add)
            tt(kb, "dve", of, of[:, :, 0:4], of, of[:, :, 0:4], unused, unused[:, :].unsqueeze(2).to_broadcast([128, NBLK, 4]), ALU.add)
            cp(kb, "dve", offs_d, offs_d[:, :, :], of, of[:, :, 0:4])
            ppf = Rot([kb.psum(s2, "ps_pf%d" % i, [128, 64], F32) for i in range(2)])
            tq = kb.sbuf(s2, "tq64", [128, NE], F32)
            tq2 = kb.sbuf(s2, "tq64b", [128, NE], F32)
            dsf = kb.sbuf(s2, "dsf", [128, NT, 2], F32)
            for i in range(NT):
                pp = ppf.next()
                mm(kb, pp, pp[:, :], striU, striU[:, :], ohs, ohs[:, i, :], start=True, stop=False)
                mm(kb, pp, pp[:, :], C.ones_f, C.ones_f[:, :], cum, cum[:, i, :], start=False, stop=True)
                tt(kb, "dve", tq, tq[:, :], pp, pp[:, :], pstart, pstart[:, :], ALU.add)
                for k_ in range(2):
                    tt(kb, "dve", tq2, tq2[:, :], tq, tq[:, :], ohE, ohE[:, i, k_, :], ALU.mult)
                    kb.op("dve", lambda e, i=i, k_=k_: e.reduce_sum(out=dsf[:, i, k_:k_ + 1], in_=tq2[:, :], axis=AX.X), r=[tq2], w=[dsf])
            cp(kb, "dve", destI, destI[:, :, :], dsf, dsf[:, :, :])
            kb.barrier()
        kb.serialize = False
        xb = io.xb
        with contextlib.ExitStack() as s2:
            zt = kb.sbuf(s2, "zt", [128, 8, D], F32)
            memset(kb, "dve", zt, zt[:, :, :], 0.0)
            for j in range(NROWS // 1024):
                kb.dma("sp", lambda e, j=j: e.dma_start(out=xb[j * 1024:(j + 1) * 1024, :].rearrange("(a p) d -> p a d", p=128),
                                                        in_=zt[:, :, :]), r=[zt], w=[xb])
            xt = Rot([kb.sbuf(s2, "sx%d" % i, [128, D], F32) for i in range(3)])
            for i in range(NT):
                x = xt.next()
                dma(kb, "sp", x, x[:, :], io.h_mid[i], io.h_mid[i][i * 128:(i + 1) * 128, :])
                for k_ in range(2):
                    idma(kb, xb, xb[:, :], destI[:, i, k_:k_ + 1], x, x[:, :], None, extra_r=[destI], nrows=NROWS)
            kb.barrier()
        yb = io.yb
        wg_v = io.w_gate[:, :, :, :].rearrange("l e (p c) n -> (l e p) (c n)", c=8)
        wu_v = io.w_up[:, :, :, :].rearrange("l e (p c) n -> (l e p) (c n)", c=8)
        wd_v = io.w_down[:, :, :, :].rearrange("l e (p c) n -> (l e p) (c n)", c=4)
        with contextlib.ExitStack() as s2:
            WDT = BF16
            wg = Rot([kb.sbuf(s2, "wg%d" % i, [128, 8, DEXP], WDT) for i in range(3)])
            wu = Rot([kb.sbuf(s2, "wu%d" % i, [128, 8, DEXP], WDT) for i in range(3)])
            wd = Rot([kb.sbuf(s2, "wd%d" % i, [128, 4, D], WDT) for i in range(3)])
            xr = Rot([kb.sbuf(s2, "xr%d" % i, [128, D], F32) for i in range(2)])
            xrb = Rot([kb.sbuf(s2, "xrb%d" % i, [128, D], BF16) for i in range(2)])
            xT = Rot([kb.sbuf(s2, "xT%d" % i, [128, 8, 128], BF16) for i in range(2)])
            hid = Rot([kb.sbuf(s2, "hid%d" % i, [128, 4, 128], BF16) for i in range(2)])
            sg_ = Rot([kb.sbuf(s2, "sgx%d" % i, [128, 4, 128], F32) for i in range(2)])
            yo = Rot([kb.sbuf(s2, "yo%d" % i, [128, D], F32) for i in range(2)])
            pst = Rot([kb.psum(s2, "ps_xt%d" % i, [128, 8, 128], BF16) for i in range(2)])
            psg = Rot([kb.psum(s2, "ps_g%d" % i, [128, 4, 128], F32) for i in range(2)])
            psu = Rot([kb.psum(s2, "ps_u%d" % i, [128, 4, 128], F32) for i in range(2)])
            psy = Rot([kb.psum(s2, "ps_yy%d" % i, [128, 512], F32) for i in range(2)])
            for j in range(dbg.get("nblk", NBLK)):
                wg_, wu_, wd_ = wg.next(), wu.next(), wd.next()
                NR = DEPTH * NE * 128
                idma(kb, wg_, wg_[:, :, :].rearrange("p c n -> p (c n)"), None, io.w_gate, wg_v, offs_gu[:, j, 0:1], extra_r=[offs_gu], nrows=NR)
                idma(kb, wu_, wu_[:, :, :].rearrange("p c n -> p (c n)"), None, io.w_up, wu_v, offs_gu[:, j, 0:1], extra_r=[offs_gu], nrows=NR)
                idma(kb, wd_, wd_[:, :, :].rearrange("p c n -> p (c n)"), None, io.w_down, wd_v, offs_gu[:, j, 0:1], extra_r=[offs_gu], nrows=NR)
                x = xr.next()
                dma(kb, "pool", x, x[:, :], xb, xb[j * 128:(j + 1) * 128, :])
                xb_ = xrb.next()
                cp(kb, "dve", xb_, xb_[:, :], x, x[:, :])
                xT_ = xT.next()
                pt = pst.next()
                xperm = xb_[:, :].rearrange("r (p c) -> r c p", c=8)
                for c in range(8):
                    tr(kb, pt, pt[:, c, :], xb_, xperm[:, c, :], C.id_b, C.id_b[:, :], inc=(c == 7))
                cp(kb, "act", xT_, xT_[:, :, :], pt, pt[:, :, :])
                pg, pu = psg.next(), psu.next()
                for m in range(4):
                    for c in range(8):
                        mm(kb, pg, pg[:, m, :], wg_, wg_[:, c, :].rearrange("k (p m) -> k m p", m=4)[:, m, :], xT_, xT_[:, c, :],
                           start=(c == 0), stop=(c == 7), inc=(c == 7 and m == 3))
                for m in range(4):
                    for c in range(8):
                        mm(kb, pu, pu[:, m, :], wu_, wu_[:, c, :].rearrange("k (p m) -> k m p", m=4)[:, m, :], xT_, xT_[:, c, :],
                           start=(c == 0), stop=(c == 7), inc=(c == 7 and m == 3))
                s_ = sg_.next()
                act(kb, s_, s_[:, :, :], pg, pg[:, :, :], AF.Silu)
                h_ = hid.next()
                tt(kb, "dve", h_, h_[:, :, :], s_, s_[:, :, :], pu, pu[:, :, :], ALU.mult)
                y_ = yo.next()
                for nh in range(2):
                    py = psy.next()
                    for c in range(4):
                        mm(kb, py, py[:, :], h_, h_[:, c, :], wd_, wd_[:, c, nh * 512:(nh + 1) * 512], start=(c == 0), stop=(c == 3))
                    cp(kb, "act", y_, y_[:, nh * 512:(nh + 1) * 512], py, py[:, :])
                dma(kb, "sp", yb, yb[j * 128:(j + 1) * 128, :], y_, y_[:, :])
            kb.barrier()
        with contextlib.ExitStack() as s2:
            y0 = Rot([kb.sbuf(s2, "gy0_%d" % i, [128, D], F32) for i in range(4)])
            y1 = Rot([kb.sbuf(s2, "gy1_%d" % i, [128, D], F32) for i in range(4)])
            hm = Rot([kb.sbuf(s2, "ghm%d" % i, [128, D], F32) for i in range(4)])

            def src(i, buf):
                a, b_, h_ = y0.next(), y1.next(), hm.next()
                idma(kb, a, a[:, :], None, yb, yb[:, :], destI[:, i, 0:1], extra_r=[destI], nrows=NROWS)
                idma(kb, b_, b_[:, :], None, yb, yb[:, :], destI[:, i, 1:2], extra_r=[destI], nrows=NROWS)
                dma(kb, "pool", h_, h_[:, :], io.h_mid[i], io.h_mid[i][i * 128:(i + 1) * 128, :])
                tsc(kb, "dve", a, a[:, :], a, a[:, :], gate[:, i, 0:1], None, ALU.mult, extra_r=[gate])
                stt(kb, "dve", a, a[:, :], b_, b_[:, :], gate[:, i, 1:2], a, a[:, :], ALU.mult, ALU.add, extra_r=[gate])
                stt(kb, "dve", buf, buf[:, :], h_, h_[:, :], float(DN_ALPHA), a, a[:, :], ALU.mult, ALU.add)
            emit_ln_rows(kb, C, s2, "l2_", src, (io.ln2_g, io.ln2_g[l:l + 1, :]), (io.ln2_b, io.ln2_b[l:l + 1, :]), io,
                         list(range(NT)), dst_tiles, nb=4)
            kb.barrier()


def build_program(dbg=None):
    dbg = dict(dbg or {})
    dbg.setdefault("serial", 0)
    kb = KB()
    io = declare_io(kb)
    with contextlib.ExitStack() as st:
        C = setup_consts(kb, st)
        stage_prologue(kb, C, io, list(range(NT)))
        stage_rope_tables(kb, C, io)
        depth = dbg.get("depth", DEPTH)
        for l in range(depth):
            stage_mixer(kb, C, io, l, list(range(NSEQ)), dbg)
            stage_moe(kb, C, io, l, io.h_res if l < depth - 1 else io.out, dbg)
        kb.finish()
    return kb


def kernel(**inputs):
    n = 8
    kb = build_program()
    in_maps = [make_in_map(inputs, c) for c in range(n)]
    res = run_bass_kernel_spmd(kb.nc, in_maps, core_ids=list(range(n)))
    out = np.concatenate([np.asarray(r["out"], dtype=np.float32).reshape(NSEQ, SEQ, D) for r in res.results], axis=0)
    return out
```

```python
import contextlib
import numpy as np
import concourse.bass as bass
import concourse.mybir as mybir
from concourse.bass_utils import run_bass_kernel_spmd

F32 = mybir.dt.float32
BF16 = mybir.dt.bfloat16
I32 = mybir.dt.int32
U32 = mybir.dt.uint32
AF = mybir.ActivationFunctionType
ALU = mybir.AluOpType
AX = mybir.AxisListType

D = 1024
SEQ = 2048
NSEQ = 2
T = NSEQ * SEQ
NT = T // 128
DEPTH = 2
H = 8
IN_DIM = 6880
OFF_Q, OFF_K, OFF_V, OFF_Z, OFF_A, OFF_BT, OFF_CQ, OFF_CKV, OFF_KR, OFF_G = (
    0, 1024, 2048, 3072, 4096, 4112, 4128, 4512, 4768, 4832)
QL, KVL, ROPE = 384, 256, 64
NE = 64
DEXP = 512
NBLK = 128
NROWS = NBLK * 128
DN_ALPHA = (2 * DEPTH) ** 0.25
LN_EPS = 1e-5
RMS_EPS = 1e-6
CH = 128
NCH = SEQ // CH


class Buf:
    def __init__(self, t, name):
        self.t = t
        self.name = name
        self.w = None
        self.r = {}

    def __getitem__(self, idx):
        return self.t[idx]


class KB:
    ENG = ("pe", "act", "dve", "pool", "sp")

    def __init__(self):
        self.nc = bass.Bass("TRN2", target_bir_lowering=False)
        nc = self.nc
        self.es = contextlib.ExitStack()
        self.eng = {"pe": nc.tensor, "act": nc.scalar, "dve": nc.vector, "pool": nc.gpsimd, "sp": nc.sync}
        self.sems = {}
        self.cnt = {}
        self.known = {e: {} for e in self.ENG}
        for e in self.ENG:
            self.sems[e] = self.es.enter_context(nc.semaphore("sem_" + e))
            self.cnt[e] = 0
        self.dq = {}
        for q, n in (("sp", 12), ("pool", 28), ("act", 2)):
            keys = []
            for i in range(n):
                k = "d_%s_%d" % (q, i)
                self.sems[k] = self.es.enter_context(nc.semaphore(k))
                self.cnt[k] = 0
                keys.append(k)
            self.dq[q] = [keys, 0]
        self.ninst = 0
        self.uid = 0
        self.scr = None
        self.serialize = False
        self.psum_rar = True
        self.chain = False
        self.chain_engines = ("pe", "act", "dve", "pool")
        self.last_op = None
        self.flush_nop = False

    def sbuf(self, stack, name, shape, dt):
        self.uid += 1
        name = "%s_u%d" % (name, self.uid)
        return Buf(stack.enter_context(self.nc.sbuf_tensor(name, list(shape), dt)), name)

    def psum(self, stack, name, shape, dt):
        self.uid += 1
        name = "%s_u%d" % (name, self.uid)
        b = Buf(stack.enter_context(self.nc.psum_tensor(name, list(shape), dt)), name)
        b.is_psum = True
        return b

    def dram(self, name, shape, dt, kind="Internal"):
        return Buf(self.nc.dram_tensor(name, list(shape), dt, kind=kind).ap(), name)

    def dram_tiles(self, name, shape, dt, n, kind="Internal"):
        ap = self.nc.dram_tensor(name, list(shape), dt, kind=kind).ap()
        return [Buf(ap, "%s_%d" % (name, i)) for i in range(n)]

    def _waits(self, en, r, w, is_dma=False):
        need = {}
        for b in r:
            if b.w is not None:
                need[b.w[0]] = max(need.get(b.w[0], 0), b.w[1])
            if self.psum_rar and getattr(b, "is_psum", False) and en != "pe":
                for sk, val in b.r.items():
                    if sk != en:
                        need[sk] = max(need.get(sk, 0), val)
        for b in w:
            if b.w is not None and (is_dma or b.w[0] != en):
                need[b.w[0]] = max(need.get(b.w[0], 0), b.w[1])
            for sk, val in b.r.items():
                if is_dma or sk != en:
                    need[sk] = max(need.get(sk, 0), val)
        E = self.eng[en]
        kn = self.known[en]
        for sk, val in need.items():
            if kn.get(sk, 0) < val:
                E.wait_ge(self.sems[sk], val)
                kn[sk] = val
                self.ninst += 1

    def _done(self, tok, r, w):
        for b in r:
            b.r[tok[0]] = max(b.r.get(tok[0], 0), tok[1])
        for b in w:
            b.w = tok
            b.r = {}

    def op(self, en, fn, r=(), w=(), inc=True):
        self._waits(en, r, w)
        if self.chain and en in self.chain_engines and self.last_op is not None and self.last_op[0] != en:
            lk, lv = self.last_op
            if self.known[en].get(lk, 0) < lv:
                self.eng[en].wait_ge(self.sems[lk], lv)
                self.known[en][lk] = lv
        ins = fn(self.eng[en])
        self.ninst += 1
        if inc:
            self.cnt[en] += 1
            ins.then_inc(self.sems[en], 1)
            self._done((en, self.cnt[en]), r, w)
        else:
            self._done((en, self.cnt[en] + 1), r, w)
        if en in self.chain_engines:
            self.last_op = (en, self.cnt[en] + (0 if inc else 1))
        if self.flush_nop and self.scr is not None and en in ("dve", "act") and any(getattr(b, "is_psum", False) for b in r):
            if en == "dve":
                self.eng[en].memset(self.scr[0:1, 0:1], 0.0)
            else:
                self.eng[en].memzero(self.scr[0:1, 2:3])
            self.ninst += 1
        if self.serialize and inc:
            self.barrier()
        return ins

    def dma(self, q, fn, r=(), w=()):
        keys, i = self.dq[q]
        k = keys[i % len(keys)]
        self.dq[q][1] = i + 1
        self._waits(q, r, w, True)
        kn = self.known[q]
        if kn.get(k, 0) < self.cnt[k]:
            self.eng[q].wait_ge(self.sems[k], self.cnt[k])
            kn[k] = self.cnt[k]
        ins = fn(self.eng[q])
        self.cnt[k] += 16
        ins.then_inc(self.sems[k], 16)
        self.ninst += 1
        self._done((k, self.cnt[k]), r, w)
        return ins

    def barrier(self):
        for en in self.ENG:
            kn = self.known[en]
            for sk, val in self.cnt.items():
                if sk != en and val > 0 and kn.get(sk, 0) < val:
                    self.eng[en].wait_ge(self.sems[sk], val)
                    kn[sk] = val

    def finish(self):
        kn = self.known["sp"]
        for sk, val in self.cnt.items():
            if sk != "sp" and val > 0 and kn.get(sk, 0) < val:
                self.nc.sync.wait_ge(self.sems[sk], val)
                kn[sk] = val
        self.es.close()


def mm(kb, ob, oap, lb, lap, rb, rap, start=True, stop=True, inc=None):
    if inc is None:
        inc = stop
    return kb.op("pe", lambda e: e.matmul(oap, lhsT=lap, rhs=rap, start=start, stop=stop), r=[lb, rb], w=[ob], inc=inc)


def tr(kb, ob, oap, ib, iap, idb, idap, inc=True):
    return kb.op("pe", lambda e: e.transpose(oap, iap, idap), r=[ib, idb], w=[ob], inc=inc)


def act(kb, ob, oap, ib, iap, func, bias=None, scale=None, extra_r=(), accum=None, en="act"):
    kw = {}
    if bias is not None:
        kw["bias"] = bias
    if scale is not None:
        kw["scale"] = scale
    w = [ob]
    if accum is not None:
        kw["accum_out"] = accum[1]
        w.append(accum[0])
    return kb.op("act", lambda e: e.activation(out=oap, in_=iap, func=func, **kw), r=[ib] + list(extra_r), w=w)


def tt(kb, en, ob, oap, ab, aap, bb, bap, op):
    return kb.op(en, lambda e: e.tensor_tensor(out=oap, in0=aap, in1=bap, op=op), r=[ab, bb], w=[ob])


def tsc(kb, en, ob, oap, ib, iap, s1, s2, op0, op1=None, extra_r=()):
    if op1 is None:
        return kb.op(en, lambda e: e.tensor_scalar(out=oap, in0=iap, scalar1=s1, scalar2=None, op0=op0),
                     r=[ib] + list(extra_r), w=[ob])
    return kb.op(en, lambda e: e.tensor_scalar(out=oap, in0=iap, scalar1=s1, scalar2=s2, op0=op0, op1=op1),
                 r=[ib] + list(extra_r), w=[ob])


def stt(kb, en, ob, oap, ab, aap, scalar, bb, bap, op0, op1, extra_r=()):
    return kb.op(en, lambda e: e.scalar_tensor_tensor(out=oap, in0=aap, scalar=scalar, in1=bap, op0=op0, op1=op1),
                 r=[ab, bb] + list(extra_r), w=[ob])


def cp(kb, en, ob, oap, ib, iap):
    if en == "act":
        return kb.op("act", lambda e: e.copy(oap, iap), r=[ib], w=[ob])
    return kb.op(en, lambda e: e.tensor_copy(oap, iap), r=[ib], w=[ob])


def memset(kb, en, ob, oap, val):
    return kb.op(en, lambda e: e.memset(oap, val), r=[], w=[ob])


def dma(kb, q, ob, oap, ib, iap):
    return kb.dma(q, lambda e: e.dma_start(out=oap, in_=iap), r=[ib], w=[ob])


class Consts:
    pass


def rsqrt(kb, C, ob, oap, ib, iap, eps):
    act(kb, ob, oap, ib, iap, AF.Sqrt, bias=C.eps_tile(eps)[0:oap.shape[0], 0:1], extra_r=[C.eps_buf])
    kb.op("dve", lambda e: e.reciprocal(oap, oap), r=[ob], w=[ob])


def setup_consts(kb, st):
    c = Consts()
    kb.scr = kb.sbuf(st, "kb_scr", [128, 8], F32).t
    c.ones_f = kb.sbuf(st, "ones_f", [128, 128], F32)
    c.ones_b = kb.sbuf(st, "ones_b", [128, 128], BF16)
    c.id_f = kb.sbuf(st, "id_f", [128, 128], F32)
    c.id_b = kb.sbuf(st, "id_b", [128, 128], BF16)
    c.id4_f = kb.sbuf(st, "id4_f", [128, 4, 128], F32)
    c.triF = kb.sbuf(st, "triF", [128, 128], F32)
    c.triB = kb.sbuf(st, "triB", [128, 128], F32)
    c.fill_zero = kb.nc.gpsimd.to_reg(0.0)
    c.fill_neg = kb.nc.gpsimd.to_reg(-30000.0)
    c.fill_pos = kb.nc.gpsimd.to_reg(30000.0)
    c.eps_buf = kb.sbuf(st, "eps_c", [128, 2], F32)
    memset(kb, "pool", c.eps_buf, c.eps_buf[:, 0:1], LN_EPS)
    memset(kb, "pool", c.eps_buf, c.eps_buf[:, 1:2], RMS_EPS)
    c.eps_tile = lambda eps: c.eps_buf[:, 0:1] if eps == LN_EPS else c.eps_buf[:, 1:2]
    memset(kb, "pool", c.ones_f, c.ones_f[:], 1.0)
    memset(kb, "pool", c.ones_b, c.ones_b[:], 1.0)
    kb.op("pool", lambda e: e.affine_select(out=c.id_f[:], in_=c.ones_f[:], pattern=[[-1, 128]],
                                            compare_op=ALU.is_equal, fill=c.fill_zero, base=0, channel_multiplier=1),
          r=[c.ones_f], w=[c.id_f])
    cp(kb, "pool", c.id_b, c.id_b[:], c.id_f, c.id_f[:])
    for i in range(4):
        cp(kb, "pool", c.id4_f, c.id4_f[:, i, :], c.id_f, c.id_f[:])
    kb.op("pool", lambda e: e.affine_select(out=c.triF[:], in_=c.ones_f[:], pattern=[[1, 128]],
                                            compare_op=ALU.is_ge, fill=c.fill_zero, base=0, channel_multiplier=-1),
          r=[c.ones_f], w=[c.triF])
    kb.op("pool", lambda e: e.affine_select(out=c.triB[:], in_=c.ones_f[:], pattern=[[-1, 128]],
                                            compare_op=ALU.is_ge, fill=c.fill_zero, base=0, channel_multiplier=1),
          r=[c.ones_f], w=[c.triB])
    zero4 = kb.sbuf(st, "zero4", [128, 4, 128], F32)
    memset(kb, "pool", zero4, zero4[:, :, :], 0.0)
    c.mT, c.mS = [], []
    for d in range(2):
        if d == 0:
            patT, cmT, pat, cm = [[0, 4], [1, 128]], -1, [[0, 4], [-1, 128]], 1
        else:
            patT, cmT, pat, cm = [[0, 4], [-1, 128]], 1, [[0, 4], [1, 128]], -1
        mT = kb.sbuf(st, "mT%d" % d, [128, 4, 128], F32)
        mS = kb.sbuf(st, "mS%d" % d, [128, 4, 128], F32)
        kb.op("pool", lambda e, mT=mT, patT=patT, cmT=cmT: e.affine_select(
            out=mT[:, :, :], in_=zero4[:, :, :], pattern=patT, compare_op=ALU.is_ge, fill=c.fill_neg, base=0,
            channel_multiplier=cmT), r=[zero4], w=[mT])
        kb.op("pool", lambda e, mS=mS, pat=pat, cm=cm: e.affine_select(
            out=mS[:, :, :], in_=zero4[:, :, :], pattern=pat, compare_op=ALU.is_ge, fill=c.fill_pos, base=-1,
            channel_multiplier=cm), r=[zero4], w=[mS])
        c.mT.append(mT)
        c.mS.append(mS)
    kb.barrier()
    return c


def sin_reduced(kb, st, nm, ob, oap, ib, iap, shift, shape):
    u = kb.sbuf(st, nm + "_u", shape, F32)
    ki = kb.sbuf(st, nm + "_ki", shape, I32)
    kf = kb.sbuf(st, nm + "_kf", shape, F32)
    mk = kb.sbuf(st, nm + "_mk", shape, F32)
    full = tuple(slice(None) for _ in shape)
    inv2pi = float(1.0 / (2.0 * np.pi))
    tsc(kb, "dve", u, u[full], ib, iap, inv2pi, float(shift) * inv2pi, ALU.mult, ALU.add)
    cp(kb, "dve", ki, ki[full], u, u[full])
    cp(kb, "dve", kf, kf[full], ki, ki[full])
    tt(kb, "dve", u, u[full], u, u[full], kf, kf[full], ALU.subtract)
    tsc(kb, "dve", mk, mk[full], u, u[full], 0.5, None, ALU.is_gt)
    tt(kb, "dve", u, u[full], u, u[full], mk, mk[full], ALU.subtract)
    tsc(kb, "dve", mk, mk[full], u, u[full], -0.5, None, ALU.is_lt)
    tt(kb, "dve", u, u[full], u, u[full], mk, mk[full], ALU.add)
    tsc(kb, "dve", u, u[full], u, u[full], -0.4999999, 0.4999999, ALU.max, ALU.min)
    act(kb, ob, oap, u, u[full], AF.Sin, scale=float(2.0 * np.pi))


def layernorm_tile(kb, wk, x_b, g_bc, b_bc, out_b, eps=LN_EPS):
    stats, mv, rstd = wk["stats"], wk["mv"], wk["rstd"]
    for ci in range(2):
        kb.op("dve", lambda e, ci=ci: e.bn_stats(stats[:, ci, :], x_b[:, ci * 512:(ci + 1) * 512]), r=[x_b], w=[stats])
    kb.op("dve", lambda e: e.bn_aggr(mv[:, :], stats[:, :, :]), r=[stats], w=[mv])
    rsqrt(kb, wk["C"], rstd, rstd[:, :], mv, mv[:, 1:2], eps)
    tsc(kb, "dve", out_b, out_b[:, :], x_b, x_b[:, :], mv[:, 0:1], rstd[:, 0:1], ALU.subtract, ALU.mult,
        extra_r=[mv, rstd])
    tt(kb, "dve", out_b, out_b[:, :], out_b, out_b[:, :], g_bc, g_bc[:, :], ALU.mult)
    tt(kb, "dve", out_b, out_b[:, :], out_b, out_b[:, :], b_bc, b_bc[:, :], ALU.add)


class IO:
    pass


def declare_io(kb, dbg=None, small=False):
    nc = kb.nc
    io = IO()

    def inp(name, shape, dt=F32):
        b = Buf(nc.dram_tensor(name, list(shape), dt, kind="ExternalInput").ap(), name)
        setattr(io, name, b)
        return b
    inp("x", [T, D])
    inp("positions", [1, T], I32)
    inp("ln_in_g", [1, D]); inp("ln_in_b", [1, D])
    inp("w_in", [DEPTH, D, IN_DIM])
    inp("conv_w", [DEPTH, 5, 3072])
    inp("a_log", [DEPTH, 1, 16]); inp("dt_bias", [DEPTH, 1, 16])
    inp("gdn_norm_w", [DEPTH, 128, 1])
    inp("w_o_gdn", [DEPTH, D, D])
    inp("mla_q_norm_w", [DEPTH, QL, 1])
    inp("w_uq", [DEPTH, QL, 1536])
    inp("mla_kv_norm_w", [DEPTH, KVL, 1])
    inp("w_ukv", [DEPTH, KVL, 2048])
    inp("w_o_mla", [DEPTH, D, D])
    inp("w_out", [DEPTH, D, D])
    inp("ln1_g", [DEPTH, D]); inp("ln1_b", [DEPTH, D])
    inp("w_router_group", [DEPTH, D, 8]); inp("b_router_group", [DEPTH, 8])
    inp("w_router_expert", [DEPTH, D, NE]); inp("b_router_expert", [DEPTH, NE])
    if not small:
        inp("w_gate", [DEPTH, NE, D, DEXP]); inp("w_up", [DEPTH, NE, D, DEXP]); inp("w_down", [DEPTH, NE, DEXP, D])
    inp("ln2_g", [DEPTH, D]); inp("ln2_b", [DEPTH, D])
    io.out = kb.dram_tiles("out", [T, D], F32, NT, kind="ExternalOutput")
    io.h_res = kb.dram_tiles("h_res", [T, D], F32, NT)
    io.h_mid = kb.dram_tiles("h_mid", [T, D], F32, NT)
    io.hT = kb.dram_tiles("hT_scr", [128, 8, T], BF16, NT)
    io.cs = kb.dram("cs_scr", [2, 64, T], F32)
    io.xb = kb.dram("xb_scr", [NROWS, D], F32)
    io.yb = kb.dram("yb_scr", [NROWS, D], F32)
    io.og_scr = kb.dram("og_scr", [NSEQ, 128, 8, SEQ], BF16)
    io.om_scr = kb.dram("om_scr", [NSEQ, 128, 8, SEQ], BF16)
    io.dbg = {}
    for name, shape in (dbg or {}).items():
        io.dbg[name] = Buf(nc.dram_tensor(name, list(shape), F32, kind="ExternalOutput").ap(), name)
    return io


def emit_ln_rows(kb, C, st, nm, src_fn, g_ap, b_ap, io, tiles, h_dst, pre_fn=None, nb=2):
    g_bc = kb.sbuf(st, nm + "g", [128, D], F32)
    b_bc = kb.sbuf(st, nm + "b", [128, D], F32)
    dma(kb, "sp", g_bc, g_bc[:, :], g_ap[0], g_ap[1].partition_broadcast(128))
    dma(kb, "sp", b_bc, b_bc[:, :], b_ap[0], b_ap[1].partition_broadcast(128))
    NB = nb
    xt = [kb.sbuf(st, nm + "x%d" % j, [128, D], F32) for j in range(NB)]
    ht = [kb.sbuf(st, nm + "h%d" % j, [128, D], F32) for j in range(NB)]
    hb = [kb.sbuf(st, nm + "hb%d" % j, [128, D], BF16) for j in range(NB)]
    hTt = [kb.sbuf(st, nm + "hT%d" % j, [128, 8, 128], BF16) for j in range(NB)]
    pst = [kb.psum(st, nm + "ps%d" % j, [128, 8, 128], BF16) for j in range(NB)]
    wk = [dict(C=C, stats=kb.sbuf(st, nm + "st%d" % j, [128, 2, 6], F32), mv=kb.sbuf(st, nm + "mv%d" % j, [128, 2], F32),
               rstd=kb.sbuf(st, nm + "rs%d" % j, [128, 1], F32)) for j in range(NB)]
    for n, i in enumerate(tiles):
        j = n % NB
        src_fn(i, xt[j])
        layernorm_tile(kb, wk[j], xt[j], g_bc, b_bc, ht[j])
        dma(kb, "sp", h_dst[i], h_dst[i][i * 128:(i + 1) * 128, :], ht[j], ht[j][:, :])
        cp(kb, "act", hb[j], hb[j][:, :], ht[j], ht[j][:, :])
        for c in range(8):
            tr(kb, pst[j], pst[j][:, c, :], hb[j], hb[j][:, c * 128:(c + 1) * 128], C.id_b, C.id_b[:, :], inc=(c == 7))
        cp(kb, "dve", hTt[j], hTt[j][:, :, :], pst[j], pst[j][:, :, :])
        dma(kb, "sp", io.hT[i], io.hT[i][:, :, i * 128:(i + 1) * 128], hTt[j], hTt[j][:, :, :])


def stage_prologue(kb, C, io, tiles):
    with contextlib.ExitStack() as st:
        def src(i, buf):
            dma(kb, "pool", buf, buf[:, :], io.x, io.x[i * 128:(i + 1) * 128, :])
        emit_ln_rows(kb, C, st, "p_", src, (io.ln_in_g, io.ln_in_g[0:1, :]), (io.ln_in_b, io.ln_in_b[0:1, :]), io,
                     tiles, io.h_res, nb=4)
        kb.barrier()


def make_in_map(inp, core, small=False):
    f = lambda k: np.ascontiguousarray(np.asarray(inp[k], dtype=np.float32))
    m = {}
    m["x"] = f("x")[NSEQ * core:NSEQ * (core + 1)].reshape(T, D)
    m["positions"] = np.ascontiguousarray(np.asarray(inp["positions"], dtype=np.int32)[NSEQ * core:NSEQ * (core + 1)].reshape(1, T))
    m["ln_in_g"] = f("ln_in_g").reshape(1, D)
    m["ln_in_b"] = f("ln_in_b").reshape(1, D)
    m["a_log"] = f("a_log").reshape(DEPTH, 1, 16)
    m["dt_bias"] = f("dt_bias").reshape(DEPTH, 1, 16)
    m["gdn_norm_w"] = f("gdn_norm_w").reshape(DEPTH, 128, 1)
    m["mla_q_norm_w"] = f("mla_q_norm_w").reshape(DEPTH, QL, 1)
    m["mla_kv_norm_w"] = f("mla_kv_norm_w").reshape(DEPTH, KVL, 1)
    for k in ("w_in", "conv_w", "w_o_gdn", "w_uq", "w_ukv", "w_o_mla", "w_out", "ln1_g", "ln1_b", "w_router_group",
              "b_router_group", "w_router_expert", "b_router_expert", "w_gate", "w_up", "w_down", "ln2_g", "ln2_b"):
        if small and k in ("w_gate", "w_up", "w_down"):
            continue
        m[k] = f(k)
    return m


class Rot:
    def __init__(self, bufs):
        self.bufs = bufs
        self.i = 0

    def next(self):
        b = self.bufs[self.i % len(self.bufs)]
        self.i += 1
        return b


def load_w_fm(kb, io_w, w_ap, wt):
    kb.dma("pool", lambda e: e.dma_start(out=wt[:, :, :], in_=w_ap.rearrange("(k p) m -> p k m", p=128)),
           r=[io_w], w=[wt])


def gdn_seq(kb, C, io, l, b, load_hT, og_scr, cwT, dbg):
    w_in = io.w_in
    with contextlib.ExitStack() as st:
        g_tm = kb.sbuf(st, "g_tm", [128, 2, NCH, 8], F32)
        be_tm = kb.sbuf(st, "be_tm", [128, 2, NCH, 8], F32)
        b_tm = kb.sbuf(st, "b_tm", [128, 2, NCH, 8], F32)
        nbe_tm = kb.sbuf(st, "nbe_tm", [128, 2, NCH, 8], F32)
        v4 = lambda x: x[:, :, :, :].rearrange("p d c h -> p c d h")
        with contextlib.ExitStack() as s2:
            hT_b = load_hT(s2)
            wab = kb.sbuf(s2, "wab", [128, 8, 32], BF16)
            load_w_fm(kb, w_in, w_in[l, :, OFF_A:OFF_A + 32], wab)
            alog = kb.sbuf(s2, "alog", [128, 16], F32)
            dtb = kb.sbuf(s2, "dtb", [128, 16], F32)
            dma(kb, "sp", alog, alog[:, :], io.a_log, io.a_log[l, 0:1, :].partition_broadcast(128))
            dma(kb, "sp", dtb, dtb[:, :], io.dt_bias, io.dt_bias[l, 0:1, :].partition_broadcast(128))
            nea = kb.sbuf(s2, "nea", [128, 16], F32)
            act(kb, nea, nea[:, :], alog, alog[:, :], AF.Exp)
            tsc(kb, "dve", nea, nea[:, :], nea, nea[:, :], -1.0, None, ALU.mult)
            ps = kb.psum(s2, "ps_ab", [128, NCH, 32], F32)
            for ci in range(NCH):
                for k in range(8):
                    mm(kb, ps, ps[:, ci, :], hT_b, hT_b[:, k, ci * 128:(ci + 1) * 128], wab, wab[:, k, :],
                       start=(k == 0), stop=(k == 7))
            tmp = kb.sbuf(s2, "ab_tmp", [128, NCH, 16], F32)
            tmp4 = tmp[:, :, :].rearrange("p c (d h) -> p c d h", d=2)
            tt(kb, "dve", tmp, tmp[:, :, :], ps, ps[:, :, 0:16], dtb, dtb[:, :].unsqueeze(1).to_broadcast([128, NCH, 16]), ALU.add)
            act(kb, tmp, tmp[:, :, :], tmp, tmp[:, :, :], AF.Exp)
            act(kb, tmp, tmp[:, :, :], tmp, tmp[:, :, :], AF.Ln, bias=1.0)
            tt(kb, "dve", g_tm, v4(g_tm), tmp, tmp4, nea,
               nea[:, :].rearrange("p (d h) -> p d h", d=2).unsqueeze(1).to_broadcast([128, NCH, 2, 8]), ALU.mult)
            act(kb, be_tm, v4(be_tm), ps, ps[:, :, 16:32].rearrange("p c (d h) -> p c d h", d=2), AF.Sigmoid)
            tsc(kb, "dve", nbe_tm, nbe_tm[:, :, :, :], be_tm, be_tm[:, :, :, :], -1.0, None, ALU.mult)
            ps2 = kb.psum(s2, "ps_b", [128, 2, NCH * 8], F32)
            mm(kb, ps2, ps2[:, 0, :], C.triF, C.triF[:, :], g_tm, g_tm[:, 0, :, :].rearrange("p c h -> p (c h)"))
            mm(kb, ps2, ps2[:, 1, :], C.triB, C.triB[:, :], g_tm, g_tm[:, 1, :, :].rearrange("p c h -> p (c h)"))
            cp(kb, "dve", b_tm, b_tm[:, :, :, :].rearrange("p d c h -> p d (c h)"), ps2, ps2[:, :, :])
            kb.barrier()
        for hg in range(2):
            gdn_group(kb, C, io, l, b, hg, load_hT, og_scr, cwT, g_tm, (be_tm, nbe_tm), b_tm, dbg)
        kb.barrier()


def gdn_group(kb, C, io, l, b, hg, load_hT, og_scr, cwT, g_tm, be_tm, b_tm, dbg):
    w_in = io.w_in
    h0 = hg * 4
    with contextlib.ExitStack() as st:
        qT = kb.sbuf(st, "qT", [128, 4, SEQ], BF16)
        kT = kb.sbuf(st, "kT", [128, 4, SEQ], BF16)
        ktok = kb.sbuf(st, "ktok", [128, NCH, 4, 128], BF16)
        vtok = kb.sbuf(st, "vtok", [128, NCH, 4, 128], BF16)
        with contextlib.ExitStack() as s2:
            hT_b = load_hT(s2)
            wts = Rot([kb.sbuf(s2, "wqkv%d" % i, [128, 8, 128], BF16) for i in range(3)])
            raw = Rot([kb.sbuf(s2, "raw%d" % i, [128, SEQ + 4], F32) for i in range(3)])
            acc = Rot([kb.sbuf(s2, "acc%d" % i, [128, SEQ], F32) for i in range(3)])
            vT = kb.sbuf(s2, "vT", [128, SEQ], BF16)
            sq = kb.sbuf(s2, "sq", [128, SEQ], F32)
            rs = kb.sbuf(s2, "rs", [128, 512], F32)
            pss = Rot([kb.psum(s2, "ps_p%d" % i, [128, 512], F32) for i in range(4)])
            psn = Rot([kb.psum(s2, "ps_n%d" % i, [128, 512], F32) for i in range(2)])
            pst = Rot([kb.psum(s2, "ps_t%d" % i, [128, 8, 128], BF16) for i in range(2)])
            def item(which, off, hh):
                h = h0 + hh
                ct = (off // 128) + h
                wt = wts.next()
                load_w_fm(kb, w_in, w_in[l, :, off + h * 128: off + (h + 1) * 128], wt)
                rw = raw.next()
                memset(kb, "pool", rw, rw[:, 0:2], 0.0)
                memset(kb, "pool", rw, rw[:, SEQ + 2:SEQ + 4], 0.0)
                for n in range(4):
                    ps = pss.next()
                    for k in range(8):
                        mm(kb, ps, ps[:, :], wt, wt[:, k, :], hT_b, hT_b[:, k, n * 512:(n + 1) * 512],
                           start=(k == 0), stop=(k == 7))
                    cp(kb, "act", rw, rw[:, 2 + n * 512: 2 + (n + 1) * 512], ps, ps[:, :])
                ac = acc.next()
                en = "dve"
                tsc(kb, en, ac, ac[:, :], rw, rw[:, 0:SEQ], cwT[:, ct, 0:1], None, ALU.mult, extra_r=[cwT])
                for j in range(1, 5):
                    stt(kb, en, ac, ac[:, :], rw, rw[:, j:j + SEQ], cwT[:, ct, j:j + 1], ac, ac[:, :],
                        ALU.mult, ALU.add, extra_r=[cwT])
                yield
                if which == "v":
                    act(kb, vT, vT[:, :], ac, ac[:, :], AF.Silu)
                    for half in range(2):
                        pt = pst.next()
                        for ci in range(8):
                            cc = half * 8 + ci
                            tr(kb, pt, pt[:, ci, :], vT, vT[:, cc * 128:(cc + 1) * 128], C.id_b, C.id_b[:, :], inc=(ci == 7))
                        cp(kb, "act", vtok, vtok[:, half * 8:(half + 1) * 8, hh, :], pt, pt[:, :, :])
                else:
                    act(kb, ac, ac[:, :], ac, ac[:, :], AF.Silu)
                    tt(kb, "pool", sq, sq[:, :], ac, ac[:, :], ac, ac[:, :], ALU.mult)
                    dst = qT if which == "q" else kT
                    for n in range(4):
                        pn = psn.next()
                        mm(kb, pn, pn[:, :], C.ones_f, C.ones_f[:, :], sq, sq[:, n * 512:(n + 1) * 512])
                        cp(kb, "act", rs, rs[:, :], pn, pn[:, :])
                        rsqrt(kb, C, rs, rs[:, :], rs, rs[:, :], RMS_EPS)
                        if which == "q":
                            stt(kb, "dve", dst, dst[:, hh, n * 512:(n + 1) * 512], ac, ac[:, n * 512:(n + 1) * 512],
                                float(128 ** -0.5), rs, rs[:, :], ALU.mult, ALU.mult)
                        else:
                            tt(kb, "dve", dst, dst[:, hh, n * 512:(n + 1) * 512], ac, ac[:, n * 512:(n + 1) * 512],
                               rs, rs[:, :], ALU.mult)
                    if which == "k":
                        for half in range(2):
                            pt = pst.next()
                            for ci in range(8):
                                cc = half * 8 + ci
                                tr(kb, pt, pt[:, ci, :], kT, kT[:, hh, cc * 128:(cc + 1) * 128], C.id_b, C.id_b[:, :], inc=(ci == 7))
                            cp(kb, "act", ktok, ktok[:, half * 8:(half + 1) * 8, hh, :], pt, pt[:, :, :])

            gens = [item(which, off, hh) for which, off in (("q", OFF_Q), ("k", OFF_K), ("v", OFF_V)) for hh in range(4)]
            next(gens[0])
            for gi in range(len(gens)):
                if gi + 1 < len(gens):
                    next(gens[gi + 1])
                for _ in gens[gi]:
                    pass
            kb.barrier()
        oT = kb.sbuf(st, "oT", [128, 4, SEQ], F32)
        if dbg.get("skip_scan"):
            memset(kb, "pool", oT, oT[:, :, :], 1.0)
            if "dump" in dbg and hg == 0:
                o = dbg["dump"]
                for j, src in enumerate((qT, kT)):
                    for hh in range(4):
                        cp(kb, "dve", oT, oT[:, hh, :], src, src[:, hh, :])
                    dma(kb, "sp", o, o[j, :, :, :], oT, oT[:, :, :])
                for j, src in enumerate((ktok, vtok)):
                    cp(kb, "dve", oT, oT[:, :, :].rearrange("p a (b c) -> p b a c", b=NCH), src, src[:, :, :, :])
                    dma(kb, "sp", o, o[2 + j, :, :, :], oT, oT[:, :, :])
                memset(kb, "pool", oT, oT[:, :, :], 1.0)
        else:
            if "scan_stop" in dbg:
                memset(kb, "pool", oT, oT[:, :, :], 1.0)
            gdn_scan(kb, C, b, h0, qT, kT, ktok, vtok, oT, g_tm, be_tm, b_tm, dbg)
        with contextlib.ExitStack() as s2:
            hT_b = load_hT(s2)
            ogb = Rot([kb.sbuf(s2, "ogb%d" % i, [128, SEQ], BF16) for i in range(2)])
            nw = kb.sbuf(s2, "gnw", [128, 1], F32)
            dma(kb, "sp", nw, nw[:, :], io.gdn_norm_w, io.gdn_norm_w[l, :, :])
            wts = Rot([kb.sbuf(s2, "wz%d" % i, [128, 8, 128], BF16) for i in range(2)])
            pss = Rot([kb.psum(s2, "ps_z%d" % i, [128, 512], F32) for i in range(2)])
            psn = Rot([kb.psum(s2, "ps_zn%d" % i, [128, 512], F32) for i in range(2)])
            sq = kb.sbuf(s2, "sqo", [128, 512], F32)
            rs = kb.sbuf(s2, "rso", [128, 512], F32)
            zs = kb.sbuf(s2, "zs", [128, 512], F32)
            for hh in range(4):
                h = h0 + hh
                wt = wts.next()
                og_ = ogb.next()
                load_w_fm(kb, w_in, w_in[l, :, OFF_Z + h * 128: OFF_Z + (h + 1) * 128], wt)
                for n in range(4):
                    sl = slice(n * 512, (n + 1) * 512)
                    ps = pss.next()
                    for k in range(8):
                        mm(kb, ps, ps[:, :], wt, wt[:, k, :], hT_b, hT_b[:, k, sl], start=(k == 0), stop=(k == 7))
                    act(kb, zs, zs[:, :], ps, ps[:, :], AF.Silu)
                    tt(kb, "pool", sq, sq[:, :], oT, oT[:, hh, sl], oT, oT[:, hh, sl], ALU.mult)
                    pn = psn.next()
                    mm(kb, pn, pn[:, :], C.ones_f, C.ones_f[:, :], sq, sq[:, :])
                    tsc(kb, "dve", rs, rs[:, :], pn, pn[:, :], 1.0 / 128.0, None, ALU.mult)
                    rsqrt(kb, C, rs, rs[:, :], rs, rs[:, :], RMS_EPS)
                    tt(kb, "dve", rs, rs[:, :], rs, rs[:, :], zs, zs[:, :], ALU.mult)
                    stt(kb, "dve", og_, og_[:, sl], oT, oT[:, hh, sl], nw[:, 0:1], rs, rs[:, :], ALU.mult, ALU.mult,
                        extra_r=[nw])
                dma(kb, "sp", og_scr, og_scr[b, :, h, :], og_, og_[:, :])
            kb.barrier()


def gdn_scan(kb, C, b, h0, qT, kT, ktok, vtok, oT, g_tm, be_pair, b_tm, dbg):
    be_tm, nbe_tm = be_pair
    kb.serialize = (dbg.get("serial") == 1)
    kb.chain = (dbg.get("serial") in (3, 4, 5))
    kb.chain_engines = {3: ("pe", "act", "dve", "pool"), 4: ("act", "dve", "pool"), 5: ("pe", "dve", "pool")}.get(dbg.get("serial"), ())
    kb.last_op = None
    kb.psum_rar = bool(dbg.get("psum_rar", 1))
    phase_bar = (lambda: kb.barrier()) if dbg.get("serial") == 2 else (lambda: None)
    with contextlib.ExitStack() as st:
        psA = Rot([kb.psum(st, "ps_s%d" % i, [128, 4, 128], F32) for i in range(7)])
        psT = kb.psum(st, "ps_sT", [128, 8, 128], BF16)
        S = kb.sbuf(st, "S", [128, 8, 128], F32)
        Sb = kb.sbuf(st, "Sb", [128, 8, 128], BF16)
        memset(kb, "dve", S, S[:, :, :], 0.0)
        memset(kb, "dve", Sb, Sb[:, :, :], 0.0)
        R2 = lambda nm, shape, dt, n=2: Rot([kb.sbuf(st, "%s%d" % (nm, i), shape, dt) for i in range(n)])
        diag = R2("diag", [128, 4, 128], F32)
        E = R2("E", [128, 4, 128], F32)
        diff = R2("diff", [128, 4, 128], F32)
        sel = R2("sel", [128, 4, 128], F32, 4)
        GT = R2("GT", [128, 4, 128], F32)
        G = R2("G", [128, 4, 128], F32)
        PT = R2("PT", [128, 4, 128], BF16)
        tmpf = R2("tmpf", [128, 4, 128], F32, 3)
        p_r = R2("p", [128, 8, 128], BF16)
        pT_r = R2("pT", [128, 8, 128], BF16)
        tTf = kb.sbuf(st, "tTf", [128, 8, 128], F32)
        tTb = R2("tTb", [128, 8, 128], BF16)
        kbT = R2("kbT", [128, 4, 128], BF16)
        qdT = R2("qdT", [128, 4, 128], BF16)
        Rb = R2("Rb", [128, 4, 128], BF16)
        Ub = R2("Ub", [128, 4, 128], BF16)
        Kd = R2("Kd", [128, 4, 128], BF16)
        wcol = R2("wcol", [128, 4], F32)
        for i in range(dbg.get("nsteps", NCH)):
            chunk = (i, NCH - 1 - i)
            Ed, dfd, PTd = [None, None], [None, None], [None, None]
            p = p_r.next()
            pT = pT_r.next()
            def prep(d):
                c = chunk[d]
                cs = slice(c * 128, (c + 1) * 128)
                bcol = b_tm[:, d, c, h0:h0 + 4]
                kk = psA.next()
                qk = psA.next()
                for hh in range(4):
                    mm(kb, kk, kk[:, hh, :], kT, kT[:, hh, cs], kT, kT[:, hh, cs], inc=(hh == 3))
                for hh in range(4):
                    mm(kb, qk, qk[:, hh, :], kT, kT[:, hh, cs], qT, qT[:, hh, cs], inc=(hh == 3))
                yield
                dg = diag.next()
                tt(kb, "dve", dg, dg[:, :, :], C.id4_f, C.id4_f[:, :, :], b_tm, bcol.unsqueeze(2).to_broadcast([128, 4, 128]), ALU.mult)
                yield
                Db = psA.next()
                mm(kb, Db, Db[:, :, :], C.ones_f, C.ones_f[:, :], dg, dg[:, :, :])
                yield
                Ed[d] = E.next()
                act(kb, Ed[d], Ed[d][:, :, :], Db, Db[:, :, :], AF.Exp)
                yield
                dfd[d] = diff.next()
                tt(kb, "dve", dfd[d], dfd[d][:, :, :], Db, Db[:, :, :], b_tm, bcol.unsqueeze(2).to_broadcast([128, 4, 128]), ALU.subtract)
                s1 = sel.next()
                tt(kb, "dve", s1, s1[:, :, :], dfd[d], dfd[d][:, :, :], C.mT[d], C.mT[d][:, :, :], ALU.add)
                s2 = sel.next()
                tt(kb, "dve", s2, s2[:, :, :], dfd[d], dfd[d][:, :, :], C.mS[d], C.mS[d][:, :, :], ALU.add)
                yield
                gt = GT.next()
                act(kb, gt, gt[:, :, :], s1, s1[:, :, :], AF.Exp)
                g = G.next()
                act(kb, g, g[:, :, :], s2, s2[:, :, :], AF.Exp, scale=-1.0)
                yield
                PTd[d] = PT.next()
                tt(kb, "dve", PTd[d], PTd[d][:, :, :], qk, qk[:, :, :], gt, gt[:, :, :], ALU.mult)
                tf = tmpf.next()
                tt(kb, "dve", tf, tf[:, :, :], kk, kk[:, :, :], g, g[:, :, :], ALU.mult)
                nbecol = nbe_tm[:, d, c, h0:h0 + 4]
                tt(kb, "dve", p, p[:, d * 4:(d + 1) * 4, :], tf, tf[:, :, :], nbe_tm,
                   nbecol.unsqueeze(2).to_broadcast([128, 4, 128]), ALU.mult)
                yield
            for _ in zip(prep(0), prep(1)):
                pass
            if dbg.get("scan_stop", 9) <= 1:
                continue
            phase_bar()
            for j in range(8):
                tr(kb, psT, psT[:, j, :], p, p[:, j, :], C.id_b, C.id_b[:, :], inc=(j == 7))
            cp(kb, "act", pT, pT[:, :, :], psT, psT[:, :, :])
            tt(kb, "dve", tTf, tTf[:, :, :], psT, psT[:, :, :], C.id_f, C.id_f[:, None, :].to_broadcast([128, 8, 128]), ALU.add)
            tb = tTb.next()
            cp(kb, "act", tb, tb[:, :, :], tTf, tTf[:, :, :])
            if dbg.get("scan_stop", 9) <= 2:
                continue
            phase_bar()
            for it in range(6):
                pn = p_r.next()
                pa = [psA.next(), psA.next()]
                for j in range(8):
                    mm(kb, pa[j // 4], pa[j // 4][:, j % 4, :], pT, pT[:, j, :], p, p[:, j, :], inc=(j % 4 == 3))
                if it < 5:
                    pTn = pT_r.next()
                    pb = [psA.next(), psA.next()]
                    for j in range(8):
                        mm(kb, pb[j // 4], pb[j // 4][:, j % 4, :], p, p[:, j, :], pT, pT[:, j, :], inc=(j % 4 == 3))
                for hf in range(2):
                    cp(kb, "act", pn, pn[:, hf * 4:(hf + 1) * 4, :], pa[hf], pa[hf][:, :, :])
                if it < 5:
                    for hf in range(2):
                        cp(kb, "dve", pTn, pTn[:, hf * 4:(hf + 1) * 4, :], pb[hf], pb[hf][:, :, :])
                phase_bar()
                pu = [psA.next(), psA.next()]
                for j in range(8):
                    mm(kb, pu[j // 4], pu[j // 4][:, j % 4, :], pn, pn[:, j, :], tb, tb[:, j, :], inc=(j % 4 == 3))
                for hf in range(2):
                    tt(kb, "dve", tTf, tTf[:, hf * 4:(hf + 1) * 4, :], tTf, tTf[:, hf * 4:(hf + 1) * 4, :],
                       pu[hf], pu[hf][:, :, :], ALU.add)
                tb = tTb.next()
                cp(kb, "act", tb, tb[:, :, :], tTf, tTf[:, :, :])
                phase_bar()
                p = pn
                if it < 5:
                    pT = pTn
            if dbg.get("scan_stop", 9) <= 3:
                continue
            def chain(d):
                c = chunk[d]
                cs = slice(c * 128, (c + 1) * 128)
                last = 127 if d == 0 else 0
                becol = be_tm[:, d, c, h0:h0 + 4]
                kb_ = kbT.next()
                tt(kb, "dve", kb_, kb_[:, :, :], kT, kT[:, :, cs], Ed[d], Ed[d][:, :, :], ALU.mult)
                qd_ = qdT.next()
                tt(kb, "dve", qd_, qd_[:, :, :], qT, qT[:, :, cs], Ed[d], Ed[d][:, :, :], ALU.mult)
                yield
                pr = psA.next()
                for hh in range(4):
                    mm(kb, pr, pr[:, hh, :], kb_, kb_[:, hh, :], Sb, Sb[:, d * 4 + hh, :], inc=(hh == 3))
                yield
                tf = tmpf.next()
                tt(kb, "dve", tf, tf[:, :, :], vtok, vtok[:, c, :, :], pr, pr[:, :, :], ALU.subtract)
                rb = Rb.next()
                tt(kb, "dve", rb, rb[:, :, :], tf, tf[:, :, :], be_tm, becol.unsqueeze(2).to_broadcast([128, 4, 128]), ALU.mult)
                yield
                pu = psA.next()
                for hh in range(4):
                    mm(kb, pu, pu[:, hh, :], tb, tb[:, d * 4 + hh, :], rb, rb[:, hh, :], inc=(hh == 3))
                yield
                ub = Ub.next()
                cp(kb, "act", ub, ub[:, :, :], pu, pu[:, :, :])
                yield
                po = psA.next()
                for hh in range(4):
                    mm(kb, po, po[:, hh, :], Sb, Sb[:, d * 4 + hh, :], qd_, qd_[:, hh, :], start=True, stop=False)
                    mm(kb, po, po[:, hh, :], ub, ub[:, hh, :], PTd[d], PTd[d][:, hh, :], start=False, stop=True, inc=(hh == 3))
                yield
                first = (d == 0) == (c < NCH // 2)
                if first:
                    cp(kb, "act", oT, oT[:, :, cs], po, po[:, :, :])
                else:
                    tt(kb, "dve", oT, oT[:, :, cs], oT, oT[:, :, cs], po, po[:, :, :], ALU.add)
                wc = wcol.next()
                act(kb, wc, wc[:, :], dfd[d], dfd[d][:, :, last], AF.Exp)
                kd = Kd.next()
                tt(kb, "dve", kd, kd[:, :, :], ktok, ktok[:, c, :, :], wc, wc[:, :].unsqueeze(2).to_broadcast([128, 4, 128]), ALU.mult)
                yield
                psn = psA.next()
                for hh in range(4):
                    mm(kb, psn, psn[:, hh, :], kd, kd[:, hh, :], ub, ub[:, hh, :], inc=(hh == 3))
                yield
                Sd = S[:, d * 4:(d + 1) * 4, :]
                tt(kb, "dve", S, Sd, S, Sd, Ed[d], Ed[d][:, :, last:last + 1].to_broadcast([128, 4, 128]), ALU.mult)
                tt(kb, "dve", S, Sd, S, Sd, psn, psn[:, :, :], ALU.add)
                cp(kb, "act", Sb, Sb[:, d * 4:(d + 1) * 4, :], S, Sd)
                yield
            for _ in zip(chain(0), chain(1)):
                pass
        kb.serialize = False
        kb.chain = False
        kb.barrier()


def stage_rope_tables(kb, C, io):
    for b in range(NSEQ):
        sl = slice(b * SEQ, (b + 1) * SEQ)
        with contextlib.ExitStack() as st:
            pi_ = kb.sbuf(st, "rp_pi", [64, SEQ], I32)
            dma(kb, "sp", pi_, pi_[:, :], io.positions, io.positions[0:1, sl].partition_broadcast(64))
            pf = kb.sbuf(st, "rp_pf", [64, SEQ], F32)
            cp(kb, "dve", pf, pf[:, :], pi_, pi_[:, :])
            idx_i = kb.sbuf(st, "rp_ii", [64, 1], I32)
            kb.op("pool", lambda e: e.iota(idx_i[:, :], pattern=[[0, 1]], base=0, channel_multiplier=1), r=[], w=[idx_i])
            idx = kb.sbuf(st, "rp_if", [64, 1], F32)
            cp(kb, "dve", idx, idx[:, :], idx_i, idx_i[:, :])
            tsc(kb, "dve", idx, idx[32:64, :], idx, idx[32:64, :], -32.0, None, ALU.add)
            invf = kb.sbuf(st, "rp_inv", [64, 1], F32)
            act(kb, invf, invf[:, :], idx, idx[:, :], AF.Exp, scale=float(-np.log(10000.0) / 32.0))
            ang = kb.sbuf(st, "rp_ang", [64, SEQ], F32)
            tsc(kb, "dve", ang, ang[:, :], pf, pf[:, :], invf[:, 0:1], None, ALU.mult, extra_r=[invf])
            res = kb.sbuf(st, "rp_res", [64, SEQ], F32)
            for which, shift in ((0, np.pi / 2.0), (1, 0.0)):
                with contextlib.ExitStack() as s2:
                    sin_reduced(kb, s2, "rp_c", res, res[:, :], ang, ang[:, :], shift, [64, SEQ])
                    if which == 1:
                        tsc(kb, "dve", res, res[0:32, :], res, res[0:32, :], -1.0, None, ALU.mult)
                    dma(kb, "sp", io.cs, io.cs[which, :, sl], res, res[:, :])
                    kb.barrier()
            kb.barrier()


def rope_apply(kb, wk, src_b, src_ap, cos_b, sin_b, dst_b, dst_ap, n):
    x, xs, t1 = wk["x"], wk["xs"], wk["t1"]
    cp(kb, "act", x, x[:, 0:n], src_b, src_ap)
    cp(kb, "dve", xs, xs[0:32, 0:n], x, x[32:64, 0:n])
    cp(kb, "dve", xs, xs[32:64, 0:n], x, x[0:32, 0:n])
    tt(kb, "dve", t1, t1[:, 0:n], x, x[:, 0:n], cos_b[0], cos_b[1], ALU.mult)
    tt(kb, "pool", xs, xs[:, 0:n], xs, xs[:, 0:n], sin_b[0], sin_b[1], ALU.mult)
    tt(kb, "dve", dst_b, dst_ap, t1, t1[:, 0:n], xs, xs[:, 0:n], ALU.add)


def mla_seq(kb, C, io, l, b, load_hT, omT_dst, dbg):
    w_in = io.w_in
    scale = float(192 ** -0.5)
    with contextlib.ExitStack() as st:
        cqn = kb.sbuf(st, "cqn", [128, 3, SEQ], BF16)
        ckvn = kb.sbuf(st, "ckvn", [128, 2, SEQ], BF16)
        krT = kb.sbuf(st, "krT", [64, SEQ], BF16)
        cosb = kb.sbuf(st, "cosb", [64, SEQ], F32)
        sinb = kb.sbuf(st, "sinb", [64, SEQ], F32)
        dma(kb, "sp", cosb, cosb[:, :], io.cs, io.cs[0, :, b * SEQ:(b + 1) * SEQ])
        dma(kb, "sp", sinb, sinb[:, :], io.cs, io.cs[1, :, b * SEQ:(b + 1) * SEQ])
        rwk = dict(x=kb.sbuf(st, "rp_x", [64, 512], F32), xs=kb.sbuf(st, "rp_xs", [64, 512], F32),
                   t1=kb.sbuf(st, "rp_t1", [64, 512], F32))
        with contextlib.ExitStack() as s2:
            hT_b = load_hT(s2)
            pss = Rot([kb.psum(s2, "ps_m%d" % i, [128, 512], F32) for i in range(3)])
            psn = Rot([kb.psum(s2, "ps_mn%d" % i, [128, 512], F32) for i in range(2)])
            for nm, off, nt_, dstn, wnorm in (("cq", OFF_CQ, 3, cqn, io.mla_q_norm_w), ("ckv", OFF_CKV, 2, ckvn, io.mla_kv_norm_w)):
                wt = kb.sbuf(s2, "w_" + nm, [128, 8, nt_ * 128], BF16)
                load_w_fm(kb, w_in, w_in[l, :, off:off + nt_ * 128], wt)
                nw = kb.sbuf(s2, "nw_" + nm, [128, nt_], F32)
                for t_ in range(nt_):
                    dma(kb, "sp", nw, nw[:, t_:t_ + 1], wnorm, wnorm[l, t_ * 128:(t_ + 1) * 128, :])
                raw = kb.sbuf(s2, "raw_" + nm, [128, nt_, 512], F32)
                sq = kb.sbuf(s2, "sq_" + nm, [128, nt_, 512], F32)
                rs = kb.sbuf(s2, "rs_" + nm, [128, 512], F32)
                for n in range(4):
                    sl = slice(n * 512, (n + 1) * 512)
                    for t_ in range(nt_):
                        ps = pss.next()
                        for k in range(8):
                            mm(kb, ps, ps[:, :], wt, wt[:, k, t_ * 128:(t_ + 1) * 128], hT_b, hT_b[:, k, sl],
                               start=(k == 0), stop=(k == 7))
                        cp(kb, "act", raw, raw[:, t_, :], ps, ps[:, :])
                    tt(kb, "pool", sq, sq[:, :, :], raw, raw[:, :, :], raw, raw[:, :, :], ALU.mult)
                    pn = psn.next()
                    for t_ in range(nt_):
                        mm(kb, pn, pn[:, :], C.ones_f, C.ones_f[:, :], sq, sq[:, t_, :], start=(t_ == 0), stop=(t_ == nt_ - 1))
                    tsc(kb, "dve", rs, rs[:, :], pn, pn[:, :], 1.0 / (nt_ * 128), None, ALU.mult)
                    rsqrt(kb, C, rs, rs[:, :], rs, rs[:, :], RMS_EPS)
                    for t_ in range(nt_):
                        stt(kb, "dve", dstn, dstn[:, t_, sl], raw, raw[:, t_, :], nw[:, t_:t_ + 1], rs, rs[:, :],
                            ALU.mult, ALU.mult, extra_r=[nw])
            wkr = kb.sbuf(s2, "w_kr", [128, 8, 64], BF16)
            load_w_fm(kb, w_in, w_in[l, :, OFF_KR:OFF_KR + 64], wkr)
            for n in range(4):
                sl = slice(n * 512, (n + 1) * 512)
                ps = pss.next()
                for k in range(8):
                    mm(kb, ps, ps[0:64, :], wkr, wkr[:, k, :], hT_b, hT_b[:, k, sl], start=(k == 0), stop=(k == 7))
                rope_apply(kb, rwk, ps, ps[0:64, :], (cosb, cosb[:, sl]), (sinb, sinb[:, sl]), krT, krT[:, sl], 512)
            kb.barrier()
        with contextlib.ExitStack() as s2:
            wq = Rot([kb.sbuf(s2, "w_uq%d" % i, [128, 3, 192], BF16) for i in range(2)])
            wkv = Rot([kb.sbuf(s2, "w_ukv%d" % i, [128, 2, 256], BF16) for i in range(2)])
            qnT = Rot([kb.sbuf(s2, "qnT%d" % i, [128, SEQ], BF16) for i in range(2)])
            qrT = Rot([kb.sbuf(s2, "qrT%d" % i, [64, SEQ], BF16) for i in range(2)])
            knT = Rot([kb.sbuf(s2, "knT%d" % i, [128, SEQ], BF16) for i in range(2)])
            vtk = Rot([kb.sbuf(s2, "vtk%d" % i, [128, NCH, 128], BF16) for i in range(2)])
            pex = Rot([kb.sbuf(s2, "pex%d" % i, [128, 512], BF16) for i in range(3)])
            oh = Rot([kb.sbuf(s2, "oh%d" % i, [128, SEQ], BF16) for i in range(2)])
            rden = kb.sbuf(s2, "rden", [128, 512], F32)
            pss = Rot([kb.psum(s2, "ps_a%d" % i, [128, 512], F32) for i in range(3)])
            pso = Rot([kb.psum(s2, "ps_o%d" % i, [128, 512], F32) for i in range(2)])
            psd = Rot([kb.psum(s2, "ps_d%d" % i, [128, 512], F32) for i in range(2)])
            psv = kb.psum(s2, "ps_v", [128, 4, 128], F32)
            for h in range(H):
                wq_ = wq.next()
                load_w_fm(kb, io.w_uq, io.w_uq[l, :, h * 192:(h + 1) * 192], wq_)
                wkv_ = wkv.next()
                load_w_fm(kb, io.w_ukv, io.w_ukv[l, :, h * 256:(h + 1) * 256], wkv_)
                qn, qr, kn, vt, o_ = qnT.next(), qrT.next(), knT.next(), vtk.next(), oh.next()
                for n in range(4):
                    sl = slice(n * 512, (n + 1) * 512)
                    ps = pss.next()
                    for k in range(3):
                        mm(kb, ps, ps[:, :], wq_, wq_[:, k, 0:128], cqn, cqn[:, k, sl], start=(k == 0), stop=(k == 2))
                    cp(kb, "act", qn, qn[:, sl], ps, ps[:, :])
                    ps = pss.next()
                    for k in range(3):
                        mm(kb, ps, ps[0:64, :], wq_, wq_[:, k, 128:192], cqn, cqn[:, k, sl], start=(k == 0), stop=(k == 2))
                    rope_apply(kb, rwk, ps, ps[0:64, :], (cosb, cosb[:, sl]), (sinb, sinb[:, sl]), qr, qr[:, sl], 512)
                    ps = pss.next()
                    for k in range(2):
                        mm(kb, ps, ps[:, :], wkv_, wkv_[:, k, 0:128], ckvn, ckvn[:, k, sl], start=(k == 0), stop=(k == 1))
                    cp(kb, "dve", kn, kn[:, sl], ps, ps[:, :])
                    for t4 in range(4):
                        tkn = n * 4 + t4
                        for k in range(2):
                            mm(kb, psv, psv[:, t4, :], ckvn, ckvn[:, k, tkn * 128:(tkn + 1) * 128], wkv_, wkv_[:, k, 128:256],
                               start=(k == 0), stop=(k == 1))
                    cp(kb, "act", vt, vt[:, n * 4:(n + 1) * 4, :], psv, psv[:, :, :])
                for qb in range(4):
                    qs = slice(qb * 512, (qb + 1) * 512)
                    po = pso.next()
                    pd = psd.next()
                    prev = None
                    for kt in range(NCH + 1):
                        cur = None
                        if kt < NCH:
                            ks = slice(kt * 128, (kt + 1) * 128)
                            ps = pss.next()
                            mm(kb, ps, ps[:, :], kn, kn[:, ks], qn, qn[:, qs], start=True, stop=False)
                            mm(kb, ps, ps[:, :], krT, krT[:, ks], qr, qr[:, qs], start=False, stop=True)
                            cur = pex.next()
                            act(kb, cur, cur[:, :], ps, ps[:, :], AF.Exp, scale=scale)
                        if prev is not None:
                            k0 = kt - 1
                            mm(kb, po, po[:, :], vt, vt[:, k0, :], prev, prev[:, :], start=(k0 == 0), stop=(k0 == NCH - 1))
                            mm(kb, pd, pd[:, :], C.ones_b, C.ones_b[:, :], prev, prev[:, :], start=(k0 == 0), stop=(k0 == NCH - 1))
                        prev = cur
                    kb.op("dve", lambda e, pd=pd: e.reciprocal(rden[:, :], pd[:, :]), r=[pd], w=[rden])
                    tt(kb, "dve", o_, o_[:, qs], po, po[:, :], rden, rden[:, :], ALU.mult)
                db, dap = omT_dst(h)
                dma(kb, "sp", db, dap, o_, o_[:, :])
            kb.barrier()


def mixer_out(kb, C, io, l, b, load_hT, og_scr, om_scr, dbg):
    with contextlib.ExitStack() as st:
        yT = kb.sbuf(st, "yT", [128, 8, SEQ], BF16)
        with contextlib.ExitStack() as s2:
            hT_b = load_hT(s2)
            ogT = kb.sbuf(s2, "ogT", [128, 8, SEQ], BF16)
            omT = kb.sbuf(s2, "omT", [128, 8, SEQ], BF16)
            dma(kb, "sp", ogT, ogT[:, :, :], og_scr, og_scr[b, :, :, :])
            dma(kb, "sp", omT, omT[:, :, :], om_scr, om_scr[b, :, :, :])
            wr = [Rot([kb.sbuf(s2, "wo%d_%d" % (j, i), [128, 8, 128], BF16) for i in range(2)]) for j in range(4)]
            ps = [Rot([kb.psum(s2, "ps_y%d_%d" % (j, i), [128, 512], F32) for i in range(2)]) for j in range(4)]
            sg = Rot([kb.sbuf(s2, "sg%d" % i, [128, 512], F32) for i in range(4)])
            tq = Rot([kb.sbuf(s2, "tq%d" % i, [128, 512], F32) for i in range(4)])
            for m in range(8):
                ms = slice(m * 128, (m + 1) * 128)
                w4 = [r_.next() for r_ in wr]
                load_w_fm(kb, io.w_o_gdn, io.w_o_gdn[l, :, ms], w4[0])
                load_w_fm(kb, io.w_o_mla, io.w_o_mla[l, :, ms], w4[1])
                load_w_fm(kb, io.w_in, io.w_in[l, :, OFF_G + m * 128:OFF_G + (m + 1) * 128], w4[2])
                load_w_fm(kb, io.w_in, io.w_in[l, :, OFF_G + 1024 + m * 128:OFF_G + 1024 + (m + 1) * 128], w4[3])
                for n in range(4):
                    sl = slice(n * 512, (n + 1) * 512)
                    p4 = [r_.next() for r_ in ps]
                    for j, src in enumerate((ogT, omT, hT_b, hT_b)):
                        for k in range(8):
                            mm(kb, p4[j], p4[j][:, :], w4[j], w4[j][:, k, :], src, src[:, k, sl], start=(k == 0), stop=(k == 7))
                    s1, s2_ = sg.next(), sg.next()
                    act(kb, s1, s1[:, :], p4[2], p4[2][:, :], AF.Sigmoid)
                    act(kb, s2_, s2_[:, :], p4[3], p4[3][:, :], AF.Sigmoid)
                    t1, t2 = tq.next(), tq.next()
                    tt(kb, "dve", t1, t1[:, :], p4[0], p4[0][:, :], s1, s1[:, :], ALU.mult)
                    tt(kb, "dve", t2, t2[:, :], p4[1], p4[1][:, :], s2_, s2_[:, :], ALU.mult)
                    tt(kb, "pool", yT, yT[:, m, sl], t1, t1[:, :], t2, t2[:, :], ALU.add)
            kb.barrier()
        with contextlib.ExitStack() as s2:
            wo = kb.sbuf(s2, "w_out", [128, 8, D], BF16)
            load_w_fm(kb, io.w_out, io.w_out[l, :, :], wo)
            mT = kb.sbuf(s2, "mT", [128, 8, 512], F32)
            psm = Rot([kb.psum(s2, "ps_mo%d" % i, [128, 512], F32) for i in range(2)])
            pst = Rot([kb.psum(s2, "ps_mt%d" % i, [128, 4, 128], F32) for i in range(4)])
            hold = Rot([kb.sbuf(s2, "hold%d" % i, [128, D], F32) for i in range(2)])
            state = {"n": -1}

            def src(i, buf):
                ti = i - b * NCH
                n, t4 = ti // 4, ti % 4
                if n != state["n"]:
                    state["n"] = n
                    sl = slice(n * 512, (n + 1) * 512)
                    for m in range(8):
                        pm = psm.next()
                        for k in range(8):
                            mm(kb, pm, pm[:, :], wo, wo[:, k, m * 128:(m + 1) * 128], yT, yT[:, k, sl], start=(k == 0), stop=(k == 7))
                        cp(kb, "act", mT, mT[:, m, :], pm, pm[:, :])
                ho = hold.next()
                dma(kb, "pool", ho, ho[:, :], io.h_res[i], io.h_res[i][i * 128:(i + 1) * 128, :])
                for half in range(2):
                    pt = pst.next()
                    for mm_ in range(4):
                        m = half * 4 + mm_
                        tr(kb, pt, pt[:, mm_, :], mT, mT[:, m, t4 * 128:(t4 + 1) * 128], C.id_f, C.id_f[:, :])
                    stt(kb, "dve", buf, buf[:, half * 512:(half + 1) * 512], ho, ho[:, half * 512:(half + 1) * 512], float(DN_ALPHA),
                        pt, pt[:, :, :].rearrange("p a b -> p (a b)"), ALU.mult, ALU.add)
            emit_ln_rows(kb, C, s2, "l1_", src, (io.ln1_g, io.ln1_g[l:l + 1, :]), (io.ln1_b, io.ln1_b[l:l + 1, :]), io,
                         list(range(b * NCH, (b + 1) * NCH)), io.h_mid)
            kb.barrier()


def stage_mixer(kb, C, io, l, seqs, dbg, parts=("gdn", "mla", "out")):
    og_scr, om_scr = io.og_scr, io.om_scr
    with contextlib.ExitStack() as st0:
        cwT = kb.sbuf(st0, "cwT", [128, 24, 5], F32)
        with contextlib.ExitStack() as s2:
            cwr = kb.sbuf(s2, "cwr", [5, 3072], F32)
            dma(kb, "sp", cwr, cwr[:, :], io.conv_w, io.conv_w[l, :, :])
            pc = kb.psum(s2, "ps_cw", [128, 24, 8], F32)
            for t_ in range(24):
                tr(kb, pc, pc[:, t_, 0:5], cwr, cwr[0:5, t_ * 128:(t_ + 1) * 128], C.id_f, C.id_f[0:5, 0:5])
            cp(kb, "dve", cwT, cwT[:, :, :], pc, pc[:, :, 0:5])
            kb.barrier()
        for b in seqs:
            def load_hT(stk, b=b):
                hT_b = kb.sbuf(stk, "hT_b", [128, 8, SEQ], BF16)
                kb.dma("sp", lambda e: e.dma_start(out=hT_b[:, :, :], in_=io.hT[0][:, :, b * SEQ:(b + 1) * SEQ]),
                       r=[io.hT[i] for i in range(b * NCH, (b + 1) * NCH)], w=[hT_b])
                return hT_b
            if "gdn" in parts:
                gdn_seq(kb, C, io, l, b, load_hT, og_scr, cwT, dbg)
            if "mla" in parts:
                mla_seq(kb, C, io, l, b, load_hT, lambda h, b=b: (om_scr, om_scr[b, :, h, :]), dbg)
            if "out" in parts:
                mixer_out(kb, C, io, l, b, load_hT, og_scr, om_scr, dbg)


def idma(kb, ob, oap, out_off, ib, iap, in_off, extra_r=(), nrows=None):
    if not hasattr(kb, "bc_regs"):
        kb.bc_regs = {}
    if nrows not in kb.bc_regs:
        kb.bc_regs[nrows] = kb.nc.gpsimd.to_reg(nrows - 1)
    bc = kb.bc_regs[nrows]

    def fn(e):
        return e.indirect_dma_start(
            out=oap, out_offset=(bass.IndirectOffsetOnAxis(ap=out_off, axis=0) if out_off is not None else None),
            in_=iap, in_offset=(bass.IndirectOffsetOnAxis(ap=in_off, axis=0) if in_off is not None else None),
            bounds_check=bc, oob_is_err=False)
    return kb.dma("pool", fn, r=[ib] + list(extra_r), w=[ob])


def stage_moe(kb, C, io, l, dst_tiles, dbg):
    NEG = -1.0e30
    with contextlib.ExitStack() as st:
        ohE = kb.sbuf(st, "ohE", [128, NT, 2, NE], F32)
        gate = kb.sbuf(st, "gate", [128, NT, 2], F32)
        destI = kb.sbuf(st, "destI", [128, NT, 2], I32)
        BEi = kb.sbuf(st, "BEi", [128, NBLK], I32)
        offs_gu = kb.sbuf(st, "offs_gu", [128, NBLK, 8], I32)
        offs_d = kb.sbuf(st, "offs_d", [128, NBLK, 4], I32)
        zt = kb.sbuf(st, "zt", [128, 4, D], F32)
        memset(kb, "dve", zt, zt[:, :, :], 0.0)
        for j in range(NROWS // 512):
            kb.dma("pool", lambda e, j=j: e.dma_start(out=io.xb[j * 512:(j + 1) * 512, :].rearrange("(a p) d -> p a d", p=128),
                                                      in_=zt[:, :, :]), r=[zt], w=[io.xb])
        with contextlib.ExitStack() as s2:
            kb.serialize = bool(dbg.get("serial_moe", False))
            wr = kb.sbuf(s2, "wr", [128, 8, 72], F32)
            dma(kb, "sp", wr, wr[:, :, 0:8], io.w_router_group, io.w_router_group[l, :, :].rearrange("(k p) g -> p k g", p=128))
            dma(kb, "sp", wr, wr[:, :, 8:72], io.w_router_expert, io.w_router_expert[l, :, :].rearrange("(k p) g -> p k g", p=128))
            br = kb.sbuf(s2, "br", [128, 72], F32)
            dma(kb, "sp", br, br[:, 0:8], io.b_router_group, io.b_router_group[l:l + 1, :].partition_broadcast(128))
            dma(kb, "sp", br, br[:, 8:72], io.b_router_expert, io.b_router_expert[l:l + 1, :].partition_broadcast(128))
            xt = Rot([kb.sbuf(s2, "mx%d" % i, [128, D], F32) for i in range(2)])
            hTf = kb.sbuf(s2, "hTf", [128, 8, 128], F32)
            pst = Rot([kb.psum(s2, "ps_rt%d" % i, [128, 4, 128], F32) for i in range(2)])
            psl = kb.psum(s2, "ps_rl", [128, 72], F32)
            lgall = kb.sbuf(s2, "lgall", [128, NT, 72], F32)
            for i in range(NT):
                x = xt.next()
                dma(kb, "sp", x, x[:, :], io.h_mid[i], io.h_mid[i][i * 128:(i + 1) * 128, :])
                for half in range(2):
                    pt = pst.next()
                    for c4 in range(4):
                        c = half * 4 + c4
                        tr(kb, pt, pt[:, c4, :], x, x[:, c * 128:(c + 1) * 128], C.id_f, C.id_f[:, :], inc=(c4 == 3))
                    cp(kb, "act", hTf, hTf[:, half * 4:(half + 1) * 4, :], pt, pt[:, :, :])
                for k in range(8):
                    mm(kb, psl, psl[:, :], hTf, hTf[:, k, :], wr, wr[:, k, :], start=(k == 0), stop=(k == 7))
                tt(kb, "dve", lgall, lgall[:, i, :], psl, psl[:, :], br, br[:, :], ALU.add)
            A3 = lambda nm, n: kb.sbuf(s2, "rb_" + nm, [128, NT, n], F32)
            A2 = lambda nm: kb.sbuf(s2, "rb_" + nm, [128, NT], F32)
            LG = lgall[:, :, 0:8]
            LE4 = lgall[:, :, 8:72].rearrange("p t (g e) -> p t g e", g=8)
            bc8 = lambda buf: buf[:, :].unsqueeze(2).to_broadcast([128, NT, 8])
            gmax, se, pg, m1, m2, r_, dd = A2("gmax"), A2("se"), A2("pg"), A2("m1"), A2("m2"), A2("r"), A2("dd")
            ohg, eg, les, oh1, le2, oh2 = A3("ohg", 8), A3("eg", 8), A3("les", 8), A3("oh1", 8), A3("le2", 8), A3("oh2", 8)
            t64 = A3("t64", 64)
            t64v = t64[:, :, :].rearrange("p t (g e) -> p t g e", g=8)
            kb.op("dve", lambda e: e.reduce_max(out=gmax[:, :], in_=LG, axis=AX.X), r=[lgall], w=[gmax])
            tt(kb, "dve", ohg, ohg[:, :, :], lgall, LG, gmax, bc8(gmax), ALU.is_equal)
            tt(kb, "dve", eg, eg[:, :, :], lgall, LG, gmax, bc8(gmax), ALU.subtract)
            act(kb, eg, eg[:, :, :], eg, eg[:, :, :], AF.Exp)
            kb.op("dve", lambda e: e.reduce_sum(out=se[:, :], in_=eg[:, :, :], axis=AX.X), r=[eg], w=[se])
            kb.op("dve", lambda e: e.reciprocal(pg[:, :], se[:, :]), r=[se], w=[pg])
            tt(kb, "dve", t64, t64v, lgall, LE4, ohg, ohg[:, :, :].unsqueeze(3).to_broadcast([128, NT, 8, 8]), ALU.mult)
            kb.op("dve", lambda e: e.reduce_sum(out=les[:, :, :], in_=t64[:, :, :].rearrange("p t (g e) -> p t e g", g=8), axis=AX.X),
                  r=[t64], w=[les])
            kb.op("dve", lambda e: e.reduce_max(out=m1[:, :], in_=les[:, :, :], axis=AX.X), r=[les], w=[m1])
            tt(kb, "dve", oh1, oh1[:, :, :], les, les[:, :, :], m1, bc8(m1), ALU.is_equal)
            stt(kb, "dve", le2, le2[:, :, :], oh1, oh1[:, :, :], NEG, les, les[:, :, :], ALU.mult, ALU.add)
            kb.op("dve", lambda e: e.reduce_max(out=m2[:, :], in_=le2[:, :, :], axis=AX.X), r=[le2], w=[m2])
            tt(kb, "dve", oh2, oh2[:, :, :], le2, le2[:, :, :], m2, bc8(m2), ALU.is_equal)
            tt(kb, "dve", r_, r_[:, :], m2, m2[:, :], m1, m1[:, :], ALU.subtract)
            act(kb, r_, r_[:, :], r_, r_[:, :], AF.Exp)
            tsc(kb, "dve", dd, dd[:, :], r_, r_[:, :], 1.0, None, ALU.add)
            kb.op("dve", lambda e: e.reciprocal(dd[:, :], dd[:, :]), r=[dd], w=[dd])
            tt(kb, "dve", dd, dd[:, :], dd, dd[:, :], pg, pg[:, :], ALU.mult)
            cp(kb, "dve", gate, gate[:, :, 0], dd, dd[:, :])
            tt(kb, "dve", gate, gate[:, :, 1], dd, dd[:, :], r_, r_[:, :], ALU.mult)
            for k_, oh in ((0, oh1), (1, oh2)):
                tt(kb, "dve", ohE, ohE[:, :, k_, :].rearrange("p t (g e) -> p t g e", g=8), ohg,
                   ohg[:, :, :].unsqueeze(3).to_broadcast([128, NT, 8, 8]), oh, oh[:, :, :].unsqueeze(2).to_broadcast([128, NT, 8, 8]), ALU.mult)
            kb.barrier()
        with contextlib.ExitStack() as s2:
            ohs = kb.sbuf(s2, "ohs", [128, NT, NE], F32)
            cum = kb.sbuf(s2, "cum", [128, NT + 1, NE], F32)
            tt(kb, "dve", ohs, ohs[:, :, :], ohE, ohE[:, :, 0, :], ohE, ohE[:, :, 1, :], ALU.add)
            memset(kb, "dve", cum, cum[:, 0, :], 0.0)
            for i in range(NT):
                tt(kb, "dve", cum, cum[:, i + 1, :], cum, cum[:, i, :], ohs, ohs[:, i, :], ALU.add)
            striU = kb.sbuf(s2, "striU", [128, 128], F32)
            tt(kb, "dve", striU, striU[:, :], C.triF, C.triF[:, :], C.id_f, C.id_f[:, :], ALU.subtract)
            pc = kb.psum(s2, "ps_cnt", [64, 128], F32)
            for i in range(NT):
                mm(kb, pc, pc[:, :], ohs, ohs[:, i, :], C.ones_f, C.ones_f[:, :], start=(i == 0), stop=(i == NT - 1))
            cntT = kb.sbuf(s2, "cntT", [64, 128], F32)
            tsc(kb, "dve", cntT, cntT[:, :], pc, pc[:, :], 127.0, None, ALU.add)
            ci = kb.sbuf(s2, "cnt_i", [64, 128], I32)
            cp(kb, "dve", ci, ci[:, :], cntT, cntT[:, :])
            tsc(kb, "dve", ci, ci[:, :], ci, ci[:, :], 7, None, ALU.arith_shift_right)
            tsc(kb, "dve", ci, ci[:, :], ci, ci[:, :], 7, None, ALU.logical_shift_left)
            padT = kb.sbuf(s2, "padT", [64, 128], F32)
            cp(kb, "dve", padT, padT[:, :], ci, ci[:, :])
            pps = kb.psum(s2, "ps_pst", [128, 64], F32)
            mm(kb, pps, pps[:, :], padT, padT[:, :], striU, striU[0:64, 0:64])
            pstart = kb.sbuf(s2, "pstart", [128, NE], F32)
            cp(kb, "dve", pstart, pstart[:, :], pps, pps[:, :])
            ppe = kb.psum(s2, "ps_pend", [64, 128], F32)
            mm(kb, ppe, ppe[:, :], C.triF, C.triF[0:64, 0:64], padT, padT[:, :])
            jrow_i = kb.sbuf(s2, "jrow_i", [64, 128], I32)
            kb.op("pool", lambda e: e.iota(jrow_i[:, :], pattern=[[128, 128]], base=0, channel_multiplier=0), r=[], w=[jrow_i])
            jrow = kb.sbuf(s2, "jrow", [64, 128], F32)
            cp(kb, "dve", jrow, jrow[:, :], jrow_i, jrow_i[:, :])
            cmpm = kb.sbuf(s2, "cmpm", [64, 128], F32)
            tt(kb, "dve", cmpm, cmpm[:, :], ppe, ppe[:, :], jrow, jrow[:, :], ALU.is_le)
            pbe = kb.psum(s2, "ps_be", [128, 128], F32)
            mm(kb, pbe, pbe[:, :], C.ones_f, C.ones_f[0:64, :], cmpm, cmpm[:, :])
            bef = kb.sbuf(s2, "bef", [128, NBLK], F32)
            unused = kb.sbuf(s2, "unused", [128, NBLK], F32)
            tsc(kb, "dve", unused, unused[:, :], pbe, pbe[:, :], 63.5, 4194304.0, ALU.is_gt, ALU.mult)
            tsc(kb, "dve", bef, bef[:, :], pbe, pbe[:, :], 63.0, None, ALU.min)
            cp(kb, "dve", BEi, BEi[:, :], bef, bef[:, :])
            prow_i = kb.sbuf(s2, "prow_i", [128, 8], I32)
            kb.op("pool", lambda e: e.iota(prow_i[:, :], pattern=[[128, 8]], base=0, channel_multiplier=1), r=[], w=[prow_i])
            prow = kb.sbuf(s2, "prow", [128, 8], F32)
            cp(kb, "dve", prow, prow[:, :], prow_i, prow_i[:, :])
            of = kb.sbuf(s2, "off_f", [128, NBLK, 8], F32)
            stt(kb, "dve", of, of[:, :, :], bef, bef[:, :].unsqueeze(2).to_broadcast([128, NBLK, 8]), 128.0, prow,
                prow[:, 0:1].unsqueeze(1).to_broadcast([128, NBLK, 8]), ALU.mult, ALU.add)
            tsc(kb, "dve", of, of[:, :, :], of, of[:, :, :], float(l * NE * 128), None, ALU.add)
            tt(kb, "dve", of, of[:, :, :], of, of[:, :, :], unused, unused[:, :].unsqueeze(2).to_broadcast([128, NBLK, 8]), ALU.add)
            cp(kb, "dve", offs_gu, offs_gu[:, :, :], of, of[:, :, :])
            stt(kb, "dve", of, of[:, :, 0:4], bef, bef[:, :].unsqueeze(2).to_broadcast([128, NBLK, 4]), 512.0, prow,
                prow[:, 0:4].unsqueeze(1).to_broadcast([128, NBLK, 4]), ALU.mult, ALU.add)
            tsc(kb, "dve", of, of[:, :, 0:4], of, of[:, :, 0:4], float(l * NE * DEXP), None, ALU.add)
            tt(kb, "dve", of, of[:, :, 0:4], of, of[:, :, 0:4], unused, unused[:, :].unsqueeze(2).to_broadcast([128, NBLK, 4]), ALU.add)
            cp(kb, "dve", offs_d, offs_d[:, :, :], of, of[:, :, 0:4])
            ppf = Rot([kb.psum(s2, "ps_pf%d" % i, [128, 64], F32) for i in range(2)])
            tq = kb.sbuf(s2, "tq64", [128, NE], F32)
            tq2 = kb.sbuf(s2, "tq64b", [128, NE], F32)
            dsf = kb.sbuf(s2, "dsf", [128, NT, 2], F32)
            for i in range(NT):
                pp = ppf.next()
                mm(kb, pp, pp[:, :], striU, striU[:, :], ohs, ohs[:, i, :], start=True, stop=False)
                mm(kb, pp, pp[:, :], C.ones_f, C.ones_f[:, :], cum, cum[:, i, :], start=False, stop=True)
                tt(kb, "dve", tq, tq[:, :], pp, pp[:, :], pstart, pstart[:, :], ALU.add)
                for k_ in range(2):
                    tt(kb, "dve", tq2, tq2[:, :], tq, tq[:, :], ohE, ohE[:, i, k_, :], ALU.mult)
                    kb.op("dve", lambda e, i=i, k_=k_: e.reduce_sum(out=dsf[:, i, k_:k_ + 1], in_=tq2[:, :], axis=AX.X), r=[tq2], w=[dsf])
            cp(kb, "dve", destI, destI[:, :, :], dsf, dsf[:, :, :])
            kb.barrier()
        kb.serialize = False
        xb = io.xb
        with contextlib.ExitStack() as s2:
            xt = Rot([kb.sbuf(s2, "sx%d" % i, [128, D], F32) for i in range(3)])
            for i in range(NT):
                x = xt.next()
                dma(kb, "sp", x, x[:, :], io.h_mid[i], io.h_mid[i][i * 128:(i + 1) * 128, :])
                for k_ in range(2):
                    idma(kb, xb, xb[:, :], destI[:, i, k_:k_ + 1], x, x[:, :], None, extra_r=[destI], nrows=NROWS)
            kb.barrier()
        yb = io.yb
        wg_v = io.w_gate[:, :, :, :].rearrange("l e (p c) n -> (l e p) (c n)", c=8)
        wu_v = io.w_up[:, :, :, :].rearrange("l e (p c) n -> (l e p) (c n)", c=8)
        wd_v = io.w_down[:, :, :, :].rearrange("l e (p c) n -> (l e p) (c n)", c=4)
        with contextlib.ExitStack() as s2:
            WDT = BF16
            wg = Rot([kb.sbuf(s2, "wg%d" % i, [128, 8, DEXP], WDT) for i in range(3)])
            wu = Rot([kb.sbuf(s2, "wu%d" % i, [128, 8, DEXP], WDT) for i in range(3)])
            wd = Rot([kb.sbuf(s2, "wd%d" % i, [128, 4, D], WDT) for i in range(3)])
            xr = Rot([kb.sbuf(s2, "xr%d" % i, [128, D], F32) for i in range(3)])
            xrb = Rot([kb.sbuf(s2, "xrb%d" % i, [128, D], BF16) for i in range(3)])
            xT = Rot([kb.sbuf(s2, "xT%d" % i, [128, 8, 128], BF16) for i in range(3)])
            hid = Rot([kb.sbuf(s2, "hid%d" % i, [128, 4, 128], BF16) for i in range(3)])
            sg_ = Rot([kb.sbuf(s2, "sgx%d" % i, [128, 4, 128], F32) for i in range(2)])
            yo = Rot([kb.sbuf(s2, "yo%d" % i, [128, D], F32) for i in range(3)])
            pst = Rot([kb.psum(s2, "ps_xt%d" % i, [128, 8, 128], BF16) for i in range(2)])
            psg = Rot([kb.psum(s2, "ps_g%d" % i, [128, 4, 128], F32) for i in range(2)])
            psu = Rot([kb.psum(s2, "ps_u%d" % i, [128, 4, 128], F32) for i in range(2)])
            psy = Rot([kb.psum(s2, "ps_yy%d" % i, [128, 512], F32) for i in range(2)])
            for j in range(dbg.get("nblk", NBLK)):
                wg_, wu_, wd_ = wg.next(), wu.next(), wd.next()
                NR = DEPTH * NE * 128
                idma(kb, wg_, wg_[:, :, :].rearrange("p c n -> p (c n)"), None, io.w_gate, wg_v, offs_gu[:, j, 0:1], extra_r=[offs_gu], nrows=NR)
                idma(kb, wu_, wu_[:, :, :].rearrange("p c n -> p (c n)"), None, io.w_up, wu_v, offs_gu[:, j, 0:1], extra_r=[offs_gu], nrows=NR)
                idma(kb, wd_, wd_[:, :, :].rearrange("p c n -> p (c n)"), None, io.w_down, wd_v, offs_gu[:, j, 0:1], extra_r=[offs_gu], nrows=NR)
                x = xr.next()
                dma(kb, "pool", x, x[:, :], xb, xb[j * 128:(j + 1) * 128, :])
                xb_ = xrb.next()
                cp(kb, "dve", xb_, xb_[:, :], x, x[:, :])
                xT_ = xT.next()
                pt = pst.next()
                xperm = xb_[:, :].rearrange("r (p c) -> r c p", c=8)
                for c in range(8):
                    tr(kb, pt, pt[:, c, :], xb_, xperm[:, c, :], C.id_b, C.id_b[:, :], inc=(c == 7))
                cp(kb, "act", xT_, xT_[:, :, :], pt, pt[:, :, :])
                pg, pu = psg.next(), psu.next()
                for m in range(4):
                    for c in range(8):
                        mm(kb, pg, pg[:, m, :], wg_, wg_[:, c, :].rearrange("k (p m) -> k m p", m=4)[:, m, :], xT_, xT_[:, c, :],
                           start=(c == 0), stop=(c == 7), inc=(c == 7 and m == 3))
                for m in range(4):
                    for c in range(8):
                        mm(kb, pu, pu[:, m, :], wu_, wu_[:, c, :].rearrange("k (p m) -> k m p", m=4)[:, m, :], xT_, xT_[:, c, :],
                           start=(c == 0), stop=(c == 7), inc=(c == 7 and m == 3))
                s_ = sg_.next()
                act(kb, s_, s_[:, :, :], pg, pg[:, :, :], AF.Silu)
                h_ = hid.next()
                tt(kb, "dve", h_, h_[:, :, :], s_, s_[:, :, :], pu, pu[:, :, :], ALU.mult)
                y_ = yo.next()
                for nh in range(2):
                    py = psy.next()
                    for c in range(4):
                        mm(kb, py, py[:, :], h_, h_[:, c, :], wd_, wd_[:, c, nh * 512:(nh + 1) * 512], start=(c == 0), stop=(c == 3))
                    cp(kb, "act", y_, y_[:, nh * 512:(nh + 1) * 512], py, py[:, :])
                dma(kb, "sp", yb, yb[j * 128:(j + 1) * 128, :], y_, y_[:, :])
            kb.barrier()
        with contextlib.ExitStack() as s2:
            y0 = Rot([kb.sbuf(s2, "gy0_%d" % i, [128, D], F32) for i in range(4)])
            y1 = Rot([kb.sbuf(s2, "gy1_%d" % i, [128, D], F32) for i in range(4)])
            hm = Rot([kb.sbuf(s2, "ghm%d" % i, [128, D], F32) for i in range(4)])

            def src(i, buf):
                a, b_, h_ = y0.next(), y1.next(), hm.next()
                idma(kb, a, a[:, :], None, yb, yb[:, :], destI[:, i, 0:1], extra_r=[destI], nrows=NROWS)
                idma(kb, b_, b_[:, :], None, yb, yb[:, :], destI[:, i, 1:2], extra_r=[destI], nrows=NROWS)
                dma(kb, "pool", h_, h_[:, :], io.h_mid[i], io.h_mid[i][i * 128:(i + 1) * 128, :])
                tsc(kb, "dve", a, a[:, :], a, a[:, :], gate[:, i, 0:1], None, ALU.mult, extra_r=[gate])
                stt(kb, "dve", a, a[:, :], b_, b_[:, :], gate[:, i, 1:2], a, a[:, :], ALU.mult, ALU.add, extra_r=[gate])
                stt(kb, "dve", buf, buf[:, :], h_, h_[:, :], float(DN_ALPHA), a, a[:, :], ALU.mult, ALU.add)
            emit_ln_rows(kb, C, s2, "l2_", src, (io.ln2_g, io.ln2_g[l:l + 1, :]), (io.ln2_b, io.ln2_b[l:l + 1, :]), io,
                         list(range(NT)), dst_tiles, nb=4)
            kb.barrier()


def build_program(dbg=None):
    dbg = dict(dbg or {})
    dbg.setdefault("serial", 0)
    kb = KB()
    io = declare_io(kb)
    with contextlib.ExitStack() as st:
        C = setup_consts(kb, st)
        stage_prologue(kb, C, io, list(range(NT)))
        stage_rope_tables(kb, C, io)
        depth = dbg.get("depth", DEPTH)
        for l in range(depth):
            stage_mixer(kb, C, io, l, list(range(NSEQ)), dbg)
            stage_moe(kb, C, io, l, io.h_res if l < depth - 1 else io.out, dbg)
        kb.finish()
    return kb


def kernel(**inputs):
    n = 8
    kb = build_program()
    in_maps = [make_in_map(inputs, c) for c in range(n)]
    res = run_bass_kernel_spmd(kb.nc, in_maps, core_ids=list(range(n)))
    out = np.concatenate([np.asarray(r["out"], dtype=np.float32).reshape(NSEQ, SEQ, D) for r in res.results], axis=0)
    return out
```

```python
import contextlib
import numpy as np
import concourse.bass as bass
import concourse.mybir as mybir
from concourse.bass_utils import run_bass_kernel_spmd

F32 = mybir.dt.float32
BF16 = mybir.dt.bfloat16
I32 = mybir.dt.int32
U32 = mybir.dt.uint32
AF = mybir.ActivationFunctionType
ALU = mybir.AluOpType
AX = mybir.AxisListType

D = 1024
SEQ = 2048
NSEQ = 2
T = NSEQ * SEQ
NT = T // 128
DEPTH = 2
H = 8
IN_DIM = 6880
OFF_Q, OFF_K, OFF_V, OFF_Z, OFF_A, OFF_BT, OFF_CQ, OFF_CKV, OFF_KR, OFF_G = (
    0, 1024, 2048, 3072, 4096, 4112, 4128, 4512, 4768, 4832)
QL, KVL, ROPE = 384, 256, 64
NE = 64
DEXP = 512
NBLK = 128
NROWS = NBLK * 128
DN_ALPHA = (2 * DEPTH) ** 0.25
LN_EPS = 1e-5
RMS_EPS = 1e-6
CH = 128
NCH = SEQ // CH


class Buf:
    def __init__(self, t, name):
        self.t = t
        self.name = name
        self.w = None
        self.r = {}

    def __getitem__(self, idx):
        return self.t[idx]


class KB:
    ENG = ("pe", "act", "dve", "pool", "sp")

    def __init__(self):
        self.nc = bass.Bass("TRN2", target_bir_lowering=False)
        nc = self.nc
        self.es = contextlib.ExitStack()
        self.eng = {"pe": nc.tensor, "act": nc.scalar, "dve": nc.vector, "pool": nc.gpsimd, "sp": nc.sync}
        self.sems = {}
        self.cnt = {}
        self.known = {e: {} for e in self.ENG}
        for e in self.ENG:
            self.sems[e] = self.es.enter_context(nc.semaphore("sem_" + e))
            self.cnt[e] = 0
        self.dq = {}
        for q, n in (("sp", 12), ("pool", 28), ("act", 2)):
            keys = []
            for i in range(n):
                k = "d_%s_%d" % (q, i)
                self.sems[k] = self.es.enter_context(nc.semaphore(k))
                self.cnt[k] = 0
                keys.append(k)
            self.dq[q] = [keys, 0]
        self.ninst = 0
        self.uid = 0
        self.scr = None
        self.serialize = False
        self.psum_rar = True
        self.chain = False
        self.chain_engines = ("pe", "act", "dve", "pool")
        self.last_op = None
        self.flush_nop = False

    def sbuf(self, stack, name, shape, dt):
        self.uid += 1
        name = "%s_u%d" % (name, self.uid)
        return Buf(stack.enter_context(self.nc.sbuf_tensor(name, list(shape), dt)), name)

    def psum(self, stack, name, shape, dt):
        self.uid += 1
        name = "%s_u%d" % (name, self.uid)
        b = Buf(stack.enter_context(self.nc.psum_tensor(name, list(shape), dt)), name)
        b.is_psum = True
        return b

    def dram(self, name, shape, dt, kind="Internal"):
        return Buf(self.nc.dram_tensor(name, list(shape), dt, kind=kind).ap(), name)

    def dram_tiles(self, name, shape, dt, n, kind="Internal"):
        ap = self.nc.dram_tensor(name, list(shape), dt, kind=kind).ap()
        return [Buf(ap, "%s_%d" % (name, i)) for i in range(n)]

    def _waits(self, en, r, w, is_dma=False):
        need = {}
        for b in r:
            if b.w is not None:
                need[b.w[0]] = max(need.get(b.w[0], 0), b.w[1])
            if self.psum_rar and getattr(b, "is_psum", False) and en != "pe":
                for sk, val in b.r.items():
                    if sk != en:
                        need[sk] = max(need.get(sk, 0), val)
        for b in w:
            if b.w is not None and (is_dma or b.w[0] != en):
                need[b.w[0]] = max(need.get(b.w[0], 0), b.w[1])
            for sk, val in b.r.items():
                if is_dma or sk != en:
                    need[sk] = max(need.get(sk, 0), val)
        E = self.eng[en]
        kn = self.known[en]
        for sk, val in need.items():
            if kn.get(sk, 0) < val:
                E.wait_ge(self.sems[sk], val)
                kn[sk] = val
                self.ninst += 1

    def _done(self, tok, r, w):
        for b in r:
            b.r[tok[0]] = max(b.r.get(tok[0], 0), tok[1])
        for b in w:
            b.w = tok
            b.r = {}

    def op(self, en, fn, r=(), w=(), inc=True):
        self._waits(en, r, w)
        if self.chain and en in self.chain_engines and self.last_op is not None and self.last_op[0] != en:
            lk, lv = self.last_op
            if self.known[en].get(lk, 0) < lv:
                self.eng[en].wait_ge(self.sems[lk], lv)
                self.known[en][lk] = lv
        ins = fn(self.eng[en])
        self.ninst += 1
        if inc:
            self.cnt[en] += 1
            ins.then_inc(self.sems[en], 1)
            self._done((en, self.cnt[en]), r, w)
        else:
            self._done((en, self.cnt[en] + 1), r, w)
        if en in self.chain_engines:
            self.last_op = (en, self.cnt[en] + (0 if inc else 1))
        if self.flush_nop and self.scr is not None and en in ("dve", "act") and any(getattr(b, "is_psum", False) for b in r):
            if en == "dve":
                self.eng[en].memset(self.scr[0:1, 0:1], 0.0)
            else:
                self.eng[en].memzero(self.scr[0:1, 2:3])
            self.ninst += 1
        if self.serialize and inc:
            self.barrier()
        return ins

    def dma(self, q, fn, r=(), w=()):
        keys, i = self.dq[q]
        k = keys[i % len(keys)]
        self.dq[q][1] = i + 1
        self._waits(q, r, w, True)
        kn = self.known[q]
        if kn.get(k, 0) < self.cnt[k]:
            self.eng[q].wait_ge(self.sems[k], self.cnt[k])
            kn[k] = self.cnt[k]
        ins = fn(self.eng[q])
        self.cnt[k] += 16
        ins.then_inc(self.sems[k], 16)
        self.ninst += 1
        self._done((k, self.cnt[k]), r, w)
        return ins

    def barrier(self):
        for en in self.ENG:
            kn = self.known[en]
            for sk, val in self.cnt.items():
                if sk != en and val > 0 and kn.get(sk, 0) < val:
                    self.eng[en].wait_ge(self.sems[sk], val)
                    kn[sk] = val

    def finish(self):
        kn = self.known["sp"]
        for sk, val in self.cnt.items():
            if sk != "sp" and val > 0 and kn.get(sk, 0) < val:
                self.nc.sync.wait_ge(self.sems[sk], val)
                kn[sk] = val
        self.es.close()


def mm(kb, ob, oap, lb, lap, rb, rap, start=True, stop=True, inc=None):
    if inc is None:
        inc = stop
    return kb.op("pe", lambda e: e.matmul(oap, lhsT=lap, rhs=rap, start=start, stop=stop), r=[lb, rb], w=[ob], inc=inc)


def tr(kb, ob, oap, ib, iap, idb, idap, inc=True):
    return kb.op("pe", lambda e: e.transpose(oap, iap, idap), r=[ib, idb], w=[ob], inc=inc)


def act(kb, ob, oap, ib, iap, func, bias=None, scale=None, extra_r=(), accum=None, en="act"):
    kw = {}
    if bias is not None:
        kw["bias"] = bias
    if scale is not None:
        kw["scale"] = scale
    w = [ob]
    if accum is not None:
        kw["accum_out"] = accum[1]
        w.append(accum[0])
    return kb.op("act", lambda e: e.activation(out=oap, in_=iap, func=func, **kw), r=[ib] + list(extra_r), w=w)


def tt(kb, en, ob, oap, ab, aap, bb, bap, op):
    return kb.op(en, lambda e: e.tensor_tensor(out=oap, in0=aap, in1=bap, op=op), r=[ab, bb], w=[ob])


def tsc(kb, en, ob, oap, ib, iap, s1, s2, op0, op1=None, extra_r=()):
    if op1 is None:
        return kb.op(en, lambda e: e.tensor_scalar(out=oap, in0=iap, scalar1=s1, scalar2=None, op0=op0),
                     r=[ib] + list(extra_r), w=[ob])
    return kb.op(en, lambda e: e.tensor_scalar(out=oap, in0=iap, scalar1=s1, scalar2=s2, op0=op0, op1=op1),
                 r=[ib] + list(extra_r), w=[ob])


def stt(kb, en, ob, oap, ab, aap, scalar, bb, bap, op0, op1, extra_r=()):
    return kb.op(en, lambda e: e.scalar_tensor_tensor(out=oap, in0=aap, scalar=scalar, in1=bap, op0=op0, op1=op1),
                 r=[ab, bb] + list(extra_r), w=[ob])


def cp(kb, en, ob, oap, ib, iap):
    if en == "act":
        return kb.op("act", lambda e: e.copy(oap, iap), r=[ib], w=[ob])
    return kb.op(en, lambda e: e.tensor_copy(oap, iap), r=[ib], w=[ob])


def memset(kb, en, ob, oap, val):
    return kb.op(en, lambda e: e.memset(oap, val), r=[], w=[ob])


def dma(kb, q, ob, oap, ib, iap):
    return kb.dma(q, lambda e: e.dma_start(out=oap, in_=iap), r=[ib], w=[ob])


class Consts:
    pass


def rsqrt(kb, C, ob, oap, ib, iap, eps):
    act(kb, ob, oap, ib, iap, AF.Sqrt, bias=C.eps_tile(eps)[0:oap.shape[0], 0:1], extra_r=[C.eps_buf])
    kb.op("dve", lambda e: e.reciprocal(oap, oap), r=[ob], w=[ob])


def setup_consts(kb, st):
    c = Consts()
    kb.scr = kb.sbuf(st, "kb_scr", [128, 8], F32).t
    c.ones_f = kb.sbuf(st, "ones_f", [128, 128], F32)
    c.ones_b = kb.sbuf(st, "ones_b", [128, 128], BF16)
    c.id_f = kb.sbuf(st, "id_f", [128, 128], F32)
    c.id_b = kb.sbuf(st, "id_b", [128, 128], BF16)
    c.id4_f = kb.sbuf(st, "id4_f", [128, 4, 128], F32)
    c.triF = kb.sbuf(st, "triF", [128, 128], F32)
    c.triB = kb.sbuf(st, "triB", [128, 128], F32)
    c.fill_zero = kb.nc.gpsimd.to_reg(0.0)
    c.fill_neg = kb.nc.gpsimd.to_reg(-30000.0)
    c.fill_pos = kb.nc.gpsimd.to_reg(30000.0)
    c.eps_buf = kb.sbuf(st, "eps_c", [128, 2], F32)
    memset(kb, "pool", c.eps_buf, c.eps_buf[:, 0:1], LN_EPS)
    memset(kb, "pool", c.eps_buf, c.eps_buf[:, 1:2], RMS_EPS)
    c.eps_tile = lambda eps: c.eps_buf[:, 0:1] if eps == LN_EPS else c.eps_buf[:, 1:2]
    memset(kb, "pool", c.ones_f, c.ones_f[:], 1.0)
    memset(kb, "pool", c.ones_b, c.ones_b[:], 1.0)
    kb.op("pool", lambda e: e.affine_select(out=c.id_f[:], in_=c.ones_f[:], pattern=[[-1, 128]],
                                            compare_op=ALU.is_equal, fill=c.fill_zero, base=0, channel_multiplier=1),
          r=[c.ones_f], w=[c.id_f])
    cp(kb, "pool", c.id_b, c.id_b[:], c.id_f, c.id_f[:])
    for i in range(4):
        cp(kb, "pool", c.id4_f, c.id4_f[:, i, :], c.id_f, c.id_f[:])
    kb.op("pool", lambda e: e.affine_select(out=c.triF[:], in_=c.ones_f[:], pattern=[[1, 128]],
                                            compare_op=ALU.is_ge, fill=c.fill_zero, base=0, channel_multiplier=-1),
          r=[c.ones_f], w=[c.triF])
    kb.op("pool", lambda e: e.affine_select(out=c.triB[:], in_=c.ones_f[:], pattern=[[-1, 128]],
                                            compare_op=ALU.is_ge, fill=c.fill_zero, base=0, channel_multiplier=1),
          r=[c.ones_f], w=[c.triB])
    zero4 = kb.sbuf(st, "zero4", [128, 4, 128], F32)
    memset(kb, "pool", zero4, zero4[:, :, :], 0.0)
    c.mT, c.mS = [], []
    for d in range(2):
        if d == 0:
            patT, cmT, pat, cm = [[0, 4], [1, 128]], -1, [[0, 4], [-1, 128]], 1
        else:
            patT, cmT, pat, cm = [[0, 4], [-1, 128]], 1, [[0, 4], [1, 128]], -1
        mT = kb.sbuf(st, "mT%d" % d, [128, 4, 128], F32)
        mS = kb.sbuf(st, "mS%d" % d, [128, 4, 128], F32)
        kb.op("pool", lambda e, mT=mT, patT=patT, cmT=cmT: e.affine_select(
            out=mT[:, :, :], in_=zero4[:, :, :], pattern=patT, compare_op=ALU.is_ge, fill=c.fill_neg, base=0,
            channel_multiplier=cmT), r=[zero4], w=[mT])
        kb.op("pool", lambda e, mS=mS, pat=pat, cm=cm: e.affine_select(
            out=mS[:, :, :], in_=zero4[:, :, :], pattern=pat, compare_op=ALU.is_ge, fill=c.fill_pos, base=-1,
            channel_multiplier=cm), r=[zero4], w=[mS])
        c.mT.append(mT)
        c.mS.append(mS)
    kb.barrier()
    return c


def sin_reduced(kb, st, nm, ob, oap, ib, iap, shift, shape):
    u = kb.sbuf(st, nm + "_u", shape, F32)
    ki = kb.sbuf(st, nm + "_ki", shape, I32)
    kf = kb.sbuf(st, nm + "_kf", shape, F32)
    mk = kb.sbuf(st, nm + "_mk", shape, F32)
    full = tuple(slice(None) for _ in shape)
    inv2pi = float(1.0 / (2.0 * np.pi))
    tsc(kb, "dve", u, u[full], ib, iap, inv2pi, float(shift) * inv2pi, ALU.mult, ALU.add)
    cp(kb, "dve", ki, ki[full], u, u[full])
    cp(kb, "dve", kf, kf[full], ki, ki[full])
    tt(kb, "dve", u, u[full], u, u[full], kf, kf[full], ALU.subtract)
    tsc(kb, "dve", mk, mk[full], u, u[full], 0.5, None, ALU.is_gt)
    tt(kb, "dve", u, u[full], u, u[full], mk, mk[full], ALU.subtract)
    tsc(kb, "dve", mk, mk[full], u, u[full], -0.5, None, ALU.is_lt)
    tt(kb, "dve", u, u[full], u, u[full], mk, mk[full], ALU.add)
    tsc(kb, "dve", u, u[full], u, u[full], -0.4999999, 0.4999999, ALU.max, ALU.min)
    act(kb, ob, oap, u, u[full], AF.Sin, scale=float(2.0 * np.pi))


def layernorm_tile(kb, wk, x_b, g_bc, b_bc, out_b, eps=LN_EPS):
    stats, mv, rstd = wk["stats"], wk["mv"], wk["rstd"]
    for ci in range(2):
        kb.op("dve", lambda e, ci=ci: e.bn_stats(stats[:, ci, :], x_b[:, ci * 512:(ci + 1) * 512]), r=[x_b], w=[stats])
    kb.op("dve", lambda e: e.bn_aggr(mv[:, :], stats[:, :, :]), r=[stats], w=[mv])
    rsqrt(kb, wk["C"], rstd, rstd[:, :], mv, mv[:, 1:2], eps)
    tsc(kb, "dve", out_b, out_b[:, :], x_b, x_b[:, :], mv[:, 0:1], rstd[:, 0:1], ALU.subtract, ALU.mult,
        extra_r=[mv, rstd])
    tt(kb, "dve", out_b, out_b[:, :], out_b, out_b[:, :], g_bc, g_bc[:, :], ALU.mult)
    tt(kb, "dve", out_b, out_b[:, :], out_b, out_b[:, :], b_bc, b_bc[:, :], ALU.add)


class IO:
    pass


def declare_io(kb, dbg=None, small=False):
    nc = kb.nc
    io = IO()

    def inp(name, shape, dt=F32):
        b = Buf(nc.dram_tensor(name, list(shape), dt, kind="ExternalInput").ap(), name)
        setattr(io, name, b)
        return b
    inp("x", [T, D])
    inp("positions", [1, T], I32)
    inp("ln_in_g", [1, D]); inp("ln_in_b", [1, D])
    inp("w_in", [DEPTH, D, IN_DIM])
    inp("conv_w", [DEPTH, 5, 3072])
    inp("a_log", [DEPTH, 1, 16]); inp("dt_bias", [DEPTH, 1, 16])
    inp("gdn_norm_w", [DEPTH, 128, 1])
    inp("w_o_gdn", [DEPTH, D, D])
    inp("mla_q_norm_w", [DEPTH, QL, 1])
    inp("w_uq", [DEPTH, QL, 1536])
    inp("mla_kv_norm_w", [DEPTH, KVL, 1])
    inp("w_ukv", [DEPTH, KVL, 2048])
    inp("w_o_mla", [DEPTH, D, D])
    inp("w_out", [DEPTH, D, D])
    inp("ln1_g", [DEPTH, D]); inp("ln1_b", [DEPTH, D])
    inp("w_router_group", [DEPTH, D, 8]); inp("b_router_group", [DEPTH, 8])
    inp("w_router_expert", [DEPTH, D, NE]); inp("b_router_expert", [DEPTH, NE])
    if not small:
        inp("w_gate", [DEPTH, NE, D, DEXP]); inp("w_up", [DEPTH, NE, D, DEXP]); inp("w_down", [DEPTH, NE, DEXP, D])
    inp("ln2_g", [DEPTH, D]); inp("ln2_b", [DEPTH, D])
    io.out = kb.dram_tiles("out", [T, D], F32, NT, kind="ExternalOutput")
    io.h_res = kb.dram_tiles("h_res", [T, D], F32, NT)
    io.h_mid = kb.dram_tiles("h_mid", [T, D], F32, NT)
    io.hT = kb.dram_tiles("hT_scr", [128, 8, T], BF16, NT)
    io.cs = kb.dram("cs_scr", [2, 64, T], F32)
    io.xb = kb.dram("xb_scr", [NROWS, D], F32)
    io.yb = kb.dram("yb_scr", [NROWS, D], F32)
    io.og_scr = kb.dram("og_scr", [NSEQ, 128, 8, SEQ], BF16)
    io.om_scr = kb.dram("om_scr", [NSEQ, 128, 8, SEQ], BF16)
    io.dbg = {}
    for name, shape in (dbg or {}).items():
        io.dbg[name] = Buf(nc.dram_tensor(name, list(shape), F32, kind="ExternalOutput").ap(), name)
    return io


def emit_ln_rows(kb, C, st, nm, src_fn, g_ap, b_ap, io, tiles, h_dst, pre_fn=None, nb=2):
    g_bc = kb.sbuf(st, nm + "g", [128, D], F32)
    b_bc = kb.sbuf(st, nm + "b", [128, D], F32)
    dma(kb, "sp", g_bc, g_bc[:, :], g_ap[0], g_ap[1].partition_broadcast(128))
    dma(kb, "sp", b_bc, b_bc[:, :], b_ap[0], b_ap[1].partition_broadcast(128))
    NB = nb
    xt = [kb.sbuf(st, nm + "x%d" % j, [128, D], F32) for j in range(NB)]
    ht = [kb.sbuf(st, nm + "h%d" % j, [128, D], F32) for j in range(NB)]
    hb = [kb.sbuf(st, nm + "hb%d" % j, [128, D], BF16) for j in range(NB)]
    hTt = [kb.sbuf(st, nm + "hT%d" % j, [128, 8, 128], BF16) for j in range(NB)]
    pst = [kb.psum(st, nm + "ps%d" % j, [128, 8, 128], BF16) for j in range(NB)]
    wk = [dict(C=C, stats=kb.sbuf(st, nm + "st%d" % j, [128, 2, 6], F32), mv=kb.sbuf(st, nm + "mv%d" % j, [128, 2], F32),
               rstd=kb.sbuf(st, nm + "rs%d" % j, [128, 1], F32)) for j in range(NB)]
    for n, i in enumerate(tiles):
        j = n % NB
        src_fn(i, xt[j])
        layernorm_tile(kb, wk[j], xt[j], g_bc, b_bc, ht[j])
        dma(kb, "sp", h_dst[i], h_dst[i][i * 128:(i + 1) * 128, :], ht[j], ht[j][:, :])
        cp(kb, "act", hb[j], hb[j][:, :], ht[j], ht[j][:, :])
        for c in range(8):
            tr(kb, pst[j], pst[j][:, c, :], hb[j], hb[j][:, c * 128:(c + 1) * 128], C.id_b, C.id_b[:, :], inc=(c == 7))
        cp(kb, "dve", hTt[j], hTt[j][:, :, :], pst[j], pst[j][:, :, :])
        dma(kb, "sp", io.hT[i], io.hT[i][:, :, i * 128:(i + 1) * 128], hTt[j], hTt[j][:, :, :])


def stage_prologue(kb, C, io, tiles):
    with contextlib.ExitStack() as st:
        def src(i, buf):
            dma(kb, "pool", buf, buf[:, :], io.x, io.x[i * 128:(i + 1) * 128, :])
        emit_ln_rows(kb, C, st, "p_", src, (io.ln_in_g, io.ln_in_g[0:1, :]), (io.ln_in_b, io.ln_in_b[0:1, :]), io,
                     tiles, io.h_res, nb=4)
        kb.barrier()


def make_in_map(inp, core, small=False):
    f = lambda k: np.ascontiguousarray(np.asarray(inp[k], dtype=np.float32))
    m = {}
    m["x"] = f("x")[NSEQ * core:NSEQ * (core + 1)].reshape(T, D)
    m["positions"] = np.ascontiguousarray(np.asarray(inp["positions"], dtype=np.int32)[NSEQ * core:NSEQ * (core + 1)].reshape(1, T))
    m["ln_in_g"] = f("ln_in_g").reshape(1, D)
    m["ln_in_b"] = f("ln_in_b").reshape(1, D)
    m["a_log"] = f("a_log").reshape(DEPTH, 1, 16)
    m["dt_bias"] = f("dt_bias").reshape(DEPTH, 1, 16)
    m["gdn_norm_w"] = f("gdn_norm_w").reshape(DEPTH, 128, 1)
    m["mla_q_norm_w"] = f("mla_q_norm_w").reshape(DEPTH, QL, 1)
    m["mla_kv_norm_w"] = f("mla_kv_norm_w").reshape(DEPTH, KVL, 1)
    for k in ("w_in", "conv_w", "w_o_gdn", "w_uq", "w_ukv", "w_o_mla", "w_out", "ln1_g", "ln1_b", "w_router_group",
              "b_router_group", "w_router_expert", "b_router_expert", "w_gate", "w_up", "w_down", "ln2_g", "ln2_b"):
        if small and k in ("w_gate", "w_up", "w_down"):
            continue
        m[k] = f(k)
    return m


class Rot:
    def __init__(self, bufs):
        self.bufs = bufs
        self.i = 0

    def next(self):
        b = self.bufs[self.i % len(self.bufs)]
        self.i += 1
        return b


def load_w_fm(kb, io_w, w_ap, wt):
    kb.dma("pool", lambda e: e.dma_start(out=wt[:, :, :], in_=w_ap.rearrange("(k p) m -> p k m", p=128)),
           r=[io_w], w=[wt])


def gdn_seq(kb, C, io, l, b, load_hT, og_scr, cwT, dbg):
    w_in = io.w_in
    with contextlib.ExitStack() as st:
        g_tm = kb.sbuf(st, "g_tm", [128, 2, NCH, 8], F32)
        be_tm = kb.sbuf(st, "be_tm", [128, 2, NCH, 8], F32)
        b_tm = kb.sbuf(st, "b_tm", [128, 2, NCH, 8], F32)
        nbe_tm = kb.sbuf(st, "nbe_tm", [128, 2, NCH, 8], F32)
        v4 = lambda x: x[:, :, :, :].rearrange("p d c h -> p c d h")
        with contextlib.ExitStack() as s2:
            hT_b = load_hT(s2)
            wab = kb.sbuf(s2, "wab", [128, 8, 32], BF16)
            load_w_fm(kb, w_in, w_in[l, :, OFF_A:OFF_A + 32], wab)
            alog = kb.sbuf(s2, "alog", [128, 16], F32)
            dtb = kb.sbuf(s2, "dtb", [128, 16], F32)
            dma(kb, "sp", alog, alog[:, :], io.a_log, io.a_log[l, 0:1, :].partition_broadcast(128))
            dma(kb, "sp", dtb, dtb[:, :], io.dt_bias, io.dt_bias[l, 0:1, :].partition_broadcast(128))
            nea = kb.sbuf(s2, "nea", [128, 16], F32)
            act(kb, nea, nea[:, :], alog, alog[:, :], AF.Exp)
            tsc(kb, "dve", nea, nea[:, :], nea, nea[:, :], -1.0, None, ALU.mult)
            ps = kb.psum(s2, "ps_ab", [128, NCH, 32], F32)
            for ci in range(NCH):
                for k in range(8):
                    mm(kb, ps, ps[:, ci, :], hT_b, hT_b[:, k, ci * 128:(ci + 1) * 128], wab, wab[:, k, :],
                       start=(k == 0), stop=(k == 7))
            tmp = kb.sbuf(s2, "ab_tmp", [128, NCH, 16], F32)
            tmp4 = tmp[:, :, :].rearrange("p c (d h) -> p c d h", d=2)
            tt(kb, "dve", tmp, tmp[:, :, :], ps, ps[:, :, 0:16], dtb, dtb[:, :].unsqueeze(1).to_broadcast([128, NCH, 16]), ALU.add)
            act(kb, tmp, tmp[:, :, :], tmp, tmp[:, :, :], AF.Exp)
            act(kb, tmp, tmp[:, :, :], tmp, tmp[:, :, :], AF.Ln, bias=1.0)
            tt(kb, "dve", g_tm, v4(g_tm), tmp, tmp4, nea,
               nea[:, :].rearrange("p (d h) -> p d h", d=2).unsqueeze(1).to_broadcast([128, NCH, 2, 8]), ALU.mult)
            act(kb, be_tm, v4(be_tm), ps, ps[:, :, 16:32].rearrange("p c (d h) -> p c d h", d=2), AF.Sigmoid)
            tsc(kb, "dve", nbe_tm, nbe_tm[:, :, :, :], be_tm, be_tm[:, :, :, :], -1.0, None, ALU.mult)
            ps2 = kb.psum(s2, "ps_b", [128, 2, NCH * 8], F32)
            mm(kb, ps2, ps2[:, 0, :], C.triF, C.triF[:, :], g_tm, g_tm[:, 0, :, :].rearrange("p c h -> p (c h)"))
            mm(kb, ps2, ps2[:, 1, :], C.triB, C.triB[:, :], g_tm, g_tm[:, 1, :, :].rearrange("p c h -> p (c h)"))
            cp(kb, "dve", b_tm, b_tm[:, :, :, :].rearrange("p d c h -> p d (c h)"), ps2, ps2[:, :, :])
            kb.barrier()
        for hg in range(2):
            gdn_group(kb, C, io, l, b, hg, load_hT, og_scr, cwT, g_tm, (be_tm, nbe_tm), b_tm, dbg)
        kb.barrier()


def gdn_group(kb, C, io, l, b, hg, load_hT, og_scr, cwT, g_tm, be_tm, b_tm, dbg):
    w_in = io.w_in
    h0 = hg * 4
    with contextlib.ExitStack() as st:
        qT = kb.sbuf(st, "qT", [128, 4, SEQ], BF16)
        kT = kb.sbuf(st, "kT", [128, 4, SEQ], BF16)
        ktok = kb.sbuf(st, "ktok", [128, NCH, 4, 128], BF16)
        vtok = kb.sbuf(st, "vtok", [128, NCH, 4, 128], BF16)
        with contextlib.ExitStack() as s2:
            hT_b = load_hT(s2)
            wts = Rot([kb.sbuf(s2, "wqkv%d" % i, [128, 8, 128], BF16) for i in range(3)])
            raw = Rot([kb.sbuf(s2, "raw%d" % i, [128, SEQ + 4], F32) for i in range(3)])
            acc = Rot([kb.sbuf(s2, "acc%d" % i, [128, SEQ], F32) for i in range(3)])
            vT = kb.sbuf(s2, "vT", [128, SEQ], BF16)
            sq = kb.sbuf(s2, "sq", [128, SEQ], F32)
            rs = kb.sbuf(s2, "rs", [128, 512], F32)
            pss = Rot([kb.psum(s2, "ps_p%d" % i, [128, 512], F32) for i in range(4)])
            psn = Rot([kb.psum(s2, "ps_n%d" % i, [128, 512], F32) for i in range(2)])
            pst = Rot([kb.psum(s2, "ps_t%d" % i, [128, 8, 128], BF16) for i in range(2)])
            def item(which, off, hh):
                h = h0 + hh
                ct = (off // 128) + h
                wt = wts.next()
                load_w_fm(kb, w_in, w_in[l, :, off + h * 128: off + (h + 1) * 128], wt)
                rw = raw.next()
                memset(kb, "pool", rw, rw[:, 0:2], 0.0)
                memset(kb, "pool", rw, rw[:, SEQ + 2:SEQ + 4], 0.0)
                for n in range(4):
                    ps = pss.next()
                    for k in range(8):
                        mm(kb, ps, ps[:, :], wt, wt[:, k, :], hT_b, hT_b[:, k, n * 512:(n + 1) * 512],
                           start=(k == 0), stop=(k == 7))
                    cp(kb, "act", rw, rw[:, 2 + n * 512: 2 + (n + 1) * 512], ps, ps[:, :])
                ac = acc.next()
                en = "dve"
                tsc(kb, en, ac, ac[:, :], rw, rw[:, 0:SEQ], cwT[:, ct, 0:1], None, ALU.mult, extra_r=[cwT])
                for j in range(1, 5):
                    stt(kb, en, ac, ac[:, :], rw, rw[:, j:j + SEQ], cwT[:, ct, j:j + 1], ac, ac[:, :],
                        ALU.mult, ALU.add, extra_r=[cwT])
                yield
                if which == "v":
                    act(kb, vT, vT[:, :], ac, ac[:, :], AF.Silu)
                    for half in range(2):
                        pt = pst.next()
                        for ci in range(8):
                            cc = half * 8 + ci
                            tr(kb, pt, pt[:, ci, :], vT, vT[:, cc * 128:(cc + 1) * 128], C.id_b, C.id_b[:, :], inc=(ci == 7))
                        cp(kb, "act", vtok, vtok[:, half * 8:(half + 1) * 8, hh, :], pt, pt[:, :, :])
                else:
                    act(kb, ac, ac[:, :], ac, ac[:, :], AF.Silu)
                    tt(kb, "pool", sq, sq[:, :], ac, ac[:, :], ac, ac[:, :], ALU.mult)
                    dst = qT if which == "q" else kT
                    for n in range(4):
                        pn = psn.next()
                        mm(kb, pn, pn[:, :], C.ones_f, C.ones_f[:, :], sq, sq[:, n * 512:(n + 1) * 512])
                        cp(kb, "act", rs, rs[:, :], pn, pn[:, :])
                        rsqrt(kb, C, rs, rs[:, :], rs, rs[:, :], RMS_EPS)
                        if which == "q":
                            stt(kb, "dve", dst, dst[:, hh, n * 512:(n + 1) * 512], ac, ac[:, n * 512:(n + 1) * 512],
                                float(128 ** -0.5), rs, rs[:, :], ALU.mult, ALU.mult)
                        else:
                            tt(kb, "dve", dst, dst[:, hh, n * 512:(n + 1) * 512], ac, ac[:, n * 512:(n + 1) * 512],
                               rs, rs[:, :], ALU.mult)
                    if which == "k":
                        for half in range(2):
                            pt = pst.next()
                            for ci in range(8):
                                cc = half * 8 + ci
                                tr(kb, pt, pt[:, ci, :], kT, kT[:, hh, cc * 128:(cc + 1) * 128], C.id_b, C.id_b[:, :], inc=(ci == 7))
                            cp(kb, "act", ktok, ktok[:, half * 8:(half + 1) * 8, hh, :], pt, pt[:, :, :])

            gens = [item(which, off, hh) for which, off in (("q", OFF_Q), ("k", OFF_K), ("v", OFF_V)) for hh in range(4)]
            next(gens[0])
            for gi in range(len(gens)):
                if gi + 1 < len(gens):
                    next(gens[gi + 1])
                for _ in gens[gi]:
                    pass
            kb.barrier()
        oT = kb.sbuf(st, "oT", [128, 4, SEQ], F32)
        if dbg.get("skip_scan"):
            memset(kb, "pool", oT, oT[:, :, :], 1.0)
            if "dump" in dbg and hg == 0:
                o = dbg["dump"]
                for j, src in enumerate((qT, kT)):
                    for hh in range(4):
                        cp(kb, "dve", oT, oT[:, hh, :], src, src[:, hh, :])
                    dma(kb, "sp", o, o[j, :, :, :], oT, oT[:, :, :])
                for j, src in enumerate((ktok, vtok)):
                    cp(kb, "dve", oT, oT[:, :, :].rearrange("p a (b c) -> p b a c", b=NCH), src, src[:, :, :, :])
                    dma(kb, "sp", o, o[2 + j, :, :, :], oT, oT[:, :, :])
                memset(kb, "pool", oT, oT[:, :, :], 1.0)
        else:
            if "scan_stop" in dbg:
                memset(kb, "pool", oT, oT[:, :, :], 1.0)
            gdn_scan(kb, C, b, h0, qT, kT, ktok, vtok, oT, g_tm, be_tm, b_tm, dbg)
        with contextlib.ExitStack() as s2:
            hT_b = load_hT(s2)
            ogb = Rot([kb.sbuf(s2, "ogb%d" % i, [128, SEQ], BF16) for i in range(2)])
            nw = kb.sbuf(s2, "gnw", [128, 1], F32)
            dma(kb, "sp", nw, nw[:, :], io.gdn_norm_w, io.gdn_norm_w[l, :, :])
            wts = Rot([kb.sbuf(s2, "wz%d" % i, [128, 8, 128], BF16) for i in range(2)])
            pss = Rot([kb.psum(s2, "ps_z%d" % i, [128, 512], F32) for i in range(2)])
            psn = Rot([kb.psum(s2, "ps_zn%d" % i, [128, 512], F32) for i in range(2)])
            sq = kb.sbuf(s2, "sqo", [128, 512], F32)
            rs = kb.sbuf(s2, "rso", [128, 512], F32)
            zs = kb.sbuf(s2, "zs", [128, 512], F32)
            for hh in range(4):
                h = h0 + hh
                wt = wts.next()
                og_ = ogb.next()
                load_w_fm(kb, w_in, w_in[l, :, OFF_Z + h * 128: OFF_Z + (h + 1) * 128], wt)
                for n in range(4):
                    sl = slice(n * 512, (n + 1) * 512)
                    ps = pss.next()
                    for k in range(8):
                        mm(kb, ps, ps[:, :], wt, wt[:, k, :], hT_b, hT_b[:, k, sl], start=(k == 0), stop=(k == 7))
                    act(kb, zs, zs[:, :], ps, ps[:, :], AF.Silu)
                    tt(kb, "pool", sq, sq[:, :], oT, oT[:, hh, sl], oT, oT[:, hh, sl], ALU.mult)
                    pn = psn.next()
                    mm(kb, pn, pn[:, :], C.ones_f, C.ones_f[:, :], sq, sq[:, :])
                    tsc(kb, "dve", rs, rs[:, :], pn, pn[:, :], 1.0 / 128.0, None, ALU.mult)
                    rsqrt(kb, C, rs, rs[:, :], rs, rs[:, :], RMS_EPS)
                    tt(kb, "dve", rs, rs[:, :], rs, rs[:, :], zs, zs[:, :], ALU.mult)
                    stt(kb, "dve", og_, og_[:, sl], oT, oT[:, hh, sl], nw[:, 0:1], rs, rs[:, :], ALU.mult, ALU.mult,
                        extra_r=[nw])
                dma(kb, "sp", og_scr, og_scr[b, :, h, :], og_, og_[:, :])
            kb.barrier()


def gdn_scan(kb, C, b, h0, qT, kT, ktok, vtok, oT, g_tm, be_pair, b_tm, dbg):
    be_tm, nbe_tm = be_pair
    kb.serialize = (dbg.get("serial") == 1)
    kb.chain = (dbg.get("serial") in (3, 4, 5))
    kb.chain_engines = {3: ("pe", "act", "dve", "pool"), 4: ("act", "dve", "pool"), 5: ("pe", "dve", "pool")}.get(dbg.get("serial"), ())
    kb.last_op = None
    kb.psum_rar = bool(dbg.get("psum_rar", 1))
    phase_bar = (lambda: kb.barrier()) if dbg.get("serial") == 2 else (lambda: None)
    with contextlib.ExitStack() as st:
        psA = Rot([kb.psum(st, "ps_s%d" % i, [128, 4, 128], F32) for i in range(7)])
        psT = kb.psum(st, "ps_sT", [128, 8, 128], BF16)
        S = kb.sbuf(st, "S", [128, 8, 128], F32)
        Sb = kb.sbuf(st, "Sb", [128, 8, 128], BF16)
        memset(kb, "dve", S, S[:, :, :], 0.0)
        memset(kb, "dve", Sb, Sb[:, :, :], 0.0)
        R2 = lambda nm, shape, dt, n=2: Rot([kb.sbuf(st, "%s%d" % (nm, i), shape, dt) for i in range(n)])
        diag = R2("diag", [128, 4, 128], F32)
        E = R2("E", [128, 4, 128], F32)
        diff = R2("diff", [128, 4, 128], F32)
        sel = R2("sel", [128, 4, 128], F32, 4)
        GT = R2("GT", [128, 4, 128], F32)
        G = R2("G", [128, 4, 128], F32)
        PT = R2("PT", [128, 4, 128], BF16)
        tmpf = R2("tmpf", [128, 4, 128], F32, 3)
        p_r = R2("p", [128, 8, 128], BF16)
        pT_r = R2("pT", [128, 8, 128], BF16)
        tTf = kb.sbuf(st, "tTf", [128, 8, 128], F32)
        tTb = R2("tTb", [128, 8, 128], BF16)
        kbT = R2("kbT", [128, 4, 128], BF16)
        qdT = R2("qdT", [128, 4, 128], BF16)
        Rb = R2("Rb", [128, 4, 128], BF16)
        Ub = R2("Ub", [128, 4, 128], BF16)
        Kd = R2("Kd", [128, 4, 128], BF16)
        wcol = R2("wcol", [128, 4], F32)
        for i in range(dbg.get("nsteps", NCH)):
            chunk = (i, NCH - 1 - i)
            Ed, dfd, PTd = [None, None], [None, None], [None, None]
            p = p_r.next()
            pT = pT_r.next()
            def prep(d):
                c = chunk[d]
                cs = slice(c * 128, (c + 1) * 128)
                bcol = b_tm[:, d, c, h0:h0 + 4]
                kk = psA.next()
                qk = psA.next()
                for hh in range(4):
                    mm(kb, kk, kk[:, hh, :], kT, kT[:, hh, cs], kT, kT[:, hh, cs], inc=(hh == 3))
                for hh in range(4):
                    mm(kb, qk, qk[:, hh, :], kT, kT[:, hh, cs], qT, qT[:, hh, cs], inc=(hh == 3))
                yield
                dg = diag.next()
                tt(kb, "dve", dg, dg[:, :, :], C.id4_f, C.id4_f[:, :, :], b_tm, bcol.unsqueeze(2).to_broadcast([128, 4, 128]), ALU.mult)
                yield
                Db = psA.next()
                mm(kb, Db, Db[:, :, :], C.ones_f, C.ones_f[:, :], dg, dg[:, :, :])
                yield
                Ed[d] = E.next()
                act(kb, Ed[d], Ed[d][:, :, :], Db, Db[:, :, :], AF.Exp)
                yield
                dfd[d] = diff.next()
                tt(kb, "dve", dfd[d], dfd[d][:, :, :], Db, Db[:, :, :], b_tm, bcol.unsqueeze(2).to_broadcast([128, 4, 128]), ALU.subtract)
                s1 = sel.next()
                tt(kb, "dve", s1, s1[:, :, :], dfd[d], dfd[d][:, :, :], C.mT[d], C.mT[d][:, :, :], ALU.add)
                s2 = sel.next()
                tt(kb, "dve", s2, s2[:, :, :], dfd[d], dfd[d][:, :, :], C.mS[d], C.mS[d][:, :, :], ALU.add)
                yield
                gt = GT.next()
                act(kb, gt, gt[:, :, :], s1, s1[:, :, :], AF.Exp)
                g = G.next()
                act(kb, g, g[:, :, :], s2, s2[:, :, :], AF.Exp, scale=-1.0)
                yield
                PTd[d] = PT.next()
                tt(kb, "dve", PTd[d], PTd[d][:, :, :], qk, qk[:, :, :], gt, gt[:, :, :], ALU.mult)
                tf = tmpf.next()
                tt(kb, "dve", tf, tf[:, :, :], kk, kk[:, :, :], g, g[:, :, :], ALU.mult)
                nbecol = nbe_tm[:, d, c, h0:h0 + 4]
                tt(kb, "dve", p, p[:, d * 4:(d + 1) * 4, :], tf, tf[:, :, :], nbe_tm,
                   nbecol.unsqueeze(2).to_broadcast([128, 4, 128]), ALU.mult)
                yield
            for _ in zip(prep(0), prep(1)):
                pass
            if dbg.get("scan_stop", 9) <= 1:
                continue
            phase_bar()
            for j in range(8):
                tr(kb, psT, psT[:, j, :], p, p[:, j, :], C.id_b, C.id_b[:, :], inc=(j == 7))
            cp(kb, "act", pT, pT[:, :, :], psT, psT[:, :, :])
            tt(kb, "dve", tTf, tTf[:, :, :], psT, psT[:, :, :], C.id_f, C.id_f[:, None, :].to_broadcast([128, 8, 128]), ALU.add)
            tb = tTb.next()
            cp(kb, "act", tb, tb[:, :, :], tTf, tTf[:, :, :])
            if dbg.get("scan_stop", 9) <= 2:
                continue
            phase_bar()
            for it in range(6):
                pn = p_r.next()
                pa = [psA.next(), psA.next()]
                for j in range(8):
                    mm(kb, pa[j // 4], pa[j // 4][:, j % 4, :], pT, pT[:, j, :], p, p[:, j, :], inc=(j % 4 == 3))
                if it < 5:
                    pTn = pT_r.next()
                    pb = [psA.next(), psA.next()]
                    for j in range(8):
                        mm(kb, pb[j // 4], pb[j // 4][:, j % 4, :], p, p[:, j, :], pT, pT[:, j, :], inc=(j % 4 == 3))
                for hf in range(2):
                    cp(kb, "act", pn, pn[:, hf * 4:(hf + 1) * 4, :], pa[hf], pa[hf][:, :, :])
                if it < 5:
                    for hf in range(2):
                        cp(kb, "dve", pTn, pTn[:, hf * 4:(hf + 1) * 4, :], pb[hf], pb[hf][:, :, :])
                phase_bar()
                pu = [psA.next(), psA.next()]
                for j in range(8):
                    mm(kb, pu[j // 4], pu[j // 4][:, j % 4, :], pn, pn[:, j, :], tb, tb[:, j, :], inc=(j % 4 == 3))
                for hf in range(2):
                    tt(kb, "dve", tTf, tTf[:, hf * 4:(hf + 1) * 4, :], tTf, tTf[:, hf * 4:(hf + 1) * 4, :],
                       pu[hf], pu[hf][:, :, :], ALU.add)
                tb = tTb.next()
                cp(kb, "act", tb, tb[:, :, :], tTf, tTf[:, :, :])
                phase_bar()
                p = pn
                if it < 5:
                    pT = pTn
            if dbg.get("scan_stop", 9) <= 3:
                continue
            def chain(d):
                c = chunk[d]
                cs = slice(c * 128, (c + 1) * 128)
                last = 127 if d == 0 else 0
                becol = be_tm[:, d, c, h0:h0 + 4]
                kb_ = kbT.next()
                tt(kb, "dve", kb_, kb_[:, :, :], kT, kT[:, :, cs], Ed[d], Ed[d][:, :, :], ALU.mult)
                qd_ = qdT.next()
                tt(kb, "dve", qd_, qd_[:, :, :], qT, qT[:, :, cs], Ed[d], Ed[d][:, :, :], ALU.mult)
                yield
                pr = psA.next()
                for hh in range(4):
                    mm(kb, pr, pr[:, hh, :], kb_, kb_[:, hh, :], Sb, Sb[:, d * 4 + hh, :], inc=(hh == 3))
                yield
                tf = tmpf.next()
                tt(kb, "dve", tf, tf[:, :, :], vtok, vtok[:, c, :, :], pr, pr[:, :, :], ALU.subtract)
                rb = Rb.next()
                tt(kb, "dve", rb, rb[:, :, :], tf, tf[:, :, :], be_tm, becol.unsqueeze(2).to_broadcast([128, 4, 128]), ALU.mult)
                yield
                pu = psA.next()
                for hh in range(4):
                    mm(kb, pu, pu[:, hh, :], tb, tb[:, d * 4 + hh, :], rb, rb[:, hh, :], inc=(hh == 3))
                yield
                ub = Ub.next()
                cp(kb, "act", ub, ub[:, :, :], pu, pu[:, :, :])
                yield
                po = psA.next()
                for hh in range(4):
                    mm(kb, po, po[:, hh, :], Sb, Sb[:, d * 4 + hh, :], qd_, qd_[:, hh, :], start=True, stop=False)
                    mm(kb, po, po[:, hh, :], ub, ub[:, hh, :], PTd[d], PTd[d][:, hh, :], start=False, stop=True, inc=(hh == 3))
                yield
                first = (d == 0) == (c < NCH // 2)
                if first:
                    cp(kb, "act", oT, oT[:, :, cs], po, po[:, :, :])
                else:
                    tt(kb, "dve", oT, oT[:, :, cs], oT, oT[:, :, cs], po, po[:, :, :], ALU.add)
                wc = wcol.next()
                act(kb, wc, wc[:, :], dfd[d], dfd[d][:, :, last], AF.Exp)
                kd = Kd.next()
                tt(kb, "dve", kd, kd[:, :, :], ktok, ktok[:, c, :, :], wc, wc[:, :].unsqueeze(2).to_broadcast([128, 4, 128]), ALU.mult)
                yield
                psn = psA.next()
                for hh in range(4):
                    mm(kb, psn, psn[:, hh, :], kd, kd[:, hh, :], ub, ub[:, hh, :], inc=(hh == 3))
                yield
                Sd = S[:, d * 4:(d + 1) * 4, :]
                tt(kb, "dve", S, Sd, S, Sd, Ed[d], Ed[d][:, :, last:last + 1].to_broadcast([128, 4, 128]), ALU.mult)
                tt(kb, "dve", S, Sd, S, Sd, psn, psn[:, :, :], ALU.add)
                cp(kb, "act", Sb, Sb[:, d * 4:(d + 1) * 4, :], S, Sd)
                yield
            for _ in zip(chain(0), chain(1)):
                pass
        kb.serialize = False
        kb.chain = False
        kb.barrier()


def stage_rope_tables(kb, C, io):
    for b in range(NSEQ):
        sl = slice(b * SEQ, (b + 1) * SEQ)
        with contextlib.ExitStack() as st:
            pi_ = kb.sbuf(st, "rp_pi", [64, SEQ], I32)
            dma(kb, "sp", pi_, pi_[:, :], io.positions, io.positions[0:1, sl].partition_broadcast(64))
            pf = kb.sbuf(st, "rp_pf", [64, SEQ], F32)
            cp(kb, "dve", pf, pf[:, :], pi_, pi_[:, :])
            idx_i = kb.sbuf(st, "rp_ii", [64, 1], I32)
            kb.op("pool", lambda e: e.iota(idx_i[:, :], pattern=[[0, 1]], base=0, channel_multiplier=1), r=[], w=[idx_i])
            idx = kb.sbuf(st, "rp_if", [64, 1], F32)
            cp(kb, "dve", idx, idx[:, :], idx_i, idx_i[:, :])
            tsc(kb, "dve", idx, idx[32:64, :], idx, idx[32:64, :], -32.0, None, ALU.add)
            invf = kb.sbuf(st, "rp_inv", [64, 1], F32)
            act(kb, invf, invf[:, :], idx, idx[:, :], AF.Exp, scale=float(-np.log(10000.0) / 32.0))
            ang = kb.sbuf(st, "rp_ang", [64, SEQ], F32)
            tsc(kb, "dve", ang, ang[:, :], pf, pf[:, :], invf[:, 0:1], None, ALU.mult, extra_r=[invf])
            res = kb.sbuf(st, "rp_res", [64, SEQ], F32)
            for which, shift in ((0, np.pi / 2.0), (1, 0.0)):
                with contextlib.ExitStack() as s2:
                    sin_reduced(kb, s2, "rp_c", res, res[:, :], ang, ang[:, :], shift, [64, SEQ])
                    if which == 1:
                        tsc(kb, "dve", res, res[0:32, :], res, res[0:32, :], -1.0, None, ALU.mult)
                    dma(kb, "sp", io.cs, io.cs[which, :, sl], res, res[:, :])
                    kb.barrier()
            kb.barrier()


def rope_apply(kb, wk, src_b, src_ap, cos_b, sin_b, dst_b, dst_ap, n):
    x, xs, t1 = wk["x"], wk["xs"], wk["t1"]
    cp(kb, "act", x, x[:, 0:n], src_b, src_ap)
    cp(kb, "dve", xs, xs[0:32, 0:n], x, x[32:64, 0:n])
    cp(kb, "dve", xs, xs[32:64, 0:n], x, x[0:32, 0:n])
    tt(kb, "dve", t1, t1[:, 0:n], x, x[:, 0:n], cos_b[0], cos_b[1], ALU.mult)
    tt(kb, "dve", xs, xs[:, 0:n], xs, xs[:, 0:n], sin_b[0], sin_b[1], ALU.mult)
    tt(kb, "dve", dst_b, dst_ap, t1, t1[:, 0:n], xs, xs[:, 0:n], ALU.add)


def mla_seq(kb, C, io, l, b, load_hT, omT_dst, dbg):
    w_in = io.w_in
    scale = float(192 ** -0.5)
    with contextlib.ExitStack() as st:
        cqn = kb.sbuf(st, "cqn", [128, 3, SEQ], BF16)
        ckvn = kb.sbuf(st, "ckvn", [128, 2, SEQ], BF16)
        krT = kb.sbuf(st, "krT", [64, SEQ], BF16)
        cosb = kb.sbuf(st, "cosb", [64, SEQ], F32)
        sinb = kb.sbuf(st, "sinb", [64, SEQ], F32)
        dma(kb, "sp", cosb, cosb[:, :], io.cs, io.cs[0, :, b * SEQ:(b + 1) * SEQ])
        dma(kb, "sp", sinb, sinb[:, :], io.cs, io.cs[1, :, b * SEQ:(b + 1) * SEQ])
        rwk = dict(x=kb.sbuf(st, "rp_x", [64, 512], F32), xs=kb.sbuf(st, "rp_xs", [64, 512], F32),
                   t1=kb.sbuf(st, "rp_t1", [64, 512], F32))
        with contextlib.ExitStack() as s2:
            hT_b = load_hT(s2)
            pss = Rot([kb.psum(s2, "ps_m%d" % i, [128, 512], F32) for i in range(3)])
            psn = Rot([kb.psum(s2, "ps_mn%d" % i, [128, 512], F32) for i in range(2)])
            for nm, off, nt_, dstn, wnorm in (("cq", OFF_CQ, 3, cqn, io.mla_q_norm_w), ("ckv", OFF_CKV, 2, ckvn, io.mla_kv_norm_w)):
                wt = kb.sbuf(s2, "w_" + nm, [128, 8, nt_ * 128], BF16)
                load_w_fm(kb, w_in, w_in[l, :, off:off + nt_ * 128], wt)
                nw = kb.sbuf(s2, "nw_" + nm, [128, nt_], F32)
                for t_ in range(nt_):
                    dma(kb, "sp", nw, nw[:, t_:t_ + 1], wnorm, wnorm[l, t_ * 128:(t_ + 1) * 128, :])
                raw = kb.sbuf(s2, "raw_" + nm, [128, nt_, 512], F32)
                sq = kb.sbuf(s2, "sq_" + nm, [128, nt_, 512], F32)
                rs = kb.sbuf(s2, "rs_" + nm, [128, 512], F32)
                for n in range(4):
                    sl = slice(n * 512, (n + 1) * 512)
                    for t_ in range(nt_):
                        ps = pss.next()
                        for k in range(8):
                            mm(kb, ps, ps[:, :], wt, wt[:, k, t_ * 128:(t_ + 1) * 128], hT_b, hT_b[:, k, sl],
                               start=(k == 0), stop=(k == 7))
                        cp(kb, "act", raw, raw[:, t_, :], ps, ps[:, :])
                    tt(kb, "dve", sq, sq[:, :, :], raw, raw[:, :, :], raw, raw[:, :, :], ALU.mult)
                    pn = psn.next()
                    for t_ in range(nt_):
                        mm(kb, pn, pn[:, :], C.ones_f, C.ones_f[:, :], sq, sq[:, t_, :], start=(t_ == 0), stop=(t_ == nt_ - 1))
                    tsc(kb, "dve", rs, rs[:, :], pn, pn[:, :], 1.0 / (nt_ * 128), None, ALU.mult)
                    rsqrt(kb, C, rs, rs[:, :], rs, rs[:, :], RMS_EPS)
                    for t_ in range(nt_):
                        stt(kb, "dve", dstn, dstn[:, t_, sl], raw, raw[:, t_, :], nw[:, t_:t_ + 1], rs, rs[:, :],
                            ALU.mult, ALU.mult, extra_r=[nw])
            wkr = kb.sbuf(s2, "w_kr", [128, 8, 64], BF16)
            load_w_fm(kb, w_in, w_in[l, :, OFF_KR:OFF_KR + 64], wkr)
            for n in range(4):
                sl = slice(n * 512, (n + 1) * 512)
                ps = pss.next()
                for k in range(8):
                    mm(kb, ps, ps[0:64, :], wkr, wkr[:, k, :], hT_b, hT_b[:, k, sl], start=(k == 0), stop=(k == 7))
                rope_apply(kb, rwk, ps, ps[0:64, :], (cosb, cosb[:, sl]), (sinb, sinb[:, sl]), krT, krT[:, sl], 512)
            kb.barrier()
        with contextlib.ExitStack() as s2:
            wq = Rot([kb.sbuf(s2, "w_uq%d" % i, [128, 3, 192], BF16) for i in range(2)])
            wkv = Rot([kb.sbuf(s2, "w_ukv%d" % i, [128, 2, 256], BF16) for i in range(2)])
            qnT = Rot([kb.sbuf(s2, "qnT%d" % i, [128, SEQ], BF16) for i in range(2)])
            qrT = Rot([kb.sbuf(s2, "qrT%d" % i, [64, SEQ], BF16) for i in range(2)])
            knT = Rot([kb.sbuf(s2, "knT%d" % i, [128, SEQ], BF16) for i in range(2)])
            vtk = Rot([kb.sbuf(s2, "vtk%d" % i, [128, NCH, 128], BF16) for i in range(2)])
            pex = Rot([kb.sbuf(s2, "pex%d" % i, [128, 512], BF16) for i in range(3)])
            oh = Rot([kb.sbuf(s2, "oh%d" % i, [128, SEQ], BF16) for i in range(2)])
            rden = kb.sbuf(s2, "rden", [128, 512], F32)
            pss = Rot([kb.psum(s2, "ps_a%d" % i, [128, 512], F32) for i in range(3)])
            pso = Rot([kb.psum(s2, "ps_o%d" % i, [128, 512], F32) for i in range(2)])
            psd = Rot([kb.psum(s2, "ps_d%d" % i, [128, 512], F32) for i in range(2)])
            psv = kb.psum(s2, "ps_v", [128, 4, 128], F32)
            for h in range(H):
                wq_ = wq.next()
                load_w_fm(kb, io.w_uq, io.w_uq[l, :, h * 192:(h + 1) * 192], wq_)
                wkv_ = wkv.next()
                load_w_fm(kb, io.w_ukv, io.w_ukv[l, :, h * 256:(h + 1) * 256], wkv_)
                qn, qr, kn, vt, o_ = qnT.next(), qrT.next(), knT.next(), vtk.next(), oh.next()
                for n in range(4):
                    sl = slice(n * 512, (n + 1) * 512)
                    ps = pss.next()
                    for k in range(3):
                        mm(kb, ps, ps[:, :], wq_, wq_[:, k, 0:128], cqn, cqn[:, k, sl], start=(k == 0), stop=(k == 2))
                    cp(kb, "act", qn, qn[:, sl], ps, ps[:, :])
                    ps = pss.next()
                    for k in range(3):
                        mm(kb, ps, ps[0:64, :], wq_, wq_[:, k, 128:192], cqn, cqn[:, k, sl], start=(k == 0), stop=(k == 2))
                    rope_apply(kb, rwk, ps, ps[0:64, :], (cosb, cosb[:, sl]), (sinb, sinb[:, sl]), qr, qr[:, sl], 512)
                    ps = pss.next()
                    for k in range(2):
                        mm(kb, ps, ps[:, :], wkv_, wkv_[:, k, 0:128], ckvn, ckvn[:, k, sl], start=(k == 0), stop=(k == 1))
                    cp(kb, "dve", kn, kn[:, sl], ps, ps[:, :])
                    for t4 in range(4):
                        tkn = n * 4 + t4
                        for k in range(2):
                            mm(kb, psv, psv[:, t4, :], ckvn, ckvn[:, k, tkn * 128:(tkn + 1) * 128], wkv_, wkv_[:, k, 128:256],
                               start=(k == 0), stop=(k == 1))
                    cp(kb, "act", vt, vt[:, n * 4:(n + 1) * 4, :], psv, psv[:, :, :])
                for qb in range(4):
                    qs = slice(qb * 512, (qb + 1) * 512)
                    po = pso.next()
                    pd = psd.next()
                    prev = None
                    for kt in range(NCH + 1):
                        cur = None
                        if kt < NCH:
                            ks = slice(kt * 128, (kt + 1) * 128)
                            ps = pss.next()
                            mm(kb, ps, ps[:, :], kn, kn[:, ks], qn, qn[:, qs], start=True, stop=False)
                            mm(kb, ps, ps[:, :], krT, krT[:, ks], qr, qr[:, qs], start=False, stop=True)
                            cur = pex.next()
                            act(kb, cur, cur[:, :], ps, ps[:, :], AF.Exp, scale=scale)
                        if prev is not None:
                            k0 = kt - 1
                            mm(kb, po, po[:, :], vt, vt[:, k0, :], prev, prev[:, :], start=(k0 == 0), stop=(k0 == NCH - 1))
                            mm(kb, pd, pd[:, :], C.ones_b, C.ones_b[:, :], prev, prev[:, :], start=(k0 == 0), stop=(k0 == NCH - 1))
                        prev = cur
                    kb.op("dve", lambda e, pd=pd: e.reciprocal(rden[:, :], pd[:, :]), r=[pd], w=[rden])
                    tt(kb, "dve", o_, o_[:, qs], po, po[:, :], rden, rden[:, :], ALU.mult)
                db, dap = omT_dst(h)
                dma(kb, "sp", db, dap, o_, o_[:, :])
            kb.barrier()


def mixer_out(kb, C, io, l, b, load_hT, og_scr, om_scr, dbg):
    with contextlib.ExitStack() as st:
        yT = kb.sbuf(st, "yT", [128, 8, SEQ], BF16)
        with contextlib.ExitStack() as s2:
            hT_b = load_hT(s2)
            ogT = kb.sbuf(s2, "ogT", [128, 8, SEQ], BF16)
            omT = kb.sbuf(s2, "omT", [128, 8, SEQ], BF16)
            dma(kb, "sp", ogT, ogT[:, :, :], og_scr, og_scr[b, :, :, :])
            dma(kb, "sp", omT, omT[:, :, :], om_scr, om_scr[b, :, :, :])
            wr = [Rot([kb.sbuf(s2, "wo%d_%d" % (j, i), [128, 8, 128], BF16) for i in range(2)]) for j in range(4)]
            ps = [Rot([kb.psum(s2, "ps_y%d_%d" % (j, i), [128, 512], F32) for i in range(2)]) for j in range(4)]
            sg = Rot([kb.sbuf(s2, "sg%d" % i, [128, 512], F32) for i in range(4)])
            tq = Rot([kb.sbuf(s2, "tq%d" % i, [128, 512], F32) for i in range(4)])
            for m in range(8):
                ms = slice(m * 128, (m + 1) * 128)
                w4 = [r_.next() for r_ in wr]
                load_w_fm(kb, io.w_o_gdn, io.w_o_gdn[l, :, ms], w4[0])
                load_w_fm(kb, io.w_o_mla, io.w_o_mla[l, :, ms], w4[1])
                load_w_fm(kb, io.w_in, io.w_in[l, :, OFF_G + m * 128:OFF_G + (m + 1) * 128], w4[2])
                load_w_fm(kb, io.w_in, io.w_in[l, :, OFF_G + 1024 + m * 128:OFF_G + 1024 + (m + 1) * 128], w4[3])
                for n in range(4):
                    sl = slice(n * 512, (n + 1) * 512)
                    p4 = [r_.next() for r_ in ps]
                    for j, src in enumerate((ogT, omT, hT_b, hT_b)):
                        for k in range(8):
                            mm(kb, p4[j], p4[j][:, :], w4[j], w4[j][:, k, :], src, src[:, k, sl], start=(k == 0), stop=(k == 7))
                    s1, s2_ = sg.next(), sg.next()
                    act(kb, s1, s1[:, :], p4[2], p4[2][:, :], AF.Sigmoid)
                    act(kb, s2_, s2_[:, :], p4[3], p4[3][:, :], AF.Sigmoid)
                    t1, t2 = tq.next(), tq.next()
                    tt(kb, "dve", t1, t1[:, :], p4[0], p4[0][:, :], s1, s1[:, :], ALU.mult)
                    tt(kb, "dve", t2, t2[:, :], p4[1], p4[1][:, :], s2_, s2_[:, :], ALU.mult)
                    tt(kb, "pool", yT, yT[:, m, sl], t1, t1[:, :], t2, t2[:, :], ALU.add)
            kb.barrier()
        with contextlib.ExitStack() as s2:
            wo = kb.sbuf(s2, "w_out", [128, 8, D], BF16)
            load_w_fm(kb, io.w_out, io.w_out[l, :, :], wo)
            mT = kb.sbuf(s2, "mT", [128, 8, 512], F32)
            psm = Rot([kb.psum(s2, "ps_mo%d" % i, [128, 512], F32) for i in range(2)])
            pst = Rot([kb.psum(s2, "ps_mt%d" % i, [128, 4, 128], F32) for i in range(4)])
            hold = Rot([kb.sbuf(s2, "hold%d" % i, [128, D], F32) for i in range(2)])
            state = {"n": -1}

            def src(i, buf):
                ti = i - b * NCH
                n, t4 = ti // 4, ti % 4
                if n != state["n"]:
                    state["n"] = n
                    sl = slice(n * 512, (n + 1) * 512)
                    for m in range(8):
                        pm = psm.next()
                        for k in range(8):
                            mm(kb, pm, pm[:, :], wo, wo[:, k, m * 128:(m + 1) * 128], yT, yT[:, k, sl], start=(k == 0), stop=(k == 7))
                        cp(kb, "act", mT, mT[:, m, :], pm, pm[:, :])
                ho = hold.next()
                dma(kb, "pool", ho, ho[:, :], io.h_res[i], io.h_res[i][i * 128:(i + 1) * 128, :])
                for half in range(2):
                    pt = pst.next()
                    for mm_ in range(4):
                        m = half * 4 + mm_
                        tr(kb, pt, pt[:, mm_, :], mT, mT[:, m, t4 * 128:(t4 + 1) * 128], C.id_f, C.id_f[:, :])
                    stt(kb, "dve", buf, buf[:, half * 512:(half + 1) * 512], ho, ho[:, half * 512:(half + 1) * 512], float(DN_ALPHA),
                        pt, pt[:, :, :].rearrange("p a b -> p (a b)"), ALU.mult, ALU.add)
            emit_ln_rows(kb, C, s2, "l1_", src, (io.ln1_g, io.ln1_g[l:l + 1, :]), (io.ln1_b, io.ln1_b[l:l + 1, :]), io,
                         list(range(b * NCH, (b + 1) * NCH)), io.h_mid)
            kb.barrier()


def stage_mixer(kb, C, io, l, seqs, dbg, parts=("gdn", "mla", "out")):
    og_scr, om_scr = io.og_scr, io.om_scr
    with contextlib.ExitStack() as st0:
        cwT = kb.sbuf(st0, "cwT", [128, 24, 5], F32)
        with contextlib.ExitStack() as s2:
            cwr = kb.sbuf(s2, "cwr", [5, 3072], F32)
            dma(kb, "sp", cwr, cwr[:, :], io.conv_w, io.conv_w[l, :, :])
            pc = kb.psum(s2, "ps_cw", [128, 24, 8], F32)
            for t_ in range(24):
                tr(kb, pc, pc[:, t_, 0:5], cwr, cwr[0:5, t_ * 128:(t_ + 1) * 128], C.id_f, C.id_f[0:5, 0:5])
            cp(kb, "dve", cwT, cwT[:, :, :], pc, pc[:, :, 0:5])
            kb.barrier()
        for b in seqs:
            def load_hT(stk, b=b):
                hT_b = kb.sbuf(stk, "hT_b", [128, 8, SEQ], BF16)
                kb.dma("sp", lambda e: e.dma_start(out=hT_b[:, :, :], in_=io.hT[0][:, :, b * SEQ:(b + 1) * SEQ]),
                       r=[io.hT[i] for i in range(b * NCH, (b + 1) * NCH)], w=[hT_b])
                return hT_b
            if "gdn" in parts:
                gdn_seq(kb, C, io, l, b, load_hT, og_scr, cwT, dbg)
            if "mla" in parts:
                mla_seq(kb, C, io, l, b, load_hT, lambda h, b=b: (om_scr, om_scr[b, :, h, :]), dbg)
            if "out" in parts:
                mixer_out(kb, C, io, l, b, load_hT, og_scr, om_scr, dbg)


def idma(kb, ob, oap, out_off, ib, iap, in_off, extra_r=(), nrows=None):
    if not hasattr(kb, "bc_regs"):
        kb.bc_regs = {}
    if nrows not in kb.bc_regs:
        kb.bc_regs[nrows] = kb.nc.gpsimd.to_reg(nrows - 1)
    bc = kb.bc_regs[nrows]

    def fn(e):
        return e.indirect_dma_start(
            out=oap, out_offset=(bass.IndirectOffsetOnAxis(ap=out_off, axis=0) if out_off is not None else None),
            in_=iap, in_offset=(bass.IndirectOffsetOnAxis(ap=in_off, axis=0) if in_off is not None else None),
            bounds_check=bc, oob_is_err=False)
    return kb.dma("pool", fn, r=[ib] + list(extra_r), w=[ob])


def stage_moe(kb, C, io, l, dst_tiles, dbg):
    NEG = -1.0e30
    with contextlib.ExitStack() as st:
        ohE = kb.sbuf(st, "ohE", [128, NT, 2, NE], F32)
        gate = kb.sbuf(st, "gate", [128, NT, 2], F32)
        destI = kb.sbuf(st, "destI", [128, NT, 2], I32)
        BEi = kb.sbuf(st, "BEi", [128, NBLK], I32)
        offs_gu = kb.sbuf(st, "offs_gu", [128, NBLK, 8], I32)
        offs_d = kb.sbuf(st, "offs_d", [128, NBLK, 4], I32)
        zt = kb.sbuf(st, "zt", [128, 4, D], F32)
        memset(kb, "dve", zt, zt[:, :, :], 0.0)
        for j in range(NROWS // 512):
            kb.dma("pool", lambda e, j=j: e.dma_start(out=io.xb[j * 512:(j + 1) * 512, :].rearrange("(a p) d -> p a d", p=128),
                                                      in_=zt[:, :, :]), r=[zt], w=[io.xb])
        with contextlib.ExitStack() as s2:
            kb.serialize = bool(dbg.get("serial_moe", False))
            wr = kb.sbuf(s2, "wr", [128, 8, 72], F32)
            dma(kb, "sp", wr, wr[:, :, 0:8], io.w_router_group, io.w_router_group[l, :, :].rearrange("(k p) g -> p k g", p=128))
            dma(kb, "sp", wr, wr[:, :, 8:72], io.w_router_expert, io.w_router_expert[l, :, :].rearrange("(k p) g -> p k g", p=128))
            br = kb.sbuf(s2, "br", [128, 72], F32)
            dma(kb, "sp", br, br[:, 0:8], io.b_router_group, io.b_router_group[l:l + 1, :].partition_broadcast(128))
            dma(kb, "sp", br, br[:, 8:72], io.b_router_expert, io.b_router_expert[l:l + 1, :].partition_broadcast(128))
            xt = Rot([kb.sbuf(s2, "mx%d" % i, [128, D], F32) for i in range(2)])
            hTf = kb.sbuf(s2, "hTf", [128, 8, 128], F32)
            pst = Rot([kb.psum(s2, "ps_rt%d" % i, [128, 4, 128], F32) for i in range(2)])
            psl = kb.psum(s2, "ps_rl", [128, 72], F32)
            lgall = kb.sbuf(s2, "lgall", [128, NT, 72], F32)
            for i in range(NT):
                x = xt.next()
                dma(kb, "sp", x, x[:, :], io.h_mid[i], io.h_mid[i][i * 128:(i + 1) * 128, :])
                for half in range(2):
                    pt = pst.next()
                    for c4 in range(4):
                        c = half * 4 + c4
                        tr(kb, pt, pt[:, c4, :], x, x[:, c * 128:(c + 1) * 128], C.id_f, C.id_f[:, :], inc=(c4 == 3))
                    cp(kb, "act", hTf, hTf[:, half * 4:(half + 1) * 4, :], pt, pt[:, :, :])
                for k in range(8):
                    mm(kb, psl, psl[:, :], hTf, hTf[:, k, :], wr, wr[:, k, :], start=(k == 0), stop=(k == 7))
                tt(kb, "dve", lgall, lgall[:, i, :], psl, psl[:, :], br, br[:, :], ALU.add)
            A3 = lambda nm, n: kb.sbuf(s2, "rb_" + nm, [128, NT, n], F32)
            A2 = lambda nm: kb.sbuf(s2, "rb_" + nm, [128, NT], F32)
            LG = lgall[:, :, 0:8]
            LE4 = lgall[:, :, 8:72].rearrange("p t (g e) -> p t g e", g=8)
            bc8 = lambda buf: buf[:, :].unsqueeze(2).to_broadcast([128, NT, 8])
            gmax, se, pg, m1, m2, r_, dd = A2("gmax"), A2("se"), A2("pg"), A2("m1"), A2("m2"), A2("r"), A2("dd")
            ohg, eg, les, oh1, le2, oh2 = A3("ohg", 8), A3("eg", 8), A3("les", 8), A3("oh1", 8), A3("le2", 8), A3("oh2", 8)
            t64 = A3("t64", 64)
            t64v = t64[:, :, :].rearrange("p t (g e) -> p t g e", g=8)
            kb.op("dve", lambda e: e.reduce_max(out=gmax[:, :], in_=LG, axis=AX.X), r=[lgall], w=[gmax])
            tt(kb, "dve", ohg, ohg[:, :, :], lgall, LG, gmax, bc8(gmax), ALU.is_equal)
            tt(kb, "dve", eg, eg[:, :, :], lgall, LG, gmax, bc8(gmax), ALU.subtract)
            act(kb, eg, eg[:, :, :], eg, eg[:, :, :], AF.Exp)
            kb.op("dve", lambda e: e.reduce_sum(out=se[:, :], in_=eg[:, :, :], axis=AX.X), r=[eg], w=[se])
            kb.op("dve", lambda e: e.reciprocal(pg[:, :], se[:, :]), r=[se], w=[pg])
            tt(kb, "dve", t64, t64v, lgall, LE4, ohg, ohg[:, :, :].unsqueeze(3).to_broadcast([128, NT, 8, 8]), ALU.mult)
            kb.op("dve", lambda e: e.reduce_sum(out=les[:, :, :], in_=t64[:, :, :].rearrange("p t (g e) -> p t e g", g=8), axis=AX.X),
                  r=[t64], w=[les])
            kb.op("dve", lambda e: e.reduce_max(out=m1[:, :], in_=les[:, :, :], axis=AX.X), r=[les], w=[m1])
            tt(kb, "dve", oh1, oh1[:, :, :], les, les[:, :, :], m1, bc8(m1), ALU.is_equal)
            stt(kb, "dve", le2, le2[:, :, :], oh1, oh1[:, :, :], NEG, les, les[:, :, :], ALU.mult, ALU.add)
            kb.op("dve", lambda e: e.reduce_max(out=m2[:, :], in_=le2[:, :, :], axis=AX.X), r=[le2], w=[m2])
            tt(kb, "dve", oh2, oh2[:, :, :], le2, le2[:, :, :], m2, bc8(m2), ALU.is_equal)
            tt(kb, "dve", r_, r_[:, :], m2, m2[:, :], m1, m1[:, :], ALU.subtract)
            act(kb, r_, r_[:, :], r_, r_[:, :], AF.Exp)
            tsc(kb, "dve", dd, dd[:, :], r_, r_[:, :], 1.0, None, ALU.add)
            kb.op("dve", lambda e: e.reciprocal(dd[:, :], dd[:, :]), r=[dd], w=[dd])
            tt(kb, "dve", dd, dd[:, :], dd, dd[:, :], pg, pg[:, :], ALU.mult)
            cp(kb, "dve", gate, gate[:, :, 0], dd, dd[:, :])
            tt(kb, "dve", gate, gate[:, :, 1], dd, dd[:, :], r_, r_[:, :], ALU.mult)
            for k_, oh in ((0, oh1), (1, oh2)):
                tt(kb, "dve", ohE, ohE[:, :, k_, :].rearrange("p t (g e) -> p t g e", g=8), ohg,
                   ohg[:, :, :].unsqueeze(3).to_broadcast([128, NT, 8, 8]), oh, oh[:, :, :].unsqueeze(2).to_broadcast([128, NT, 8, 8]), ALU.mult)
            kb.barrier()
        with contextlib.ExitStack() as s2:
            ohs = kb.sbuf(s2, "ohs", [128, NT, NE], F32)
            cum = kb.sbuf(s2, "cum", [128, NT + 1, NE], F32)
            tt(kb, "dve", ohs, ohs[:, :, :], ohE, ohE[:, :, 0, :], ohE, ohE[:, :, 1, :], ALU.add)
            memset(kb, "dve", cum, cum[:, 0, :], 0.0)
            for i in range(NT):
                tt(kb, "dve", cum, cum[:, i + 1, :], cum, cum[:, i, :], ohs, ohs[:, i, :], ALU.add)
            striU = kb.sbuf(s2, "striU", [128, 128], F32)
            tt(kb, "dve", striU, striU[:, :], C.triF, C.triF[:, :], C.id_f, C.id_f[:, :], ALU.subtract)
            pc = kb.psum(s2, "ps_cnt", [64, 128], F32)
            for i in range(NT):
                mm(kb, pc, pc[:, :], ohs, ohs[:, i, :], C.ones_f, C.ones_f[:, :], start=(i == 0), stop=(i == NT - 1))
            cntT = kb.sbuf(s2, "cntT", [64, 128], F32)
            tsc(kb, "dve", cntT, cntT[:, :], pc, pc[:, :], 127.0, None, ALU.add)
            ci = kb.sbuf(s2, "cnt_i", [64, 128], I32)
            cp(kb, "dve", ci, ci[:, :], cntT, cntT[:, :])
            tsc(kb, "dve", ci, ci[:, :], ci, ci[:, :], 7, None, ALU.arith_shift_right)
            tsc(kb, "dve", ci, ci[:, :], ci, ci[:, :], 7, None, ALU.logical_shift_left)
            padT = kb.sbuf(s2, "padT", [64, 128], F32)
            cp(kb, "dve", padT, padT[:, :], ci, ci[:, :])
            pps = kb.psum(s2, "ps_pst", [128, 64], F32)
            mm(kb, pps, pps[:, :], padT, padT[:, :], striU, striU[0:64, 0:64])
            pstart = kb.sbuf(s2, "pstart", [128, NE], F32)
            cp(kb, "dve", pstart, pstart[:, :], pps, pps[:, :])
            ppe = kb.psum(s2, "ps_pend", [64, 128], F32)
            mm(kb, ppe, ppe[:, :], C.triF, C.triF[0:64, 0:64], padT, padT[:, :])
            jrow_i = kb.sbuf(s2, "jrow_i", [64, 128], I32)
            kb.op("pool", lambda e: e.iota(jrow_i[:, :], pattern=[[128, 128]], base=0, channel_multiplier=0), r=[], w=[jrow_i])
            jrow = kb.sbuf(s2, "jrow", [64, 128], F32)
            cp(kb, "dve", jrow, jrow[:, :], jrow_i, jrow_i[:, :])
            cmpm = kb.sbuf(s2, "cmpm", [64, 128], F32)
            tt(kb, "dve", cmpm, cmpm[:, :], ppe, ppe[:, :], jrow, jrow[:, :], ALU.is_le)
            pbe = kb.psum(s2, "ps_be", [128, 128], F32)
            mm(kb, pbe, pbe[:, :], C.ones_f, C.ones_f[0:64, :], cmpm, cmpm[:, :])
            bef = kb.sbuf(s2, "bef", [128, NBLK], F32)
            unused = kb.sbuf(s2, "unused", [128, NBLK], F32)
            tsc(kb, "dve", unused, unused[:, :], pbe, pbe[:, :], 63.5, 4194304.0, ALU.is_gt, ALU.mult)
            tsc(kb, "dve", bef, bef[:, :], pbe, pbe[:, :], 63.0, None, ALU.min)
            cp(kb, "dve", BEi, BEi[:, :], bef, bef[:, :])
            prow_i = kb.sbuf(s2, "prow_i", [128, 8], I32)
            kb.op("pool", lambda e: e.iota(prow_i[:, :], pattern=[[128, 8]], base=0, channel_multiplier=1), r=[], w=[prow_i])
            prow = kb.sbuf(s2, "prow", [128, 8], F32)
            cp(kb, "dve", prow, prow[:, :], prow_i, prow_i[:, :])
            of = kb.sbuf(s2, "off_f", [128, NBLK, 8], F32)
            stt(kb, "dve", of, of[:, :, :], bef, bef[:, :].unsqueeze(2).to_broadcast([128, NBLK, 8]), 128.0, prow,
                prow[:, 0:1].unsqueeze(1).to_broadcast([128, NBLK, 8]), ALU.mult, ALU.add)
            tsc(kb, "dve", of, of[:, :, :], of, of[:, :, :], float(l * NE * 128), None, ALU.add)
            tt(kb, "dve", of, of[:, :, :], of, of[:, :, :], unused, unused[:, :].unsqueeze(2).to_broadcast([128, NBLK, 8]), ALU.add)
            cp(kb, "dve", offs_gu, offs_gu[:, :, :], of, of[:, :, :])
            stt(kb, "dve", of, of[:, :, 0:4], bef, bef[:, :].unsqueeze(2).to_broadcast([128, NBLK, 4]), 512.0, prow,
                prow[:, 0:4].unsqueeze(1).to_broadcast([128, NBLK, 4]), ALU.mult, ALU.add)
            tsc(kb, "dve", of, of[:, :, 0:4], of, of[:, :, 0:4], float(l * NE * DEXP), None, ALU.add)
            tt(kb, "dve", of, of[:, :, 0:4], of, of[:, :, 0:4], unused, unused[:, :].unsqueeze(2).to_broadcast([128, NBLK, 4]), ALU.add)
            cp(kb, "dve", offs_d, offs_d[:, :, :], of, of[:, :, 0:4])
            ppf = Rot([kb.psum(s2, "ps_pf%d" % i, [128, 64], F32) for i in range(2)])
            tq = kb.sbuf(s2, "tq64", [128, NE], F32)
            tq2 = kb.sbuf(s2, "tq64b", [128, NE], F32)
            dsf = kb.sbuf(s2, "dsf", [128, NT, 2], F32)
            for i in range(NT):
                pp = ppf.next()
                mm(kb, pp, pp[:, :], striU, striU[:, :], ohs, ohs[:, i, :], start=True, stop=False)
                mm(kb, pp, pp[:, :], C.ones_f, C.ones_f[:, :], cum, cum[:, i, :], start=False, stop=True)
                tt(kb, "dve", tq, tq[:, :], pp, pp[:, :], pstart, pstart[:, :], ALU.add)
                for k_ in range(2):
                    tt(kb, "dve", tq2, tq2[:, :], tq, tq[:, :], ohE, ohE[:, i, k_, :], ALU.mult)
                    kb.op("dve", lambda e, i=i, k_=k_: e.reduce_sum(out=dsf[:, i, k_:k_ + 1], in_=tq2[:, :], axis=AX.X), r=[tq2], w=[dsf])
            cp(kb, "dve", destI, destI[:, :, :], dsf, dsf[:, :, :])
            kb.barrier()
        kb.serialize = False
        xb = io.xb
        with contextlib.ExitStack() as s2:
            xt = Rot([kb.sbuf(s2, "sx%d" % i, [128, D], F32) for i in range(3)])
            for i in range(NT):
                x = xt.next()
                dma(kb, "sp", x, x[:, :], io.h_mid[i], io.h_mid[i][i * 128:(i + 1) * 128, :])
                for k_ in range(2):
                    idma(kb, xb, xb[:, :], destI[:, i, k_:k_ + 1], x, x[:, :], None, extra_r=[destI], nrows=NROWS)
            kb.barrier()
        yb = io.yb
        wg_v = io.w_gate[:, :, :, :].rearrange("l e (p c) n -> (l e p) (c n)", c=8)
        wu_v = io.w_up[:, :, :, :].rearrange("l e (p c) n -> (l e p) (c n)", c=8)
        wd_v = io.w_down[:, :, :, :].rearrange("l e (p c) n -> (l e p) (c n)", c=4)
        with contextlib.ExitStack() as s2:
            WDT = BF16
            wg = Rot([kb.sbuf(s2, "wg%d" % i, [128, 8, DEXP], WDT) for i in range(3)])
            wu = Rot([kb.sbuf(s2, "wu%d" % i, [128, 8, DEXP], WDT) for i in range(3)])
            wd = Rot([kb.sbuf(s2, "wd%d" % i, [128, 4, D], WDT) for i in range(3)])
            xr = Rot([kb.sbuf(s2, "xr%d" % i, [128, D], F32) for i in range(3)])
            xrb = Rot([kb.sbuf(s2, "xrb%d" % i, [128, D], BF16) for i in range(3)])
            xT = Rot([kb.sbuf(s2, "xT%d" % i, [128, 8, 128], BF16) for i in range(3)])
            hid = Rot([kb.sbuf(s2, "hid%d" % i, [128, 4, 128], BF16) for i in range(3)])
            sg_ = Rot([kb.sbuf(s2, "sgx%d" % i, [128, 4, 128], F32) for i in range(2)])
            yo = Rot([kb.sbuf(s2, "yo%d" % i, [128, D], F32) for i in range(3)])
            pst = Rot([kb.psum(s2, "ps_xt%d" % i, [128, 8, 128], BF16) for i in range(2)])
            psg = Rot([kb.psum(s2, "ps_g%d" % i, [128, 4, 128], F32) for i in range(2)])
            psu = Rot([kb.psum(s2, "ps_u%d" % i, [128, 4, 128], F32) for i in range(2)])
            psy = Rot([kb.psum(s2, "ps_yy%d" % i, [128, 512], F32) for i in range(2)])
            for j in range(dbg.get("nblk", NBLK)):
                wg_, wu_, wd_ = wg.next(), wu.next(), wd.next()
                NR = DEPTH * NE * 128
                idma(kb, wg_, wg_[:, :, :].rearrange("p c n -> p (c n)"), None, io.w_gate, wg_v, offs_gu[:, j, 0:1], extra_r=[offs_gu], nrows=NR)
                idma(kb, wu_, wu_[:, :, :].rearrange("p c n -> p (c n)"), None, io.w_up, wu_v, offs_gu[:, j, 0:1], extra_r=[offs_gu], nrows=NR)
                idma(kb, wd_, wd_[:, :, :].rearrange("p c n -> p (c n)"), None, io.w_down, wd_v, offs_gu[:, j, 0:1], extra_r=[offs_gu], nrows=NR)
                x = xr.next()
                dma(kb, "pool", x, x[:, :], xb, xb[j * 128:(j + 1) * 128, :])
                xb_ = xrb.next()
                cp(kb, "dve", xb_, xb_[:, :], x, x[:, :])
                xT_ = xT.next()
                pt = pst.next()
                xperm = xb_[:, :].rearrange("r (p c) -> r c p", c=8)
                for c in range(8):
                    tr(kb, pt, pt[:, c, :], xb_, xperm[:, c, :], C.id_b, C.id_b[:, :], inc=(c == 7))
                cp(kb, "act", xT_, xT_[:, :, :], pt, pt[:, :, :])
                pg, pu = psg.next(), psu.next()
                for m in range(4):
                    for c in range(8):
                        mm(kb, pg, pg[:, m, :], wg_, wg_[:, c, :].rearrange("k (p m) -> k m p", m=4)[:, m, :], xT_, xT_[:, c, :],
                           start=(c == 0), stop=(c == 7), inc=(c == 7 and m == 3))
                for m in range(4):
                    for c in range(8):
                        mm(kb, pu, pu[:, m, :], wu_, wu_[:, c, :].rearrange("k (p m) -> k m p", m=4)[:, m, :], xT_, xT_[:, c, :],
                           start=(c == 0), stop=(c == 7), inc=(c == 7 and m == 3))
                s_ = sg_.next()
                act(kb, s_, s_[:, :, :], pg, pg[:, :, :], AF.Silu)
                h_ = hid.next()
                tt(kb, "dve", h_, h_[:, :, :], s_, s_[:, :, :], pu, pu[:, :, :], ALU.mult)
                y_ = yo.next()
                for nh in range(2):
                    py = psy.next()
                    for c in range(4):
                        mm(kb, py, py[:, :], h_, h_[:, c, :], wd_, wd_[:, c, nh * 512:(nh + 1) * 512], start=(c == 0), stop=(c == 3))
                    cp(kb, "act", y_, y_[:, nh * 512:(nh + 1) * 512], py, py[:, :])
                dma(kb, "sp", yb, yb[j * 128:(j + 1) * 128, :], y_, y_[:, :])
            kb.barrier()
        with contextlib.ExitStack() as s2:
            y0 = Rot([kb.sbuf(s2, "gy0_%d" % i, [128, D], F32) for i in range(4)])
            y1 = Rot([kb.sbuf(s2, "gy1_%d" % i, [128, D], F32) for i in range(4)])
            hm = Rot([kb.sbuf(s2, "ghm%d" % i, [128, D], F32) for i in range(4)])

            def src(i, buf):
                a, b_, h_ = y0.next(), y1.next(), hm.next()
                idma(kb, a, a[:, :], None, yb, yb[:, :], destI[:, i, 0:1], extra_r=[destI], nrows=NROWS)
                idma(kb, b_, b_[:, :], None, yb, yb[:, :], destI[:, i, 1:2], extra_r=[destI], nrows=NROWS)
                dma(kb, "pool", h_, h_[:, :], io.h_mid[i], io.h_mid[i][i * 128:(i + 1) * 128, :])
                tsc(kb, "dve", a, a[:, :], a, a[:, :], gate[:, i, 0:1], None, ALU.mult, extra_r=[gate])
                stt(kb, "dve", a, a[:, :], b_, b_[:, :], gate[:, i, 1:2], a, a[:, :], ALU.mult, ALU.add, extra_r=[gate])
                stt(kb, "dve", buf, buf[:, :], h_, h_[:, :], float(DN_ALPHA), a, a[:, :], ALU.mult, ALU.add)
            emit_ln_rows(kb, C, s2, "l2_", src, (io.ln2_g, io.ln2_g[l:l + 1, :]), (io.ln2_b, io.ln2_b[l:l + 1, :]), io,
                         list(range(NT)), dst_tiles, nb=4)
            kb.barrier()


def build_program(dbg=None):
    dbg = dict(dbg or {})
    dbg.setdefault("serial", 0)
    kb = KB()
    io = declare_io(kb)
    with contextlib.ExitStack() as st:
        C = setup_consts(kb, st)
        stage_prologue(kb, C, io, list(range(NT)))
        stage_rope_tables(kb, C, io)
        depth = dbg.get("depth", DEPTH)
        for l in range(depth):
            stage_mixer(kb, C, io, l, list(range(NSEQ)), dbg)
            stage_moe(kb, C, io, l, io.h_res if l < depth - 1 else io.out, dbg)
        kb.finish()
    return kb


def kernel(**inputs):
    n = 8
    kb = build_program()
    in_maps = [make_in_map(inputs, c) for c in range(n)]
    res = run_bass_kernel_spmd(kb.nc, in_maps, core_ids=list(range(n)))
    out = np.concatenate([np.asarray(r["out"], dtype=np.float32).reshape(NSEQ, SEQ, D) for r in res.results], axis=0)
    return out
```
